# Optimizing a Trainium2 kernel written in Bass

```python
import jax, jax.numpy as jnp
from jax import lax
import numpy as np

D_MODEL = 1024
BATCH = 4
SEQ = 8192
DEPTH = 4

HEAD_DIM = 64
N_HEADS_A = 8
N_HEADS_B = 8
N_KV_B = 2
GQA_GROUP = N_HEADS_B // N_KV_B
DILATED_BRANCHES = ((128, 1), (512, 4), (2048, 16))
SWA_WINDOW = 128
BLK = 128
ROPE_THETA = 10000.0
D_FF = 3584
N_EXPERTS = 8
TOP_K = 2
RMS_EPS = 1e-5
WIDTH_A = N_HEADS_A * HEAD_DIM
WIDTH_B = N_HEADS_B * HEAD_DIM
KV_WIDTH_B = N_KV_B * HEAD_DIM
MIX_WIDTH = WIDTH_A + WIDTH_B
IN_SIZES = (WIDTH_A, WIDTH_A, WIDTH_A, WIDTH_B, KV_WIDTH_B, KV_WIDTH_B)
IN_WIDTH = sum(IN_SIZES)
IN_SPLITS = tuple(int(c) for c in np.cumsum(IN_SIZES)[:-1])
N_DENSE = (DEPTH + 1) // 2
N_MOE = DEPTH // 2

kernel_name = "hybrid_dilated_swa_sink_moe_trunk"


def rmsnorm(x, g):
    xf = x.astype(jnp.float32)
    y = xf * lax.rsqrt(jnp.mean(xf * xf, axis=-1, keepdims=True) + RMS_EPS)
    return (y * g.astype(jnp.float32)).astype(x.dtype)


def rope_tables(positions):
    inv = ROPE_THETA ** (-jnp.arange(0, HEAD_DIM, 2, dtype=jnp.float32) / HEAD_DIM)
    ang = positions.astype(jnp.float32)[:, None] * inv[None, :]
    ang = jnp.concatenate([ang, ang], axis=-1)
    return jnp.cos(ang), jnp.sin(ang)


def apply_rope(t, cos, sin):
    t1, t2 = jnp.split(t, 2, axis=-1)
    rot = jnp.concatenate([-t2, t1], axis=-1)
    return t * cos.astype(t.dtype) + rot * sin.astype(t.dtype)


def banded_attention(q, k, v, max_dist, sink=None):
    *lead, G, L, Dh = q.shape
    nb = L // BLK
    qb = q.reshape(*lead, G, nb, BLK, Dh)

    def band(t):
        tb = t.reshape(*lead, nb, BLK, Dh)
        prev = jnp.concatenate([jnp.zeros_like(tb[..., :1, :, :]), tb[..., :-1, :, :]], axis=-3)
        return jnp.concatenate([prev, tb], axis=-2)

    s = jnp.einsum('...gnqd,...nkd->...gnqk', qb, band(k),
                   preferred_element_type=jnp.float32) * (Dh ** -0.5)
    dist = (BLK + jnp.arange(BLK))[:, None] - jnp.arange(2 * BLK)[None, :]
    in_band = (dist >= 0) & (dist <= max_dist)
    has_prev = (jnp.arange(nb) > 0)[:, None, None] | (jnp.arange(2 * BLK) >= BLK)[None, None, :]
    mask = in_band[None] & has_prev
    s = jnp.where(mask, s, -jnp.inf)
    m = jnp.max(s, axis=-1)
    if sink is not None:
        sk = sink.astype(jnp.float32)[..., None, None]
        m = jnp.maximum(m, sk)
    p = jnp.exp(s - m[..., None])
    l = jnp.sum(p, axis=-1)
    if sink is not None:
        l = l + jnp.exp(sk - m)
    o = jnp.einsum('...gnqk,...nkd->...gnqd', p.astype(v.dtype), band(v),
                   preferred_element_type=jnp.float32) / l[..., None]
    lse = m + jnp.log(l)
    return o.reshape(*lead, G, L, Dh).astype(q.dtype), lse.reshape(*lead, G, L)


def dilated_branch(q, k, v, window, dilation):
    B, H, S, Dh = q.shape
    seg = dilation * BLK
    Sp = -(-S // seg) * seg

    def by_stride(t):
        t = jnp.pad(t, ((0, 0), (0, 0), (0, Sp - S), (0, 0)))
        return t.reshape(B, H, Sp // dilation, dilation, Dh).transpose(0, 1, 3, 2, 4)

    o, lse = banded_attention(by_stride(q)[..., None, :, :], by_stride(k), by_stride(v),
                              max_dist=window // dilation)
    o = o[:, :, :, 0].transpose(0, 1, 3, 2, 4).reshape(B, H, Sp, Dh)[:, :, :S]
    lse = lse[:, :, :, 0].transpose(0, 1, 3, 2).reshape(B, H, Sp)[:, :, :S]
    return o, lse


def dilated_mixer(q, k, v):
    outs, lses = zip(*[dilated_branch(q, k, v, w, d) for (w, d) in DILATED_BRANCHES])
    wts = jax.nn.softmax(jnp.stack(lses, axis=0), axis=0)
    o = jnp.einsum('rbhs,rbhsd->bhsd', wts, jnp.stack(outs, axis=0).astype(jnp.float32))
    return o.astype(q.dtype)


def swiglu(h, wg, wu, wd):
    return (jax.nn.silu(h @ wg) * (h @ wu)) @ wd


def moe_swiglu(h, router, wg, wu, wd):
    logits = (h @ router).astype(jnp.float32)
    top_vals, top_idx = lax.top_k(logits, TOP_K)
    gates = jax.nn.softmax(top_vals, axis=-1)
    gate_e = jnp.sum(jax.nn.one_hot(top_idx, N_EXPERTS, dtype=jnp.float32) * gates[..., None], axis=-2)
    gate_e = gate_e.astype(h.dtype)
    out = jnp.zeros_like(h)
    for e in range(N_EXPERTS):
        out = out + gate_e[..., e:e + 1] * swiglu(h, wg[e], wu[e], wd[e])
    return out


def heads(t, n):
    B, S, _ = t.shape
    return t.reshape(B, S, n, HEAD_DIM).transpose(0, 2, 1, 3)


def merge_heads(t):
    B, H, S, Dh = t.shape
    return t.transpose(0, 2, 1, 3).reshape(B, S, H * Dh)


def setup_inputs(seed: int = 0) -> dict:
    key = jax.random.key(seed)
    ks = jax.random.split(key, 20)
    f32 = jnp.float32
    nrm = lambda k, shape, fan_in: jax.random.normal(k, shape, f32) * (fan_in ** -0.5)
    gain = lambda k, shape: 1.0 + 0.02 * jax.random.normal(k, shape, f32)
    return {
        "x": jax.random.normal(ks[0], (BATCH, SEQ, D_MODEL), f32),
        "positions": jnp.arange(SEQ, dtype=jnp.int32),
        "attn_norm": gain(ks[1], (DEPTH, D_MODEL)),
        "w_in": nrm(ks[2], (DEPTH, D_MODEL, IN_WIDTH), D_MODEL),
        "mix_norm_a": gain(ks[3], (DEPTH, WIDTH_A)),
        "mix_norm_b": gain(ks[4], (DEPTH, WIDTH_B)),
        "sinks": 0.5 * jax.random.normal(ks[5], (DEPTH, N_HEADS_B), f32),
        "w_out": nrm(ks[6], (DEPTH, MIX_WIDTH, D_MODEL), MIX_WIDTH) * 0.5,
        "ffn_norm": gain(ks[7], (DEPTH, D_MODEL)),
        "dense_w_gate": nrm(ks[8], (N_DENSE, D_MODEL, D_FF), D_MODEL),
        "dense_w_up": nrm(ks[9], (N_DENSE, D_MODEL, D_FF), D_MODEL),
        "dense_w_down": nrm(ks[10], (N_DENSE, D_FF, D_MODEL), D_FF) * 0.5,
        "router": nrm(ks[11], (N_MOE, D_MODEL, N_EXPERTS), D_MODEL),
        "moe_w_gate": nrm(ks[12], (N_MOE, N_EXPERTS, D_MODEL, D_FF), D_MODEL),
        "moe_w_up": nrm(ks[13], (N_MOE, N_EXPERTS, D_MODEL, D_FF), D_MODEL),
        "moe_w_down": nrm(ks[14], (N_MOE, N_EXPERTS, D_FF, D_MODEL), D_FF) * 0.5,
        "final_norm": gain(ks[15], (D_MODEL,)),
    }


def reference(x, positions, attn_norm, w_in, mix_norm_a, mix_norm_b, sinks, w_out,
              ffn_norm, dense_w_gate, dense_w_up, dense_w_down, router,
              moe_w_gate, moe_w_up, moe_w_down, final_norm):
    B, S, _ = x.shape
    cos, sin = rope_tables(positions)
    for i in range(DEPTH):
        h = rmsnorm(x, attn_norm[i])
        proj = h @ w_in[i]
        qa, ka, va, qb, kb, vb = jnp.split(proj, IN_SPLITS, axis=-1)
        qa = apply_rope(heads(qa, N_HEADS_A), cos, sin)
        ka = apply_rope(heads(ka, N_HEADS_A), cos, sin)
        oa = dilated_mixer(qa, ka, heads(va, N_HEADS_A))
        qb = apply_rope(heads(qb, N_HEADS_B), cos, sin).reshape(B, N_KV_B, GQA_GROUP, S, HEAD_DIM)
        kb = apply_rope(heads(kb, N_KV_B), cos, sin)
        ob, _ = banded_attention(qb, kb, heads(vb, N_KV_B), SWA_WINDOW - 1,
                                 sink=sinks[i].reshape(N_KV_B, GQA_GROUP))
        ob = ob.reshape(B, N_HEADS_B, S, HEAD_DIM)
        mix = jnp.concatenate([rmsnorm(merge_heads(oa), mix_norm_a[i]),
                               rmsnorm(merge_heads(ob), mix_norm_b[i])], axis=-1)
        x = x + mix @ w_out[i]
        h = rmsnorm(x, ffn_norm[i])
        j = i // 2
        if i % 2 == 0:
            x = x + swiglu(h, dense_w_gate[j], dense_w_up[j], dense_w_down[j])
        else:
            x = x + moe_swiglu(h, router[j], moe_w_gate[j], moe_w_up[j], moe_w_down[j])
    return rmsnorm(x, final_norm)
```

```python
import contextlib
import os
import numpy as np
import concourse.bass as bass
import concourse.mybir as mybir
from concourse.bass_utils import run_bass_kernel_spmd

F32 = mybir.dt.float32
BF16 = mybir.dt.bfloat16
I32 = mybir.dt.int32
AF = mybir.ActivationFunctionType
ALU = mybir.AluOpType
AX = mybir.AxisListType

ENGS = ("pe", "act", "dve", "pool", "sp")

NCORES = 8
T = 4096
TH = 2048
TE = T + TH
D = 1024
KC = 8
FF = 3584
NJ = FF // 128
NE = 8
INW = 2304
EPS = 1e-5
DIL = (1, 4, 16)


class Op:
    __slots__ = ("eng", "fn", "deps", "signal", "count", "dma_key", "is_dma", "inc")

    def __init__(self, eng, fn, dma_key, inc=16):
        self.eng = eng
        self.fn = fn
        self.deps = []
        self.signal = False
        self.count = None
        self.dma_key = dma_key
        self.is_dma = dma_key is not None
        self.inc = inc


class Sched:
    def __init__(self, nc, same_engine_sync=True):
        self.nc = nc
        self.ops = {e: [] for e in ENGS}
        self.last_w = {}
        self.readers = {}
        self.dma_counts = {}
        self.same_engine_sync = same_engine_sync

    def op(self, eng, fn, reads=(), writes=(), dma_key=None, inc=16):
        o = Op(eng, fn, dma_key, inc)
        deps = []
        for t in reads:
            w = self.last_w.get(t)
            if w is not None:
                deps.append((w, 0))
        for t in writes:
            w = self.last_w.get(t)
            if w is not None:
                deps.append((w, 1))
            for r in self.readers.get(t, ()):
                deps.append((r, 2))
        seen = set()
        for p, kind in deps:
            if id(p) in seen:
                continue
            if (not p.is_dma) and p.eng == eng:
                if eng == "pe" or kind == 2 or not self.same_engine_sync:
                    continue
            seen.add(id(p))
            o.deps.append(p)
            if not p.is_dma:
                p.signal = True
        for t in reads:
            self.readers.setdefault(t, []).append(o)
        for t in writes:
            self.last_w[t] = o
            self.readers[t] = []
        if o.is_dma:
            c = self.dma_counts.get(dma_key, 0) + 1
            self.dma_counts[dma_key] = c
            o.count = c
        self.ops[eng].append(o)
        return o

    def barrier(self):
        lasts = []
        dma_last = {}
        for e in ENGS:
            lst = [o for o in self.ops[e] if not o.is_dma and o.fn is not None]
            if lst:
                lasts.append(lst[-1])
            for o in self.ops[e]:
                if o.is_dma:
                    dma_last[o.dma_key] = o
        for e in ENGS:
            o = Op(e, None, None)
            for p in lasts:
                if p.eng != e:
                    o.deps.append(p)
                    p.signal = True
            o.deps.extend(dma_last.values())
            self.ops[e].append(o)
        self.last_w = {}
        self.readers = {}

    def emit(self):
        nc = self.nc
        with contextlib.ExitStack() as st:
            esem = {e: st.enter_context(nc.semaphore("s_" + e)) for e in ENGS}
            dsem = {k: st.enter_context(nc.semaphore("d_%d" % i)) for i, k in enumerate(self.dma_counts)}
            block = st.enter_context(nc.Block())
            for e in ENGS:
                c = 0
                for o in self.ops[e]:
                    if (not o.is_dma) and o.signal:
                        c += 1
                        o.count = c

            def event(p):
                if p.is_dma:
                    return dsem[p.dma_key], p.inc * p.count
                return esem[p.eng], p.count

            def run(e, h):
                waited = {}
                for o in self.ops[e]:
                    for p in o.deps:
                        s, v = event(p)
                        if waited.get(id(s), 0) >= v:
                            continue
                        waited[id(s)] = v
                        h.wait_ge(s, v)
                    if o.fn is None:
                        continue
                    ins = o.fn(h)
                    if o.is_dma:
                        if o.inc == 1:
                            ins.then_inc(dsem[o.dma_key])
                        else:
                            ins.then_inc(dsem[o.dma_key], o.inc)
                    elif o.signal:
                        ins.then_inc(esem[e], 1)

            block.tensor(lambda h: run("pe", h))
            block.scalar(lambda h: run("act", h))
            block.vector(lambda h: run("dve", h))
            block.gpsimd(lambda h: run("pool", h))
            block.sync(lambda h: run("sp", h))


class Arena:
    def __init__(self, nc, nbytes=206 * 1024):
        self.big = nc.alloc_sbuf_tensor("arena", [128, nbytes], mybir.dt.uint8)
        self.off = 0
        self.limit = nbytes

    def alloc(self, shape, dtype):
        esz = 2 if dtype == BF16 else 4
        n = int(np.prod(shape[1:]))
        off = (self.off + 63) // 64 * 64
        assert off + n * esz <= self.limit, ("SBUF arena overflow", off, n * esz, self.limit)
        self.off = off + n * esz
        ap = self.big[0:shape[0], off:off + n * esz].bitcast(dtype)
        if len(shape) == 3:
            ap = ap.rearrange("p (a b) -> p a b", b=shape[2])
        elif len(shape) == 4:
            ap = ap.rearrange("p (a b c) -> p a b c", b=shape[2], c=shape[3])
        return ap

    def mark(self):
        return self.off

    def release(self, m):
        self.off = m


def build_program(layer_types, groups=None, stop_after=None, debug_out=()):
    L = len(layer_types)
    ND = max(1, sum(1 for t in layer_types if t == 0))
    NM = max(1, sum(1 for t in layer_types if t == 1))
    if groups is None:
        groups = [[0, 1], [2, 3], [4, 5], [6, 7]]
    nc = bass.Bass("TRN2", target_bir_lowering=False)
    S = Sched(nc)
    A = Arena(nc)

    def din(name, shape, dt=F32):
        return nc.dram_tensor(name, list(shape), dt, kind="ExternalInput").ap()

    x_in = din("x", [T, D])
    pos_in = din("pos", [1, T], I32)
    flag_in = din("flag", [128, 1])
    cident_in = din("c_ident", [128, 128])
    cperm_in = din("c_perm", [128, 128])
    cmask_in = din("c_mask", [128, 3, 128])
    ccol_in = din("c_col", [128, 2])
    attn_norm = din("attn_norm", [L, D])
    w_in = din("w_in", [L, D, INW])
    mix_a = din("mix_norm_a", [L, 512])
    mix_b = din("mix_norm_b", [L, 512])
    sinks = din("sinks", [1, L * 8])
    w_out = din("w_out", [L, D, D])
    ffn_norm = din("ffn_norm", [L, D])
    if 0 in layer_types:
        dwg = din("dense_w_gate", [ND, D, FF])
        dwu = din("dense_w_up", [ND, D, FF])
        dwd = din("dense_w_down", [ND, FF, D])
    if 1 in layer_types:
        router = din("router", [NM, D, NE])
        mwg = din("moe_w_gate", [NM, NE, D, FF])
        mwu = din("moe_w_up", [NM, NE, D, FF])
        mwd = din("moe_w_down", [NM, NE, FF, D])
    fin_norm = din("final_norm", [1, D])
    out = nc.dram_tensor("out", [T, D], F32, kind="ExternalOutput").ap()

    def dscr(name, shape, dt):
        if name in debug_out:
            return nc.dram_tensor(name, list(shape), dt, kind="ExternalOutput").ap()
        return nc.dram_tensor(name, list(shape), dt).ap()

    xres = dscr("xres", [T, D], F32)
    qT = dscr("qT", [128, 8, T], BF16)
    kT = dscr("kT", [128, 5, TE], BF16)
    vbuf = dscr("vbuf", [TE, 640], BF16)
    oT = dscr("oT", [128, 8, T], BF16)
    cs = dscr("cs", [128, 2, T], F32)
    sendk = dscr("sendk", [5, 128, TH], BF16)
    recvk = dscr("recvk", [5, 256, TH], BF16)
    sendv = dscr("sendv", [2, 1024, 640], BF16)
    recvv = dscr("recvv", [2, 2048, 640], BF16)

    ps = [nc.alloc_psum_tensor("ps%d" % i, [128, 512], F32)[:, :] for i in range(8)]

    ident = A.alloc([128, 128], F32)
    ones_bf = A.alloc([128, 128], BF16)
    perm_bf = A.alloc([128, 128], BF16)
    maskf = A.alloc([128, 3, 128], F32)
    flagc = A.alloc([128, 1], F32)
    ccol = A.alloc([128, 2], F32)
    NG = 24
    gcol = A.alloc([128, L * NG], F32)
    esink = A.alloc([128, L * 8], F32)
    sinkcol = A.alloc([128, L * 4], F32)
    epsc = A.alloc([128, 1], F32)
    negpi = A.alloc([128, 1], F32)

    def dma(q, out_ap, in_ap, reads=(), writes=(), key=None, **kw):
        return S.op(q, lambda e: e.dma_start(out=out_ap, in_=in_ap, **kw), reads=reads, writes=writes, dma_key=key)

    m0 = A.mark()
    permf = A.alloc([128, 128], F32)
    dma("sp", ident, cident_in, writes=["ident"], key="c0")
    dma("sp", permf, cperm_in, writes=["permf"], key="c1")
    dma("sp", maskf, cmask_in, writes=["maskf"], key="c2")
    dma("sp", flagc, flag_in, writes=["flagc"], key="c3")
    dma("sp", ccol, ccol_in, writes=["ccol"], key="c4")
    dma("sp", esink, sinks.partition_broadcast(128), writes=["esink"], key="c5")
    for l in range(L):
        b0 = l * NG
        dma("sp", gcol[:, b0:b0 + 8], attn_norm[l].rearrange("(k p) -> p k", p=128), writes=["gcol%d" % l],
            key="g0", allow_slow_non_contiguous=True)
        dma("sp", gcol[:, b0 + 8:b0 + 16], ffn_norm[l].rearrange("(k p) -> p k", p=128), writes=["gcol%d" % l],
            key="g0", allow_slow_non_contiguous=True)
        dma("sp", gcol[:, b0 + 16:b0 + 20], mix_a[l].rearrange("(k p) -> p k", p=128), writes=["gcol%d" % l],
            key="g0", allow_slow_non_contiguous=True)
        dma("sp", gcol[:, b0 + 20:b0 + 24], mix_b[l].rearrange("(k p) -> p k", p=128), writes=["gcol%d" % l],
            key="g0", allow_slow_non_contiguous=True)
    S.op("pool", lambda e: e.memset(ones_bf, 1.0), writes=["ones"])
    S.op("pool", lambda e: e.memset(epsc, EPS), writes=["epsc"])
    S.op("pool", lambda e: e.memset(negpi, -3.1415925), writes=["negpi"])
    S.op("dve", lambda e: e.tensor_copy(out=perm_bf, in_=permf), reads=["permf"], writes=["perm"])
    S.op("act", lambda e: e.activation(out=esink, in_=esink, func=AF.Exp), reads=["esink"], writes=["esink"])
    for l in range(L):
        for j in range(4):
            c0 = l * 8 + 2 * j
            S.op("dve", lambda e, c0=c0, l=l, j=j: e.tensor_copy(out=sinkcol[0:64, l * 4 + j:l * 4 + j + 1],
                                                              in_=esink[0:64, c0:c0 + 1]),
                 reads=["esink"], writes=["sinkcol"])
            S.op("dve", lambda e, c0=c0, l=l, j=j: e.tensor_copy(out=sinkcol[64:128, l * 4 + j:l * 4 + j + 1],
                                                              in_=esink[64:128, c0 + 1:c0 + 2]),
                 reads=["esink"], writes=["sinkcol"])
    CH = 1024
    posi = A.alloc([128, CH], I32)
    posf = A.alloc([128, CH], F32)
    tt_ = A.alloc([128, CH], F32)
    kf_ = A.alloc([128, CH], F32)
    ki_ = A.alloc([128, CH], I32)
    msk_ = A.alloc([128, CH], F32)
    tab = A.alloc([128, 2, CH], F32)
    INV2PI = float(1.0 / (2.0 * np.pi))
    for ch in range(T // CH):
        dma("sp", posi, pos_in[:, ch * CH:(ch + 1) * CH].partition_broadcast(128), writes=["posi"], key="posi")
        S.op("dve", lambda e: e.tensor_copy(out=posf, in_=posi), reads=["posi"], writes=["posf"])
        S.op("dve", lambda e: e.tensor_scalar(out=posf, in0=posf, scalar1=ccol[:, 0:1], scalar2=INV2PI,
                                              op0=ALU.mult, op1=ALU.mult), reads=["posf", "ccol"], writes=["posf"])
        for which, off in ((1, 0.5), (0, 0.75)):
            S.op("dve", lambda e, off=off: e.tensor_scalar(out=tt_, in0=posf, scalar1=float(off), scalar2=None,
                                                           op0=ALU.add), reads=["posf"], writes=["tt"])
            S.op("dve", lambda e: e.tensor_copy(out=ki_, in_=tt_), reads=["tt"], writes=["ki"])
            S.op("dve", lambda e: e.tensor_copy(out=kf_, in_=ki_), reads=["ki"], writes=["kf"])
            S.op("dve", lambda e: e.tensor_tensor(out=tt_, in0=tt_, in1=kf_, op=ALU.subtract), reads=["tt", "kf"], writes=["tt"])
            S.op("dve", lambda e: e.tensor_scalar(out=msk_, in0=tt_, scalar1=0.0, scalar2=None, op0=ALU.is_lt),
                 reads=["tt"], writes=["msk"])
            S.op("dve", lambda e: e.tensor_tensor(out=tt_, in0=tt_, in1=msk_, op=ALU.add), reads=["tt", "msk"], writes=["tt"])
            S.op("act", lambda e, which=which: e.activation(out=tab[:, which, :], in_=tt_, func=AF.Sin,
                                                            bias=negpi[:, 0:1], scale=6.283185),
                 reads=["tt", "negpi"], writes=["tab%d" % which])
        S.op("dve", lambda e: e.tensor_scalar(out=tab[:, 1, :], in0=tab[:, 1, :], scalar1=ccol[:, 1:2], scalar2=None,
                                              op0=ALU.mult), reads=["tab1", "ccol"], writes=["tab1"])
        dma("sp", cs[:, :, ch * CH:(ch + 1) * CH], tab, reads=["tab0", "tab1"], writes=["cs"], key="cs")
    S.barrier()
    A.release(m0)

    def rms_rstd(ssq_ap, rt_ap, rstd_ap, n, reads, wtok):
        S.op("act", lambda e: e.activation(out=rt_ap, in_=ssq_ap, func=AF.Sqrt, bias=epsc[:, 0:1], scale=1.0 / n),
             reads=list(reads) + ["epsc"], writes=[wtok + "_rt"])
        S.op("dve", lambda e: e.reciprocal(out=rstd_ap, in_=rt_ap), reads=[wtok + "_rt"], writes=[wtok])

    def phase_proj(l):
        mk = A.mark()
        win = A.alloc([128, KC, INW], BF16)
        for kc in range(KC):
            dma("pool", win[:, kc, :], w_in[l][kc * 128:(kc + 1) * 128, :],
                writes=["win%d" % (kc // 2)], key="win%d" % (kc // 2), max_dma_last_dim=4096)
        xt = [A.alloc([128, 4, D], F32) for _ in range(2)]
        xn = A.alloc([128, 4, D], F32)
        junk = A.alloc([128, D], BF16)
        ssq = A.alloc([128, 4], F32)
        rt = A.alloc([128, 4], F32)
        rstd = A.alloc([128, 4], F32)
        hT = [A.alloc([128, KC, 512], BF16) for _ in range(2)]
        cst = [A.alloc([128, 2, 512], F32) for _ in range(2)]
        tb = [A.alloc([128, 512], BF16) for _ in range(2)]
        ra = [A.alloc([128, 512], F32) for _ in range(2)]
        rb = [A.alloc([128, 512], F32) for _ in range(2)]
        qkst = A.alloc([128, 13, 512], BF16)
        vst = A.alloc([128, 4, 640], BF16)
        xsrc = x_in if l == 0 else xres
        g0 = l * NG
        colstarts = [0, 128, 256, 384, 1536, 1664, 1792, 1920, 512, 640, 768, 896, 2048]
        NT = T // 512

        def load(tt):
            b = tt % 2
            dma("sp", xt[b], xsrc[tt * 512:(tt + 1) * 512, :].rearrange("(b p) d -> p b d", p=128),
                writes=["xt%d" % b], key="xt%d" % b)
            dma("sp", cst[b], cs[:, :, tt * 512:(tt + 1) * 512], writes=["cst%d" % b], key="cst%d" % b)

        load(0)
        for tt in range(NT):
            b = tt % 2
            if tt + 1 < NT:
                load(tt + 1)
            X = xt[b]
            H = hT[b]
            for bb in range(4):
                S.op("act", lambda e, bb=bb, X=X: e.activation(out=junk, in_=X[:, bb, :], func=AF.Square,
                                                               accum_out=ssq[:, bb:bb + 1]),
                     reads=["xt%d" % b], writes=["ssq%d" % bb])
            rms_rstd(ssq, rt, rstd, D, ["ssq%d" % i for i in range(4)], "rstd")
            for bb in range(4):
                S.op("dve", lambda e, bb=bb, X=X: e.tensor_scalar(out=xn[:, bb, :], in0=X[:, bb, :],
                                                                  scalar1=rstd[:, bb:bb + 1], scalar2=None, op0=ALU.mult),
                     reads=["xt%d" % b, "rstd"], writes=["xn%d" % bb])
            for kc in range(KC):
                pt = ps[kc % 2]
                for bb in range(4):
                    S.op("pe", lambda e, bb=bb, kc=kc, pt=pt: e.transpose(pt[:, bb * 128:(bb + 1) * 128],
                                                                          xn[:, bb, kc * 128:(kc + 1) * 128], ident),
                         reads=["xn%d" % bb, "ident"], writes=["ps%d" % (kc % 2)])
                eng = "act" if kc % 2 == 0 else "dve"
                if eng == "act":
                    S.op("act", lambda e, kc=kc, pt=pt, H=H: e.activation(out=H[:, kc, :], in_=pt, func=AF.Copy,
                                                                          scale=gcol[:, g0 + kc:g0 + kc + 1]),
                         reads=["ps%d" % (kc % 2), "gcol%d" % l], writes=["hT%d_%d" % (b, kc)])
                else:
                    S.op("dve", lambda e, kc=kc, pt=pt, H=H: e.tensor_scalar(out=H[:, kc, :], in0=pt,
                                                                             scalar1=gcol[:, g0 + kc:g0 + kc + 1],
                                                                             scalar2=None, op0=ALU.mult),
                         reads=["ps%d" % (kc % 2), "gcol%d" % l], writes=["hT%d_%d" % (b, kc)])
            hreads = ["hT%d_%d" % (b, kc) for kc in range(KC)]

            def rope(si, b=b):
                a = si % 2
                cb = cst[b]
                S.op("pe", lambda e, a=a: e.matmul(ps[4 + a][:, :], lhsT=perm_bf, rhs=tb[a], start=True, stop=True),
                     reads=["tb%d" % a, "perm"], writes=["ps%d" % (4 + a)])
                S.op("dve", lambda e, a=a, cb=cb: e.tensor_tensor(out=ra[a], in0=tb[a], in1=cb[:, 0, :], op=ALU.mult),
                     reads=["tb%d" % a, "cst%d" % b], writes=["ra%d" % a])
                S.op("dve", lambda e, a=a, cb=cb: e.tensor_tensor(out=rb[a], in0=ps[4 + a][:, :], in1=cb[:, 1, :], op=ALU.mult),
                     reads=["ps%d" % (4 + a), "cst%d" % b], writes=["rb%d" % a])
                S.op("pool", lambda e, a=a, si=si: e.tensor_tensor(out=qkst[:, si, :], in0=ra[a], in1=rb[a], op=ALU.add),
                     reads=["ra%d" % a, "rb%d" % a], writes=["qkst%d" % si])

            pending = None
            for si, c0 in enumerate(colstarts):
                a = si % 2
                for kc in range(KC):
                    S.op("pe", lambda e, a=a, kc=kc, c0=c0, H=H: e.matmul(ps[2 + a][:, :], lhsT=win[:, kc, c0:c0 + 128],
                                                                          rhs=H[:, kc, :], start=(kc == 0), stop=(kc == KC - 1)),
                         reads=[hreads[kc], "win%d" % (kc // 2)], writes=["ps%d" % (2 + a)])
                S.op("act", lambda e, a=a: e.copy(out=tb[a], in_=ps[2 + a][:, :]), reads=["ps%d" % (2 + a)], writes=["tb%d" % a])
                if pending is not None:
                    rope(pending)
                pending = si
            rope(pending)
            for bb in range(4):
                pv = ps[6 + bb % 2]
                for kc in range(KC):
                    S.op("pe", lambda e, bb=bb, kc=kc, pv=pv, H=H: e.matmul(pv[:, :], lhsT=H[:, kc, bb * 128:(bb + 1) * 128],
                                                                            rhs=win[:, kc, 1024:1536], start=(kc == 0), stop=(kc == KC - 1)),
                         reads=[hreads[kc], "win%d" % (kc // 2)], writes=["ps%d" % (6 + bb % 2)])
                S.op("act", lambda e, bb=bb, pv=pv: e.copy(out=vst[:, bb, 0:512], in_=pv[:, :]),
                     reads=["ps%d" % (6 + bb % 2)], writes=["vst%d" % bb])
                pw = ps[4 + bb % 2]
                for kc in range(KC):
                    S.op("pe", lambda e, bb=bb, kc=kc, pw=pw, H=H: e.matmul(pw[:, 0:128], lhsT=H[:, kc, bb * 128:(bb + 1) * 128],
                                                                            rhs=win[:, kc, 2176:2304], start=(kc == 0), stop=(kc == KC - 1)),
                         reads=[hreads[kc], "win%d" % (kc // 2)], writes=["ps%d" % (4 + bb % 2)])
                S.op("dve", lambda e, bb=bb, pw=pw: e.tensor_copy(out=vst[:, bb, 512:640], in_=pw[:, 0:128]),
                     reads=["ps%d" % (4 + bb % 2)], writes=["vst%d" % bb])
            dma("sp", qT[:, :, tt * 512:(tt + 1) * 512], qkst[:, 0:8, :], reads=["qkst%d" % i for i in range(8)],
                writes=["qT"], key="qTst")
            dma("sp", kT[:, :, TH + tt * 512:TH + (tt + 1) * 512], qkst[:, 8:13, :],
                reads=["qkst%d" % i for i in range(8, 13)], writes=["kT"], key="kTst")
            dma("sp", vbuf[TH + tt * 512:TH + (tt + 1) * 512, :].rearrange("(b p) c -> p b c", p=128), vst,
                reads=["vst%d" % i for i in range(4)], writes=["vbuf"], key="vst")
        S.barrier()
        A.release(mk)

    def phase_exchange():
        for c in range(5):
            dma("sp", sendk[c], kT[:, c, T:TE], writes=["sendk%d" % c], key="ex0")
        for u in range(2):
            dma("sp", sendv[u], vbuf[T + u * 1024:T + (u + 1) * 1024, :], writes=["sendv%d" % u], key="ex1")
        for c in range(5):
            S.op("pool", lambda e, c=c: e.collective_compute("AllGather", ALU.bypass, replica_groups=groups,
                                                             ins=[sendk[c].opt()], outs=[recvk[c].opt()]),
                 reads=["sendk%d" % c], writes=["recvk%d" % c], dma_key="cc", inc=1)
        for u in range(2):
            S.op("pool", lambda e, u=u: e.collective_compute("AllGather", ALU.bypass, replica_groups=groups,
                                                             ins=[sendv[u].opt()], outs=[recvv[u].opt()]),
                 reads=["sendv%d" % u], writes=["recvv%d" % u], dma_key="cc", inc=1)
        for c in range(5):
            dma("sp", kT[:, c, 0:TH], recvk[c, 0:128, :], reads=["recvk%d" % c], writes=["kT"], key="ex2")
        for u in range(2):
            dma("sp", vbuf[u * 1024:(u + 1) * 1024, :], recvv[u, 0:1024, :], reads=["recvv%d" % u], writes=["vbuf"], key="ex3")
        S.barrier()

    def phase_attn(l):
        mk = A.mark()
        kct = [A.alloc([128, TE], BF16) for _ in range(2)]
        qct = [A.alloc([128, T], BF16) for _ in range(2)]
        vA = {d: A.alloc([128, TE // 128, 128], BF16) for d in DIL}
        vB = A.alloc([128, TE // 128, 64], BF16)
        accn = A.alloc([128, T], F32)
        accd = A.alloc([128, T], F32)
        rden = A.alloc([128, T], F32)
        oc = A.alloc([128, T], BF16)
        P = [A.alloc([128, 2, 256], BF16) for _ in range(2)]
        M4 = {}
        for nm, prev_idx in (("A", 1), ("B", 2)):
            for halo in (0, 1):
                mt = A.alloc([128, 2, 256], BF16)
                M4[(nm, halo)] = mt
                for h in range(2):
                    if halo:
                        S.op("dve", lambda e, mt=mt, h=h, prev_idx=prev_idx: e.tensor_scalar(
                            out=mt[:, h, 0:128], in0=maskf[:, prev_idx, :], scalar1=flagc[:, 0:1], scalar2=None, op0=ALU.mult),
                             reads=["maskf", "flagc"], writes=["M4"])
                    else:
                        S.op("dve", lambda e, mt=mt, h=h, prev_idx=prev_idx: e.tensor_copy(out=mt[:, h, 0:128], in_=maskf[:, prev_idx, :]),
                             reads=["maskf"], writes=["M4"])
                    S.op("dve", lambda e, mt=mt, h=h: e.tensor_copy(out=mt[:, h, 128:256], in_=maskf[:, 0, :]),
                         reads=["maskf"], writes=["M4"])
        ps_s = [ps[0], ps[1]]

        def load_chunk(c):
            b = c % 2
            if c < 4:
                dma("sp", kct[b], kT[:, c, :], writes=["kct%d" % b], key="kct%d" % b)
            else:
                g = (c - 4) // 2
                dma("sp", kct[b][0:64, :], kT[g * 64:(g + 1) * 64, 4, :], writes=["kct%d" % b], key="kct%d" % b)
                dma("sp", kct[b][64:128, :], kT[g * 64:(g + 1) * 64, 4, :], writes=["kct%d" % b], key="kct%d" % b)
            dma("sp", qct[b], qT[:, c, :], writes=["qct%d" % b], key="qct%d" % b)

        def load_v(c, d):
            if c < 4:
                cols = slice(c * 128, (c + 1) * 128)
                if d == 1:
                    for u in range(TE // 2048):
                        src = vbuf[u * 2048:(u + 1) * 2048, cols].rearrange("(s i) c -> i s c", i=128)
                        dma("sp", vA[d][:, u * 16:(u + 1) * 16, :], src, writes=["vA%d" % d], key="vA%d" % d)
                else:
                    seg = 128 * d
                    for sg_ in range(TE // seg):
                        src = vbuf[sg_ * seg:(sg_ + 1) * seg, cols].rearrange("(i r) c -> i r c", r=d)
                        dma("sp", vA[d][:, sg_ * d:(sg_ + 1) * d, :], src, writes=["vA%d_%d" % (d, sg_ % 2)], key="vA%d_%d" % (d, sg_ % 2))
            else:
                g = (c - 4) // 2
                for u in range(TE // 2048):
                    src = vbuf[u * 2048:(u + 1) * 2048, 512 + g * 64:512 + (g + 1) * 64].rearrange("(s i) c -> i s c", i=128)
                    dma("sp", vB[:, u * 16:(u + 1) * 16, :], src, writes=["vB"], key="vB")

        DBG_NCH = int(os.environ.get("ATT_NCH", "8"))
        DBG_BR = tuple(int(v) for v in os.environ.get("ATT_BR", "1,4,16").split(","))
        DBG_NU = int(os.environ.get("ATT_NU", "9999"))
        load_chunk(0)
        for d in DIL:
            load_v(0, d)
        load_v(4, 1)
        for c in range(DBG_NCH):
            b = c % 2
            grpA = c < 4
            branches = tuple(d for d in DIL if d in DBG_BR) if grpA else (1,)
            KCt, QCt = kct[b], qct[b]
            first_branch = True
            for d in branches:
                nseg = T // (128 * d)
                Sh = TH // (128 * d)
                Qr = QCt.rearrange("p (s i r) -> p s r i", i=128, r=d)
                Kr = KCt.rearrange("p (s i r) -> p s r i", i=128, r=d)
                accn_r = accn.rearrange("p (s i r) -> p s r i", i=128, r=d)
                accd_r = accd.rearrange("p (s i r) -> p s r i", i=128, r=d)
                if grpA:
                    Vt = vA[d]
                    vtok = ["vA%d" % d] if d == 1 else ["vA%d_0" % d, "vA%d_1" % d]
                else:
                    Vt = vB
                    vtok = ["vB"]
                if d == 1:
                    groups_ = [[(s, 0) for s in range(g4 * 4, g4 * 4 + 4)] for g4 in range(nseg // 4)]
                else:
                    groups_ = [[(s, r) for r in range(r0, r0 + 4)] for s in range(nseg) for r0 in range(0, d, 4)]
                units = []
                for gi, g in enumerate(groups_):
                    for j, (s, r) in enumerate(g):
                        units.append((gi, j, s, r))

                def scores(n, u):
                    gi, j, s, r = u
                    alt = n % 2
                    Qv = Qr[:, s, r, :]
                    Kp = Kr[:, s + Sh - 1, r, :]
                    Kc_ = Kr[:, s + Sh, r, :]
                    for h in range(2):
                        lo, hi = h * 64, (h + 1) * 64
                        S.op("pe", lambda e, h=h, lo=lo, hi=hi, Kp=Kp, Qv=Qv, alt=alt: e.matmul(
                            ps_s[h][:, alt * 256:alt * 256 + 128], lhsT=Kp[lo:hi, :], rhs=Qv[lo:hi, :], start=True, stop=True),
                             reads=["kct%d" % b, "qct%d" % b], writes=["pss%d" % alt])
                        S.op("pe", lambda e, h=h, lo=lo, hi=hi, Kc_=Kc_, Qv=Qv, alt=alt: e.matmul(
                            ps_s[h][:, alt * 256 + 128:alt * 256 + 256], lhsT=Kc_[lo:hi, :], rhs=Qv[lo:hi, :], start=True, stop=True),
                             reads=["kct%d" % b, "qct%d" % b], writes=["pss%d" % alt])

                def rest(n, u):
                    gi, j, s, r = u
                    alt = n % 2
                    Pt = P[alt]
                    pn = ps[2 + gi % 2]
                    pd = ps[4 + gi % 2]
                    for h in range(2):
                        S.op("act", lambda e, h=h, Pt=Pt, alt=alt: e.activation(out=Pt[:, h, :], in_=ps_s[h][:, alt * 256:alt * 256 + 256],
                                                                               func=AF.Exp, scale=0.125),
                             reads=["pss%d" % alt], writes=["P%d" % alt])
                    mt = M4[("A" if grpA else "B", 1 if s == 0 else 0)]
                    S.op("pool", lambda e, Pt=Pt, mt=mt: e.tensor_tensor(out=Pt, in0=Pt, in1=mt, op=ALU.mult),
                         reads=["P%d" % alt, "M4"], writes=["P%d" % alt])
                    nprev = (s + Sh - 1) * d + r
                    ncur = (s + Sh) * d + r
                    for h in range(2):
                        lo, hi = h * 64, (h + 1) * 64
                        if grpA:
                            Vp, Vc = Vt[:, nprev, lo:hi], Vt[:, ncur, lo:hi]
                        else:
                            Vp, Vc = Vt[:, nprev, :], Vt[:, ncur, :]
                        cols = slice(j * 128, (j + 1) * 128)
                        S.op("pe", lambda e, Vp=Vp, Pt=Pt, h=h, lo=lo, hi=hi, cols=cols, pn=pn: e.matmul(
                            pn[lo:hi, cols], lhsT=Vp, rhs=Pt[:, h, 0:128], start=True, stop=False),
                             reads=["P%d" % alt] + vtok, writes=["ps%d" % (2 + gi % 2)])
                        S.op("pe", lambda e, Vc=Vc, Pt=Pt, h=h, lo=lo, hi=hi, cols=cols, pn=pn: e.matmul(
                            pn[lo:hi, cols], lhsT=Vc, rhs=Pt[:, h, 128:256], start=False, stop=True),
                             reads=["P%d" % alt] + vtok, writes=["ps%d" % (2 + gi % 2)])
                        S.op("pe", lambda e, Pt=Pt, h=h, lo=lo, hi=hi, cols=cols, pd=pd: e.matmul(
                            pd[lo:hi, cols], lhsT=ones_bf[:, 0:64], rhs=Pt[:, h, 0:128], start=True, stop=False),
                             reads=["P%d" % alt, "ones"], writes=["ps%d" % (4 + gi % 2)])
                        S.op("pe", lambda e, Pt=Pt, h=h, lo=lo, hi=hi, cols=cols, pd=pd: e.matmul(
                            pd[lo:hi, cols], lhsT=ones_bf[:, 0:64], rhs=Pt[:, h, 128:256], start=False, stop=True),
                             reads=["P%d" % alt, "ones"], writes=["ps%d" % (4 + gi % 2)])
                    if j == 3 and not os.environ.get("ATT_NOACC"):
                        g = groups_[gi]
                        s0, r0 = g[0]
                        if d == 1:
                            an = accn[:, s0 * 128:(s0 + 4) * 128]
                            ad = accd[:, s0 * 128:(s0 + 4) * 128]
                            pnv, pdv = pn[:, :], pd[:, :]
                        else:
                            an = accn_r[:, s0, r0:r0 + 4, :]
                            ad = accd_r[:, s0, r0:r0 + 4, :]
                            pnv = pn[:, :].rearrange("p (r i) -> p r i", i=128)
                            pdv = pd[:, :].rearrange("p (r i) -> p r i", i=128)
                        if first_branch:
                            S.op("dve", lambda e, an=an, pnv=pnv: e.tensor_copy(out=an, in_=pnv),
                                 reads=["ps%d" % (2 + gi % 2)], writes=["accn"])
                            S.op("dve", lambda e, ad=ad, pdv=pdv: e.tensor_copy(out=ad, in_=pdv),
                                 reads=["ps%d" % (4 + gi % 2)], writes=["accd"])
                        else:
                            S.op("dve", lambda e, an=an, pnv=pnv: e.tensor_tensor(out=an, in0=an, in1=pnv, op=ALU.add),
                                 reads=["ps%d" % (2 + gi % 2), "accn"], writes=["accn"])
                            S.op("dve", lambda e, ad=ad, pdv=pdv: e.tensor_tensor(out=ad, in0=ad, in1=pdv, op=ALU.add),
                                 reads=["ps%d" % (4 + gi % 2), "accd"], writes=["accd"])

                units = units[:DBG_NU]
                NOPIPE = not os.environ.get("ATT_PIPE")
                if units and not NOPIPE:
                    scores(0, units[0])
                for n, u in enumerate(units):
                    if NOPIPE:
                        scores(n, u)
                    elif n + 1 < len(units):
                        scores(n + 1, units[n + 1])
                    rest(n, u)
                first_branch = False
                if c + 1 < 8 and d == branches[0]:
                    load_chunk(c + 1)
                if c + 1 < 4:
                    load_v(c + 1, d)
            if c == 5:
                load_v(6, 1)
            if not grpA:
                j4 = l * 4 + (c - 4)
                S.op("dve", lambda e, j4=j4: e.tensor_scalar(out=accd, in0=accd, scalar1=sinkcol[:, j4:j4 + 1], scalar2=None, op0=ALU.add),
                     reads=["accd", "sinkcol"], writes=["accd"])
            S.op("dve", lambda e: e.reciprocal(out=rden, in_=accd), reads=["accd"], writes=["rden"])
            S.op("pool", lambda e: e.tensor_tensor(out=oc, in0=accn, in1=rden, op=ALU.mult), reads=["accn", "rden"], writes=["oc"])
            dma("sp", oT[:, c, :], oc, reads=["oc"], writes=["oT"], key="oc")
        S.barrier()
        A.release(mk)

    def phase_ffn(l, li_dense, li_moe, last):
        moe = layer_types[l] == 1
        mk = A.mark()
        g0 = l * NG
        yacc = A.alloc([128, 8, D], F32)
        hTf = A.alloc([128, KC, 1024], BF16)
        gate = A.alloc([128, 8, NE], F32)
        wd = [A.alloc([128, NJ // 2, D], BF16) for _ in range(2)]
        wg = [A.alloc([128, KC, 256], BF16) for _ in range(2)]
        wu = [A.alloc([128, KC, 256], BF16) for _ in range(2)]
        sg = [A.alloc([128, 512], F32) for _ in range(2)]
        rt32 = A.alloc([128, KC, NE], F32)
        mU = A.mark()
        aT = A.alloc([128, NJ, 1024], BF16)
        A.release(mU)
        wout = A.alloc([128, KC, D], BF16)
        ot = A.alloc([128, 8, 512], BF16)
        mixT = A.alloc([128, 8, 512], BF16)
        rsa = A.alloc([128, 2, 512], F32)
        rsb = A.alloc([128, 2, 512], F32)
        xn = A.alloc([128, D], F32)
        junk = A.alloc([128, D], BF16)
        h32 = A.alloc([128, KC, 512], F32)
        sm = A.alloc([128, 16], F32)
        gsc = A.alloc([128, 48], F32)
        gfin = A.alloc([128, D], F32) if last else None
        NEXP = NE if moe else 1
        if moe:
            dma("sp", rt32, router[li_moe].rearrange("(k p) e -> p k e", p=128), writes=["rt32"], key="rt32")
        if last:
            dma("sp", gfin, fin_norm.partition_broadcast(128), writes=["gfin"], key="gfin")

        def wsrc(e):
            if moe:
                return mwg[li_moe, e], mwu[li_moe, e], mwd[li_moe, e]
            return dwg[li_dense], dwu[li_dense], dwd[li_dense]

        def load_gu(e, g):
            b = g % 2
            G_, U_, _ = wsrc(e)
            dma("pool", wg[b], G_[:, g * 256:(g + 1) * 256].rearrange("(k p) c -> p k c", p=128), writes=["wg%d" % b], key="wg%d" % b)
            dma("pool", wu[b], U_[:, g * 256:(g + 1) * 256].rearrange("(k p) c -> p k c", p=128), writes=["wu%d" % b], key="wu%d" % b)

        def load_wd(e, half):
            _, _, D_ = wsrc(e)
            for q in range(2):
                j0 = half * 14 + q * 7
                dma("pool", wd[half][:, q * 7:(q + 1) * 7, :], D_[j0 * 128:(j0 + 7) * 128, :].rearrange("(j p) c -> p j c", p=128),
                    writes=["wd%d" % half], key="wd%d" % half)

        for t4 in range(T // 1024):
            load_gu(0, 0)
            load_gu(0, 1)
            load_wd(0, 0)
            load_wd(0, 1)
            dma("pool", wout[:, 0:4, :], w_out[l][0:512, :].rearrange("(k p) c -> p k c", p=128), writes=["wout0"], key="wout0")
            dma("pool", wout[:, 4:8, :], w_out[l][512:1024, :].rearrange("(k p) c -> p k c", p=128), writes=["wout1"], key="wout1")
            dma("sp", yacc, (x_in if l == 0 else xres)[t4 * 1024:(t4 + 1) * 1024, :].rearrange("(b p) d -> p b d", p=128),
                writes=["yacc%d" % i for i in range(8)], key="yacc")
            for st in range(2):
                tok0 = t4 * 1024 + st * 512
                dma("sp", ot, oT[:, :, tok0:tok0 + 512], writes=["ot"], key="ot")
                S.op("act", lambda e: e.activation(out=mixT, in_=ot, func=AF.Square), reads=["ot"],
                     writes=["mixT"] + ["mixT%d" % c for c in range(8)])
                for grp in range(2):
                    for c in range(4):
                        S.op("pe", lambda e, grp=grp, c=c: e.matmul(ps[grp][:, :], lhsT=ones_bf, rhs=mixT[:, grp * 4 + c, :],
                                                                    start=(c == 0), stop=(c == 3)),
                             reads=["mixT", "ones"], writes=["ps%d" % grp])
                    rsx = rsa if grp == 0 else rsb
                    S.op("act", lambda e, grp=grp, rsx=rsx: e.activation(out=rsx[:, 0, :], in_=ps[grp][:, :], func=AF.Sqrt,
                                                                         bias=epsc[:, 0:1], scale=1.0 / 512),
                         reads=["ps%d" % grp, "epsc"], writes=["rs%d_0" % grp])
                    S.op("dve", lambda e, rsx=rsx: e.reciprocal(out=rsx[:, 1, :], in_=rsx[:, 0, :]),
                         reads=["rs%d_0" % grp], writes=["rs%d_1" % grp])
                for c in range(8):
                    rsx = rsa if c < 4 else rsb
                    gc = g0 + 16 + c
                    S.op("dve", lambda e, c=c, rsx=rsx, gc=gc: e.scalar_tensor_tensor(
                        out=mixT[:, c, :], in0=ot[:, c, :], scalar=gcol[:, gc:gc + 1], in1=rsx[:, 1, :], op0=ALU.mult, op1=ALU.mult),
                         reads=["ot", "rs%d_1" % (0 if c < 4 else 1), "gcol%d" % l], writes=["mixT%d" % c, "mixT"])
                mreads = ["mixT%d" % c for c in range(8)]
                for bb in range(4):
                    blk = st * 4 + bb
                    for half in range(2):
                        pb = ps[2 + half]
                        for c in range(8):
                            S.op("pe", lambda e, bb=bb, half=half, c=c, pb=pb: e.matmul(
                                pb[:, :], lhsT=mixT[:, c, bb * 128:(bb + 1) * 128], rhs=wout[:, c, half * 512:(half + 1) * 512],
                                start=(c == 0), stop=(c == 7)),
                                 reads=[mreads[c], "wout%d" % (c // 4)], writes=["ps%d" % (2 + half)])
                        S.op("dve", lambda e, blk=blk, half=half, pb=pb: e.tensor_tensor(
                            out=yacc[:, blk, half * 512:(half + 1) * 512], in0=yacc[:, blk, half * 512:(half + 1) * 512],
                            in1=pb[:, :], op=ALU.add),
                             reads=["ps%d" % (2 + half), "yacc%d" % blk], writes=["yacc%d" % blk])
                    S.op("act", lambda e, blk=blk: e.activation(out=junk, in_=yacc[:, blk, :], func=AF.Square, accum_out=sm[:, 0:1]),
                         reads=["yacc%d" % blk], writes=["sm0"])
                    S.op("act", lambda e: e.activation(out=sm[:, 1:2], in_=sm[:, 0:1], func=AF.Sqrt, bias=epsc[:, 0:1], scale=1.0 / D),
                         reads=["sm0", "epsc"], writes=["sm1"])
                    S.op("dve", lambda e: e.reciprocal(out=sm[:, 2:3], in_=sm[:, 1:2]), reads=["sm1"], writes=["sm2"])
                    S.op("dve", lambda e, blk=blk: e.tensor_scalar(out=xn, in0=yacc[:, blk, :], scalar1=sm[:, 2:3], scalar2=None, op0=ALU.mult),
                         reads=["yacc%d" % blk, "sm2"], writes=["xn"])
                    for kc in range(KC):
                        pt = ps[4 + (kc // 4)]
                        S.op("pe", lambda e, kc=kc, pt=pt: e.transpose(pt[:, (kc % 4) * 128:(kc % 4 + 1) * 128],
                                                                       xn[:, kc * 128:(kc + 1) * 128], ident),
                             reads=["xn", "ident"], writes=["ps%d" % (4 + kc // 4)])
                    for kc in range(KC):
                        pt = ps[4 + (kc // 4)]
                        S.op("act", lambda e, kc=kc, pt=pt, bb=bb: e.activation(
                            out=h32[:, kc, bb * 128:(bb + 1) * 128], in_=pt[:, (kc % 4) * 128:(kc % 4 + 1) * 128], func=AF.Copy,
                            scale=gcol[:, g0 + 8 + kc:g0 + 8 + kc + 1]),
                             reads=["ps%d" % (4 + kc // 4), "gcol%d" % l], writes=["h32_%d" % bb])
                    if moe:
                        for kc in range(KC):
                            S.op("pe", lambda e, kc=kc, bb=bb: e.matmul(ps[6][:, bb * 8:(bb + 1) * 8], lhsT=h32[:, kc, bb * 128:(bb + 1) * 128],
                                                                        rhs=rt32[:, kc, :], start=(kc == 0), stop=(kc == KC - 1)),
                                 reads=["h32_%d" % bb, "rt32"], writes=["ps6"])
                S.op("pool", lambda e, st=st: e.tensor_copy(out=hTf[:, :, st * 512:(st + 1) * 512], in_=h32),
                     reads=["h32_%d" % i for i in range(4)], writes=["hTf%d" % st])
                if moe:
                    lg = sm
                    for bb in range(4):
                        blk = st * 4 + bb
                        Lg = ps[6][:, bb * 8:(bb + 1) * 8]
                        gb = gate[:, blk, :]
                        S.op("dve", lambda e, Lg=Lg: e.tensor_copy(out=gsc[:, 0:8], in_=Lg), reads=["ps6"], writes=["lg"])
                        S.op("dve", lambda e: e.tensor_reduce(out=sm[:, 4:5], in_=gsc[:, 0:8], axis=AX.X, op=ALU.max),
                             reads=["lg"], writes=["m1"])
                        S.op("dve", lambda e: e.tensor_scalar(out=gsc[:, 8:16], in0=gsc[:, 0:8], scalar1=sm[:, 4:5], scalar2=None,
                                                              op0=ALU.is_equal), reads=["lg", "m1"], writes=["eq"])
                        S.op("dve", lambda e: e.scalar_tensor_tensor(out=gsc[:, 16:24], in0=gsc[:, 8:16], scalar=-1e30,
                                                                     in1=gsc[:, 0:8], op0=ALU.mult, op1=ALU.add),
                             reads=["eq", "lg"], writes=["lg2"])
                        S.op("dve", lambda e: e.tensor_reduce(out=sm[:, 5:6], in_=gsc[:, 16:24], axis=AX.X, op=ALU.max),
                             reads=["lg2"], writes=["m2"])
                        S.op("dve", lambda e: e.tensor_scalar(out=gsc[:, 24:32], in0=gsc[:, 0:8], scalar1=sm[:, 5:6], scalar2=None,
                                                              op0=ALU.is_ge), reads=["lg", "m2"], writes=["sel"])
                        S.op("dve", lambda e: e.tensor_scalar(out=sm[:, 6:7], in0=sm[:, 4:5], scalar1=-1.0, scalar2=None, op0=ALU.mult),
                             reads=["m1"], writes=["nm1"])
                        S.op("act", lambda e: e.activation(out=gsc[:, 32:40], in_=gsc[:, 0:8], func=AF.Exp, bias=sm[:, 6:7]),
                             reads=["lg", "nm1"], writes=["ex"])
                        S.op("dve", lambda e: e.tensor_tensor(out=gsc[:, 40:48], in0=gsc[:, 32:40], in1=gsc[:, 24:32], op=ALU.mult),
                             reads=["ex", "sel"], writes=["exs"])
                        S.op("dve", lambda e: e.tensor_reduce(out=sm[:, 7:8], in_=gsc[:, 40:48], axis=AX.X, op=ALU.add),
                             reads=["exs"], writes=["se"])
                        S.op("dve", lambda e: e.reciprocal(out=sm[:, 8:9], in_=sm[:, 7:8]), reads=["se"], writes=["rse"])
                        S.op("dve", lambda e, gb=gb: e.tensor_scalar(out=gb, in0=gsc[:, 40:48], scalar1=sm[:, 8:9], scalar2=None, op0=ALU.mult),
                             reads=["exs", "rse"], writes=["gate%d" % blk])
            S.barrier()
            hreads = ["hTf0", "hTf1"]
            for e_ in range(NEXP):
                for g in range(14):
                    b = g % 2
                    for jj in range(2):
                        j = g * 2 + jj
                        for th in range(2):
                            a = (jj * 2 + th) % 2
                            pG, pU = ps[a], ps[2 + a]
                            for kc in range(KC):
                                S.op("pe", lambda e, kc=kc, b=b, jj=jj, th=th, pG=pG: e.matmul(
                                    pG[:, :], lhsT=wg[b][:, kc, jj * 128:(jj + 1) * 128], rhs=hTf[:, kc, th * 512:(th + 1) * 512],
                                    start=(kc == 0), stop=(kc == KC - 1)),
                                     reads=["wg%d" % b, hreads[th]], writes=["ps%d" % a])
                            for kc in range(KC):
                                S.op("pe", lambda e, kc=kc, b=b, jj=jj, th=th, pU=pU: e.matmul(
                                    pU[:, :], lhsT=wu[b][:, kc, jj * 128:(jj + 1) * 128], rhs=hTf[:, kc, th * 512:(th + 1) * 512],
                                    start=(kc == 0), stop=(kc == KC - 1)),
                                     reads=["wu%d" % b, hreads[th]], writes=["ps%d" % (2 + a)])
                            S.op("act", lambda e, a=a, pG=pG: e.activation(out=sg[a], in_=pG[:, :], func=AF.Silu),
                                 reads=["ps%d" % a], writes=["sg%d" % a])
                            S.op("dve", lambda e, a=a, pU=pU, j=j, th=th: e.tensor_tensor(
                                out=aT[:, j, th * 512:(th + 1) * 512], in0=sg[a], in1=pU[:, :], op=ALU.mult),
                                 reads=["sg%d" % a, "ps%d" % (2 + a)], writes=["aT%d" % (j // 14)])
                    if g + 2 < 14:
                        load_gu(e_, g + 2)
                    elif e_ + 1 < NEXP:
                        load_gu(e_ + 1, g + 2 - 14)
                for half in range(2):
                    for blk in range(8):
                        for ch in range(2):
                            pb = ps[4 + (blk * 2 + ch) % 4]
                            ptok = "ps%d" % (4 + (blk * 2 + ch) % 4)
                            for jx in range(14):
                                j = half * 14 + jx
                                S.op("pe", lambda e, blk=blk, ch=ch, jx=jx, j=j, pb=pb, half=half: e.matmul(
                                    pb[:, :], lhsT=aT[:, j, blk * 128:(blk + 1) * 128], rhs=wd[half][:, jx, ch * 512:(ch + 1) * 512],
                                    start=(jx == 0), stop=(jx == 13)),
                                     reads=["aT%d" % half, "wd%d" % half], writes=[ptok])
                            ys = yacc[:, blk, ch * 512:(ch + 1) * 512]
                            if moe:
                                S.op("dve", lambda e, pb=pb, ys=ys, blk=blk, e_=e_: e.scalar_tensor_tensor(
                                    out=ys, in0=pb[:, :], scalar=gate[:, blk, e_:e_ + 1], in1=ys, op0=ALU.mult, op1=ALU.add),
                                     reads=[ptok, "yacc%d" % blk, "gate%d" % blk], writes=["yacc%d" % blk])
                            else:
                                S.op("dve", lambda e, pb=pb, ys=ys: e.tensor_tensor(out=ys, in0=ys, in1=pb[:, :], op=ALU.add),
                                     reads=[ptok, "yacc%d" % blk], writes=["yacc%d" % blk])
                    if e_ + 1 < NEXP:
                        load_wd(e_ + 1, half)
            if not last:
                dma("sp", xres[t4 * 1024:(t4 + 1) * 1024, :].rearrange("(b p) d -> p b d", p=128), yacc,
                    reads=["yacc%d" % i for i in range(8)], writes=["xres"], key="xst")
            else:
                S.barrier()
                for blk in range(8):
                    S.op("act", lambda e, blk=blk: e.activation(out=junk, in_=yacc[:, blk, :], func=AF.Square, accum_out=sm[:, 0:1]),
                         reads=["yacc%d" % blk], writes=["sm0"])
                    S.op("act", lambda e: e.activation(out=sm[:, 1:2], in_=sm[:, 0:1], func=AF.Sqrt, bias=epsc[:, 0:1], scale=1.0 / D),
                         reads=["sm0", "epsc"], writes=["sm1"])
                    S.op("dve", lambda e: e.reciprocal(out=sm[:, 2:3], in_=sm[:, 1:2]), reads=["sm1"], writes=["sm2"])
                    S.op("dve", lambda e, blk=blk: e.scalar_tensor_tensor(out=yacc[:, blk, :], in0=yacc[:, blk, :], scalar=sm[:, 2:3],
                                                                         in1=gfin, op0=ALU.mult, op1=ALU.mult),
                         reads=["yacc%d" % blk, "sm2", "gfin"], writes=["yacc%d" % blk])
                dma("sp", out[t4 * 1024:(t4 + 1) * 1024, :].rearrange("(b p) d -> p b d", p=128), yacc,
                    reads=["yacc%d" % i for i in range(8)], writes=["out"], key="xst")
            S.barrier()
        A.release(mk)

    nd = nm = 0
    for l, lt in enumerate(layer_types):
        if stop_after == "setup":
            break
        if not os.environ.get("SKIP_PROJ"):
            phase_proj(l)
            if stop_after == "proj":
                break
            phase_exchange()
            if stop_after == "exch":
                break
        phase_attn(l)
        if stop_after == "attn":
            break
        phase_ffn(l, nd, nm, l == L - 1)
        if lt == 0:
            nd += 1
        else:
            nm += 1
    S.emit()
    return nc


def _consts():
    k = np.arange(128)[:, None]
    q = np.arange(128)[None, :]
    ident = (k == q).astype(np.float32)
    src = (q // 64) * 64 + ((q % 64) + 32) % 64
    perm = (k == src).astype(np.float32)
    mask = np.stack([(q >= k), (k >= q), (k > q)], axis=1).astype(np.float32)
    p = np.arange(128)
    invf = (10000.0 ** (-(p % 32).astype(np.float64) / 32.0)).astype(np.float32)
    sgn = np.where((p % 64) < 32, -1.0, 1.0).astype(np.float32)
    col = np.stack([invf, sgn], axis=1).astype(np.float32)
    return ident, perm, np.ascontiguousarray(mask), np.ascontiguousarray(col)


_CACHE = {}


def run_layers(inputs, layer_types, debug_out=(), stop_after=None):
    L = len(layer_types)
    key = (tuple(layer_types), tuple(debug_out), stop_after)
    if key not in _CACHE:
        _CACHE[key] = build_program(list(layer_types), debug_out=debug_out, stop_after=stop_after)
    nc = _CACHE[key]
    ident, perm, mask, col = _consts()
    f32 = lambda a: np.ascontiguousarray(np.asarray(a, dtype=np.float32))
    x = f32(inputs["x"])
    pos = np.ascontiguousarray(np.asarray(inputs["positions"], dtype=np.int32))
    shared = {
        "c_ident": ident, "c_perm": perm, "c_mask": mask, "c_col": col,
        "attn_norm": f32(inputs["attn_norm"])[:L], "w_in": f32(inputs["w_in"])[:L],
        "mix_norm_a": f32(inputs["mix_norm_a"])[:L], "mix_norm_b": f32(inputs["mix_norm_b"])[:L],
        "sinks": f32(inputs["sinks"])[:L].reshape(1, L * 8), "w_out": f32(inputs["w_out"])[:L],
        "ffn_norm": f32(inputs["ffn_norm"])[:L], "final_norm": f32(inputs["final_norm"]).reshape(1, D),
    }
    nd = sum(1 for t in layer_types if t == 0)
    nm = sum(1 for t in layer_types if t == 1)
    if nd:
        shared["dense_w_gate"] = f32(inputs["dense_w_gate"])[:nd]
        shared["dense_w_up"] = f32(inputs["dense_w_up"])[:nd]
        shared["dense_w_down"] = f32(inputs["dense_w_down"])[:nd]
    if nm:
        shared["router"] = f32(inputs["router"])[:nm]
        shared["moe_w_gate"] = f32(inputs["moe_w_gate"])[:nm]
        shared["moe_w_up"] = f32(inputs["moe_w_up"])[:nm]
        shared["moe_w_down"] = f32(inputs["moe_w_down"])[:nm]
    in_maps = []
    for c in range(NCORES):
        b, h = c // 2, c % 2
        m = dict(shared)
        m["x"] = np.ascontiguousarray(x[b, h * T:(h + 1) * T, :])
        m["pos"] = np.ascontiguousarray(pos[h * T:(h + 1) * T][None, :])
        m["flag"] = np.full((128, 1), float(h), dtype=np.float32)
        in_maps.append(m)
    res = run_bass_kernel_spmd(nc, in_maps, core_ids=list(range(NCORES)))
    outp = np.empty((4, 2 * T, D), dtype=np.float32)
    for c in range(NCORES):
        b, h = c // 2, c % 2
        outp[b, h * T:(h + 1) * T, :] = np.asarray(res.results[c]["out"], dtype=np.float32)
    if debug_out:
        return outp, res.results
    return outp


def kernel(**inputs):
    return run_layers(inputs, [0, 1, 0, 1])
```

```python
import contextlib
import os
import numpy as np
import concourse.bass as bass
import concourse.mybir as mybir
from concourse.bass_utils import run_bass_kernel_spmd

F32 = mybir.dt.float32
BF16 = mybir.dt.bfloat16
I32 = mybir.dt.int32
AF = mybir.ActivationFunctionType
ALU = mybir.AluOpType
AX = mybir.AxisListType

ENGS = ("pe", "act", "dve", "pool", "sp")

NCORES = 8
T = 4096
TH = 2048
TE = T + TH
D = 1024
KC = 8
FF = 3584
NJ = FF // 128
NE = 8
INW = 2304
EPS = 1e-5
DIL = (1, 4, 16)


class Op:
    __slots__ = ("eng", "fn", "deps", "signal", "count", "dma_key", "is_dma", "inc")

    def __init__(self, eng, fn, dma_key, inc=16):
        self.eng = eng
        self.fn = fn
        self.deps = []
        self.signal = False
        self.count = None
        self.dma_key = dma_key
        self.is_dma = dma_key is not None
        self.inc = inc


class Sched:
    def __init__(self, nc, same_engine_sync=True):
        self.nc = nc
        self.ops = {e: [] for e in ENGS}
        self.last_w = {}
        self.readers = {}
        self.dma_counts = {}
        self.same_engine_sync = same_engine_sync

    def op(self, eng, fn, reads=(), writes=(), dma_key=None, inc=16):
        o = Op(eng, fn, dma_key, inc)
        deps = []
        for t in reads:
            w = self.last_w.get(t)
            if w is not None:
                deps.append((w, 0))
        for t in writes:
            w = self.last_w.get(t)
            if w is not None:
                deps.append((w, 1))
            for r in self.readers.get(t, ()):
                deps.append((r, 2))
        seen = set()
        for p, kind in deps:
            if id(p) in seen:
                continue
            if (not p.is_dma) and p.eng == eng:
                if eng == "pe" or kind == 2 or not self.same_engine_sync:
                    continue
            seen.add(id(p))
            o.deps.append(p)
            if not p.is_dma:
                p.signal = True
        for t in reads:
            self.readers.setdefault(t, []).append(o)
        for t in writes:
            self.last_w[t] = o
            self.readers[t] = []
        if o.is_dma:
            c = self.dma_counts.get(dma_key, 0) + 1
            self.dma_counts[dma_key] = c
            o.count = c
        self.ops[eng].append(o)
        return o

    def barrier(self):
        lasts = []
        dma_last = {}
        for e in ENGS:
            lst = [o for o in self.ops[e] if not o.is_dma and o.fn is not None]
            if lst:
                lasts.append(lst[-1])
            for o in self.ops[e]:
                if o.is_dma:
                    dma_last[o.dma_key] = o
        for e in ENGS:
            o = Op(e, None, None)
            for p in lasts:
                if p.eng != e:
                    o.deps.append(p)
                    p.signal = True
            o.deps.extend(dma_last.values())
            self.ops[e].append(o)
        self.last_w = {}
        self.readers = {}

    def emit(self):
        nc = self.nc
        with contextlib.ExitStack() as st:
            esem = {e: st.enter_context(nc.semaphore("s_" + e)) for e in ENGS}
            dsem = {k: st.enter_context(nc.semaphore("d_%d" % i)) for i, k in enumerate(self.dma_counts)}
            block = st.enter_context(nc.Block())
            for e in ENGS:
                c = 0
                for o in self.ops[e]:
                    if (not o.is_dma) and o.signal:
                        c += 1
                        o.count = c

            def event(p):
                if p.is_dma:
                    return dsem[p.dma_key], p.inc * p.count
                return esem[p.eng], p.count

            def run(e, h):
                waited = {}
                for o in self.ops[e]:
                    for p in o.deps:
                        s, v = event(p)
                        if waited.get(id(s), 0) >= v:
                            continue
                        waited[id(s)] = v
                        h.wait_ge(s, v)
                    if o.fn is None:
                        continue
                    ins = o.fn(h)
                    if o.is_dma:
                        if o.inc == 1:
                            ins.then_inc(dsem[o.dma_key])
                        else:
                            ins.then_inc(dsem[o.dma_key], o.inc)
                    elif o.signal:
                        ins.then_inc(esem[e], 1)

            block.tensor(lambda h: run("pe", h))
            block.scalar(lambda h: run("act", h))
            block.vector(lambda h: run("dve", h))
            block.gpsimd(lambda h: run("pool", h))
            block.sync(lambda h: run("sp", h))


class Arena:
    def __init__(self, nc, nbytes=206 * 1024):
        self.big = nc.alloc_sbuf_tensor("arena", [128, nbytes], mybir.dt.uint8)
        self.off = 0
        self.limit = nbytes

    def alloc(self, shape, dtype):
        esz = 2 if dtype == BF16 else 4
        n = int(np.prod(shape[1:]))
        off = (self.off + 63) // 64 * 64
        assert off + n * esz <= self.limit, ("SBUF arena overflow", off, n * esz, self.limit)
        self.off = off + n * esz
        ap = self.big[0:shape[0], off:off + n * esz].bitcast(dtype)
        if len(shape) == 3:
            ap = ap.rearrange("p (a b) -> p a b", b=shape[2])
        elif len(shape) == 4:
            ap = ap.rearrange("p (a b c) -> p a b c", b=shape[2], c=shape[3])
        return ap

    def mark(self):
        return self.off

    def release(self, m):
        self.off = m


def build_program(layer_types, groups=None, stop_after=None, debug_out=()):
    L = len(layer_types)
    ND = max(1, sum(1 for t in layer_types if t == 0))
    NM = max(1, sum(1 for t in layer_types if t == 1))
    if groups is None:
        groups = [[0, 1], [2, 3], [4, 5], [6, 7]]
    nc = bass.Bass("TRN2", target_bir_lowering=False)
    S = Sched(nc)
    A = Arena(nc)

    def din(name, shape, dt=F32):
        return nc.dram_tensor(name, list(shape), dt, kind="ExternalInput").ap()

    x_in = din("x", [T, D])
    pos_in = din("pos", [1, T], I32)
    flag_in = din("flag", [128, 1])
    cident_in = din("c_ident", [128, 128])
    cperm_in = din("c_perm", [128, 128])
    cmask_in = din("c_mask", [128, 3, 128])
    ccol_in = din("c_col", [128, 2])
    attn_norm = din("attn_norm", [L, D])
    w_in = din("w_in", [L, D, INW])
    mix_a = din("mix_norm_a", [L, 512])
    mix_b = din("mix_norm_b", [L, 512])
    sinks = din("sinks", [1, L * 8])
    w_out = din("w_out", [L, D, D])
    ffn_norm = din("ffn_norm", [L, D])
    if 0 in layer_types:
        dwg = din("dense_w_gate", [ND, D, FF])
        dwu = din("dense_w_up", [ND, D, FF])
        dwd = din("dense_w_down", [ND, FF, D])
    if 1 in layer_types:
        router = din("router", [NM, D, NE])
        mwg = din("moe_w_gate", [NM, NE, D, FF])
        mwu = din("moe_w_up", [NM, NE, D, FF])
        mwd = din("moe_w_down", [NM, NE, FF, D])
    fin_norm = din("final_norm", [1, D])
    out = nc.dram_tensor("out", [T, D], F32, kind="ExternalOutput").ap()

    def dscr(name, shape, dt):
        if name in debug_out:
            return nc.dram_tensor(name, list(shape), dt, kind="ExternalOutput").ap()
        return nc.dram_tensor(name, list(shape), dt).ap()

    xres = dscr("xres", [T, D], F32)
    qT = dscr("qT", [128, 8, T], BF16)
    kT = dscr("kT", [128, 5, TE], BF16)
    vbuf = dscr("vbuf", [TE, 640], BF16)
    oT = dscr("oT", [128, 8, T], BF16)
    cs = dscr("cs", [128, 2, T], F32)
    sendk = dscr("sendk", [5, 128, TH], BF16)
    recvk = dscr("recvk", [5, 256, TH], BF16)
    sendv = dscr("sendv", [2, 1024, 640], BF16)
    recvv = dscr("recvv", [2, 2048, 640], BF16)

    ps = [nc.alloc_psum_tensor("ps%d" % i, [128, 512], F32)[:, :] for i in range(8)]

    ident = A.alloc([128, 128], F32)
    ones_bf = A.alloc([128, 128], BF16)
    perm_bf = A.alloc([128, 128], BF16)
    maskf = A.alloc([128, 3, 128], F32)
    flagc = A.alloc([128, 1], F32)
    ccol = A.alloc([128, 2], F32)
    NG = 24
    gcol = A.alloc([128, L * NG], F32)
    esink = A.alloc([128, L * 8], F32)
    sinkcol = A.alloc([128, L * 4], F32)
    epsc = A.alloc([128, 1], F32)
    negpi = A.alloc([128, 1], F32)

    def dma(q, out_ap, in_ap, reads=(), writes=(), key=None, **kw):
        return S.op(q, lambda e: e.dma_start(out=out_ap, in_=in_ap, **kw), reads=reads, writes=writes, dma_key=key)

    m0 = A.mark()
    permf = A.alloc([128, 128], F32)
    dma("sp", ident, cident_in, writes=["ident"], key="c0")
    dma("sp", permf, cperm_in, writes=["permf"], key="c1")
    dma("sp", maskf, cmask_in, writes=["maskf"], key="c2")
    dma("sp", flagc, flag_in, writes=["flagc"], key="c3")
    dma("sp", ccol, ccol_in, writes=["ccol"], key="c4")
    dma("sp", esink, sinks.partition_broadcast(128), writes=["esink"], key="c5")
    for l in range(L):
        b0 = l * NG
        dma("sp", gcol[:, b0:b0 + 8], attn_norm[l].rearrange("(k p) -> p k", p=128), writes=["gcol%d" % l],
            key="g0", allow_slow_non_contiguous=True)
        dma("sp", gcol[:, b0 + 8:b0 + 16], ffn_norm[l].rearrange("(k p) -> p k", p=128), writes=["gcol%d" % l],
            key="g0", allow_slow_non_contiguous=True)
        dma("sp", gcol[:, b0 + 16:b0 + 20], mix_a[l].rearrange("(k p) -> p k", p=128), writes=["gcol%d" % l],
            key="g0", allow_slow_non_contiguous=True)
        dma("sp", gcol[:, b0 + 20:b0 + 24], mix_b[l].rearrange("(k p) -> p k", p=128), writes=["gcol%d" % l],
            key="g0", allow_slow_non_contiguous=True)
    S.op("pool", lambda e: e.memset(ones_bf, 1.0), writes=["ones"])
    S.op("pool", lambda e: e.memset(epsc, EPS), writes=["epsc"])
    S.op("pool", lambda e: e.memset(negpi, -3.1415925), writes=["negpi"])
    S.op("dve", lambda e: e.tensor_copy(out=perm_bf, in_=permf), reads=["permf"], writes=["perm"])
    S.op("act", lambda e: e.activation(out=esink, in_=esink, func=AF.Exp), reads=["esink"], writes=["esink"])
    for l in range(L):
        for j in range(4):
            c0 = l * 8 + 2 * j
            S.op("dve", lambda e, c0=c0, l=l, j=j: e.tensor_copy(out=sinkcol[0:64, l * 4 + j:l * 4 + j + 1],
                                                              in_=esink[0:64, c0:c0 + 1]),
                 reads=["esink"], writes=["sinkcol"])
            S.op("dve", lambda e, c0=c0, l=l, j=j: e.tensor_copy(out=sinkcol[64:128, l * 4 + j:l * 4 + j + 1],
                                                              in_=esink[64:128, c0 + 1:c0 + 2]),
                 reads=["esink"], writes=["sinkcol"])
    CH = 1024
    posi = A.alloc([128, CH], I32)
    posf = A.alloc([128, CH], F32)
    tt_ = A.alloc([128, CH], F32)
    kf_ = A.alloc([128, CH], F32)
    ki_ = A.alloc([128, CH], I32)
    msk_ = A.alloc([128, CH], F32)
    tab = A.alloc([128, 2, CH], F32)
    INV2PI = float(1.0 / (2.0 * np.pi))
    for ch in range(T // CH):
        dma("sp", posi, pos_in[:, ch * CH:(ch + 1) * CH].partition_broadcast(128), writes=["posi"], key="posi")
        S.op("dve", lambda e: e.tensor_copy(out=posf, in_=posi), reads=["posi"], writes=["posf"])
        S.op("dve", lambda e: e.tensor_scalar(out=posf, in0=posf, scalar1=ccol[:, 0:1], scalar2=INV2PI,
                                              op0=ALU.mult, op1=ALU.mult), reads=["posf", "ccol"], writes=["posf"])
        for which, off in ((1, 0.5), (0, 0.75)):
            S.op("dve", lambda e, off=off: e.tensor_scalar(out=tt_, in0=posf, scalar1=float(off), scalar2=None,
                                                           op0=ALU.add), reads=["posf"], writes=["tt"])
            S.op("dve", lambda e: e.tensor_copy(out=ki_, in_=tt_), reads=["tt"], writes=["ki"])
            S.op("dve", lambda e: e.tensor_copy(out=kf_, in_=ki_), reads=["ki"], writes=["kf"])
            S.op("dve", lambda e: e.tensor_tensor(out=tt_, in0=tt_, in1=kf_, op=ALU.subtract), reads=["tt", "kf"], writes=["tt"])
            S.op("dve", lambda e: e.tensor_scalar(out=msk_, in0=tt_, scalar1=0.0, scalar2=None, op0=ALU.is_lt),
                 reads=["tt"], writes=["msk"])
            S.op("dve", lambda e: e.tensor_tensor(out=tt_, in0=tt_, in1=msk_, op=ALU.add), reads=["tt", "msk"], writes=["tt"])
            S.op("act", lambda e, which=which: e.activation(out=tab[:, which, :], in_=tt_, func=AF.Sin,
                                                            bias=negpi[:, 0:1], scale=6.283185),
                 reads=["tt", "negpi"], writes=["tab%d" % which])
        S.op("dve", lambda e: e.tensor_scalar(out=tab[:, 1, :], in0=tab[:, 1, :], scalar1=ccol[:, 1:2], scalar2=None,
                                              op0=ALU.mult), reads=["tab1", "ccol"], writes=["tab1"])
        dma("sp", cs[:, :, ch * CH:(ch + 1) * CH], tab, reads=["tab0", "tab1"], writes=["cs"], key="cs")
    S.barrier()
    A.release(m0)

    def rms_rstd(ssq_ap, rt_ap, rstd_ap, n, reads, wtok):
        S.op("act", lambda e: e.activation(out=rt_ap, in_=ssq_ap, func=AF.Sqrt, bias=epsc[:, 0:1], scale=1.0 / n),
             reads=list(reads) + ["epsc"], writes=[wtok + "_rt"])
        S.op("dve", lambda e: e.reciprocal(out=rstd_ap, in_=rt_ap), reads=[wtok + "_rt"], writes=[wtok])

    def phase_proj(l):
        mk = A.mark()
        win = A.alloc([128, KC, INW], BF16)
        for kc in range(KC):
            dma("pool", win[:, kc, :], w_in[l][kc * 128:(kc + 1) * 128, :],
                writes=["win%d" % (kc // 2)], key="win%d" % (kc // 2), max_dma_last_dim=4096)
        xt = [A.alloc([128, 4, D], F32) for _ in range(2)]
        xn = A.alloc([128, 4, D], F32)
        junk = A.alloc([128, D], BF16)
        ssq = A.alloc([128, 4], F32)
        rt = A.alloc([128, 4], F32)
        rstd = A.alloc([128, 4], F32)
        hT = [A.alloc([128, KC, 512], BF16) for _ in range(2)]
        cst = [A.alloc([128, 2, 512], F32) for _ in range(2)]
        tb = [A.alloc([128, 512], BF16) for _ in range(2)]
        ra = [A.alloc([128, 512], F32) for _ in range(2)]
        rb = [A.alloc([128, 512], F32) for _ in range(2)]
        qkst = A.alloc([128, 13, 512], BF16)
        vst = A.alloc([128, 4, 640], BF16)
        xsrc = x_in if l == 0 else xres
        g0 = l * NG
        colstarts = [0, 128, 256, 384, 1536, 1664, 1792, 1920, 512, 640, 768, 896, 2048]
        NT = T // 512

        def load(tt):
            b = tt % 2
            dma("sp", xt[b], xsrc[tt * 512:(tt + 1) * 512, :].rearrange("(b p) d -> p b d", p=128),
                writes=["xt%d" % b], key="xt%d" % b)
            dma("sp", cst[b], cs[:, :, tt * 512:(tt + 1) * 512], writes=["cst%d" % b], key="cst%d" % b)

        load(0)
        for tt in range(NT):
            b = tt % 2
            if tt + 1 < NT:
                load(tt + 1)
            X = xt[b]
            H = hT[b]
            for bb in range(4):
                S.op("act", lambda e, bb=bb, X=X: e.activation(out=junk, in_=X[:, bb, :], func=AF.Square,
                                                               accum_out=ssq[:, bb:bb + 1]),
                     reads=["xt%d" % b], writes=["ssq%d" % bb])
            rms_rstd(ssq, rt, rstd, D, ["ssq%d" % i for i in range(4)], "rstd")
            for bb in range(4):
                S.op("dve", lambda e, bb=bb, X=X: e.tensor_scalar(out=xn[:, bb, :], in0=X[:, bb, :],
                                                                  scalar1=rstd[:, bb:bb + 1], scalar2=None, op0=ALU.mult),
                     reads=["xt%d" % b, "rstd"], writes=["xn%d" % bb])
            for kc in range(KC):
                pt = ps[kc % 2]
                for bb in range(4):
                    S.op("pe", lambda e, bb=bb, kc=kc, pt=pt: e.transpose(pt[:, bb * 128:(bb + 1) * 128],
                                                                          xn[:, bb, kc * 128:(kc + 1) * 128], ident),
                         reads=["xn%d" % bb, "ident"], writes=["ps%d" % (kc % 2)])
                eng = "act" if kc % 2 == 0 else "dve"
                if eng == "act":
                    S.op("act", lambda e, kc=kc, pt=pt, H=H: e.activation(out=H[:, kc, :], in_=pt, func=AF.Copy,
                                                                          scale=gcol[:, g0 + kc:g0 + kc + 1]),
                         reads=["ps%d" % (kc % 2), "gcol%d" % l], writes=["hT%d_%d" % (b, kc)])
                else:
                    S.op("dve", lambda e, kc=kc, pt=pt, H=H: e.tensor_scalar(out=H[:, kc, :], in0=pt,
                                                                             scalar1=gcol[:, g0 + kc:g0 + kc + 1],
                                                                             scalar2=None, op0=ALU.mult),
                         reads=["ps%d" % (kc % 2), "gcol%d" % l], writes=["hT%d_%d" % (b, kc)])
            hreads = ["hT%d_%d" % (b, kc) for kc in range(KC)]

            def rope(si, b=b):
                a = si % 2
                cb = cst[b]
                S.op("pe", lambda e, a=a: e.matmul(ps[4 + a][:, :], lhsT=perm_bf, rhs=tb[a], start=True, stop=True),
                     reads=["tb%d" % a, "perm"], writes=["ps%d" % (4 + a)])
                S.op("dve", lambda e, a=a, cb=cb: e.tensor_tensor(out=ra[a], in0=tb[a], in1=cb[:, 0, :], op=ALU.mult),
                     reads=["tb%d" % a, "cst%d" % b], writes=["ra%d" % a])
                S.op("dve", lambda e, a=a, cb=cb: e.tensor_tensor(out=rb[a], in0=ps[4 + a][:, :], in1=cb[:, 1, :], op=ALU.mult),
                     reads=["ps%d" % (4 + a), "cst%d" % b], writes=["rb%d" % a])
                S.op("pool", lambda e, a=a, si=si: e.tensor_tensor(out=qkst[:, si, :], in0=ra[a], in1=rb[a], op=ALU.add),
                     reads=["ra%d" % a, "rb%d" % a], writes=["qkst%d" % si])

            pending = None
            for si, c0 in enumerate(colstarts):
                a = si % 2
                for kc in range(KC):
                    S.op("pe", lambda e, a=a, kc=kc, c0=c0, H=H: e.matmul(ps[2 + a][:, :], lhsT=win[:, kc, c0:c0 + 128],
                                                                          rhs=H[:, kc, :], start=(kc == 0), stop=(kc == KC - 1)),
                         reads=[hreads[kc], "win%d" % (kc // 2)], writes=["ps%d" % (2 + a)])
                S.op("act", lambda e, a=a: e.copy(out=tb[a], in_=ps[2 + a][:, :]), reads=["ps%d" % (2 + a)], writes=["tb%d" % a])
                if pending is not None:
                    rope(pending)
                pending = si
            rope(pending)
            for bb in range(4):
                pv = ps[6 + bb % 2]
                for kc in range(KC):
                    S.op("pe", lambda e, bb=bb, kc=kc, pv=pv, H=H: e.matmul(pv[:, :], lhsT=H[:, kc, bb * 128:(bb + 1) * 128],
                                                                            rhs=win[:, kc, 1024:1536], start=(kc == 0), stop=(kc == KC - 1)),
                         reads=[hreads[kc], "win%d" % (kc // 2)], writes=["ps%d" % (6 + bb % 2)])
                S.op("act", lambda e, bb=bb, pv=pv: e.copy(out=vst[:, bb, 0:512], in_=pv[:, :]),
                     reads=["ps%d" % (6 + bb % 2)], writes=["vst%d" % bb])
                pw = ps[4 + bb % 2]
                for kc in range(KC):
                    S.op("pe", lambda e, bb=bb, kc=kc, pw=pw, H=H: e.matmul(pw[:, 0:128], lhsT=H[:, kc, bb * 128:(bb + 1) * 128],
                                                                            rhs=win[:, kc, 2176:2304], start=(kc == 0), stop=(kc == KC - 1)),
                         reads=[hreads[kc], "win%d" % (kc // 2)], writes=["ps%d" % (4 + bb % 2)])
                S.op("dve", lambda e, bb=bb, pw=pw: e.tensor_copy(out=vst[:, bb, 512:640], in_=pw[:, 0:128]),
                     reads=["ps%d" % (4 + bb % 2)], writes=["vst%d" % bb])
            dma("sp", qT[:, :, tt * 512:(tt + 1) * 512], qkst[:, 0:8, :], reads=["qkst%d" % i for i in range(8)],
                writes=["qT"], key="qTst")
            dma("sp", kT[:, :, TH + tt * 512:TH + (tt + 1) * 512], qkst[:, 8:13, :],
                reads=["qkst%d" % i for i in range(8, 13)], writes=["kT"], key="kTst")
            dma("sp", vbuf[TH + tt * 512:TH + (tt + 1) * 512, :].rearrange("(b p) c -> p b c", p=128), vst,
                reads=["vst%d" % i for i in range(4)], writes=["vbuf"], key="vst")
        S.barrier()
        A.release(mk)

    def phase_exchange():
        for c in range(5):
            dma("sp", sendk[c], kT[:, c, T:TE], writes=["sendk%d" % c], key="ex0")
        for u in range(2):
            dma("sp", sendv[u], vbuf[T + u * 1024:T + (u + 1) * 1024, :], writes=["sendv%d" % u], key="ex1")
        for c in range(5):
            S.op("pool", lambda e, c=c: e.collective_compute("AllGather", ALU.bypass, replica_groups=groups,
                                                             ins=[sendk[c].opt()], outs=[recvk[c].opt()]),
                 reads=["sendk%d" % c], writes=["recvk%d" % c], dma_key="cc", inc=1)
        for u in range(2):
            S.op("pool", lambda e, u=u: e.collective_compute("AllGather", ALU.bypass, replica_groups=groups,
                                                             ins=[sendv[u].opt()], outs=[recvv[u].opt()]),
                 reads=["sendv%d" % u], writes=["recvv%d" % u], dma_key="cc", inc=1)
        for c in range(5):
            dma("sp", kT[:, c, 0:TH], recvk[c, 0:128, :], reads=["recvk%d" % c], writes=["kT"], key="ex2")
        for u in range(2):
            dma("sp", vbuf[u * 1024:(u + 1) * 1024, :], recvv[u, 0:1024, :], reads=["recvv%d" % u], writes=["vbuf"], key="ex3")
        S.barrier()

    def phase_attn(l):
        mk = A.mark()
        kct = [A.alloc([128, TE], BF16) for _ in range(2)]
        qct = [A.alloc([128, T], BF16) for _ in range(2)]
        vA = {d: A.alloc([128, TE // 128, 128], BF16) for d in DIL}
        vB = A.alloc([128, TE // 128, 64], BF16)
        accn = A.alloc([128, T], F32)
        accd = A.alloc([128, T], F32)
        rden = A.alloc([128, T], F32)
        oc = A.alloc([128, T], BF16)
        P = [A.alloc([128, 2, 256], BF16) for _ in range(2)]
        M4 = {}
        for nm, prev_idx in (("A", 1), ("B", 2)):
            for halo in (0, 1):
                mt = A.alloc([128, 2, 256], BF16)
                M4[(nm, halo)] = mt
                for h in range(2):
                    if halo:
                        S.op("dve", lambda e, mt=mt, h=h, prev_idx=prev_idx: e.tensor_scalar(
                            out=mt[:, h, 0:128], in0=maskf[:, prev_idx, :], scalar1=flagc[:, 0:1], scalar2=None, op0=ALU.mult),
                             reads=["maskf", "flagc"], writes=["M4"])
                    else:
                        S.op("dve", lambda e, mt=mt, h=h, prev_idx=prev_idx: e.tensor_copy(out=mt[:, h, 0:128], in_=maskf[:, prev_idx, :]),
                             reads=["maskf"], writes=["M4"])
                    S.op("dve", lambda e, mt=mt, h=h: e.tensor_copy(out=mt[:, h, 128:256], in_=maskf[:, 0, :]),
                         reads=["maskf"], writes=["M4"])
        ps_sa = [[ps[0], ps[1]], [ps[6], ps[7]]]

        def load_chunk(c):
            b = c % 2
            if c < 4:
                dma("sp", kct[b], kT[:, c, :], writes=["kct%d" % b], key="kct%d" % b)
            else:
                g = (c - 4) // 2
                dma("sp", kct[b][0:64, :], kT[g * 64:(g + 1) * 64, 4, :], writes=["kct%d" % b], key="kct%d" % b)
                dma("sp", kct[b][64:128, :], kT[g * 64:(g + 1) * 64, 4, :], writes=["kct%d" % b], key="kct%d" % b)
            dma("sp", qct[b], qT[:, c, :], writes=["qct%d" % b], key="qct%d" % b)

        def load_v(c, d):
            if c < 4:
                cols = slice(c * 128, (c + 1) * 128)
                if d == 1:
                    for u in range(TE // 2048):
                        src = vbuf[u * 2048:(u + 1) * 2048, cols].rearrange("(s i) c -> i s c", i=128)
                        dma("sp", vA[d][:, u * 16:(u + 1) * 16, :], src, writes=["vA%d" % d], key="vA%d" % d)
                else:
                    seg = 128 * d
                    for sg_ in range(TE // seg):
                        src = vbuf[sg_ * seg:(sg_ + 1) * seg, cols].rearrange("(i r) c -> i r c", r=d)
                        dma("sp", vA[d][:, sg_ * d:(sg_ + 1) * d, :], src, writes=["vA%d_%d" % (d, sg_ % 2)], key="vA%d_%d" % (d, sg_ % 2))
            else:
                g = (c - 4) // 2
                for u in range(TE // 2048):
                    src = vbuf[u * 2048:(u + 1) * 2048, 512 + g * 64:512 + (g + 1) * 64].rearrange("(s i) c -> i s c", i=128)
                    dma("sp", vB[:, u * 16:(u + 1) * 16, :], src, writes=["vB"], key="vB")

        DBG_NCH = int(os.environ.get("ATT_NCH", "8"))
        DBG_BR = tuple(int(v) for v in os.environ.get("ATT_BR", "1,4,16").split(","))
        DBG_NU = int(os.environ.get("ATT_NU", "9999"))
        load_chunk(0)
        for d in DIL:
            load_v(0, d)
        load_v(4, 1)
        for c in range(DBG_NCH):
            b = c % 2
            grpA = c < 4
            branches = tuple(d for d in DIL if d in DBG_BR) if grpA else (1,)
            KCt, QCt = kct[b], qct[b]
            first_branch = True
            for d in branches:
                nseg = T // (128 * d)
                Sh = TH // (128 * d)
                Qr = QCt.rearrange("p (s i r) -> p s r i", i=128, r=d)
                Kr = KCt.rearrange("p (s i r) -> p s r i", i=128, r=d)
                accn_r = accn.rearrange("p (s i r) -> p s r i", i=128, r=d)
                accd_r = accd.rearrange("p (s i r) -> p s r i", i=128, r=d)
                if grpA:
                    Vt = vA[d]
                    vtok = ["vA%d" % d] if d == 1 else ["vA%d_0" % d, "vA%d_1" % d]
                else:
                    Vt = vB
                    vtok = ["vB"]
                if d == 1:
                    groups_ = [[(s, 0) for s in range(g4 * 4, g4 * 4 + 4)] for g4 in range(nseg // 4)]
                else:
                    groups_ = [[(s, r) for r in range(r0, r0 + 4)] for s in range(nseg) for r0 in range(0, d, 4)]
                units = []
                for gi, g in enumerate(groups_):
                    for j, (s, r) in enumerate(g):
                        units.append((gi, j, s, r))

                def scores(n, u):
                    gi, j, s, r = u
                    alt = n % 2
                    Qv = Qr[:, s, r, :]
                    Kp = Kr[:, s + Sh - 1, r, :]
                    Kc_ = Kr[:, s + Sh, r, :]
                    for h in range(2):
                        lo, hi = h * 64, (h + 1) * 64
                        S.op("pe", lambda e, h=h, lo=lo, hi=hi, Kp=Kp, Qv=Qv, alt=alt: e.matmul(
                            ps_sa[alt][h][:, 0:128], lhsT=Kp[lo:hi, :], rhs=Qv[lo:hi, :], start=True, stop=True),
                             reads=["kct%d" % b, "qct%d" % b], writes=["pss%d" % alt])
                        S.op("pe", lambda e, h=h, lo=lo, hi=hi, Kc_=Kc_, Qv=Qv, alt=alt: e.matmul(
                            ps_sa[alt][h][:, 128:256], lhsT=Kc_[lo:hi, :], rhs=Qv[lo:hi, :], start=True, stop=True),
                             reads=["kct%d" % b, "qct%d" % b], writes=["pss%d" % alt])

                def rest(n, u):
                    gi, j, s, r = u
                    alt = n % 2
                    Pt = P[alt]
                    pn = ps[2 + gi % 2]
                    pd = ps[4 + gi % 2]
                    for h in range(2):
                        S.op("act", lambda e, h=h, Pt=Pt, alt=alt: e.activation(out=Pt[:, h, :], in_=ps_sa[alt][h][:, 0:256],
                                                                               func=AF.Exp, scale=0.125),
                             reads=["pss%d" % alt], writes=["P%d" % alt])
                    mt = M4[("A" if grpA else "B", 1 if s == 0 else 0)]
                    S.op("pool", lambda e, Pt=Pt, mt=mt: e.tensor_tensor(out=Pt, in0=Pt, in1=mt, op=ALU.mult),
                         reads=["P%d" % alt, "M4"], writes=["P%d" % alt])
                    nprev = (s + Sh - 1) * d + r
                    ncur = (s + Sh) * d + r
                    for h in range(2):
                        lo, hi = h * 64, (h + 1) * 64
                        if grpA:
                            Vp, Vc = Vt[:, nprev, lo:hi], Vt[:, ncur, lo:hi]
                        else:
                            Vp, Vc = Vt[:, nprev, :], Vt[:, ncur, :]
                        cols = slice(j * 128, (j + 1) * 128)
                        S.op("pe", lambda e, Vp=Vp, Pt=Pt, h=h, lo=lo, hi=hi, cols=cols, pn=pn: e.matmul(
                            pn[lo:hi, cols], lhsT=Vp, rhs=Pt[:, h, 0:128], start=True, stop=False),
                             reads=["P%d" % alt] + vtok, writes=["ps%d" % (2 + gi % 2)])
                        S.op("pe", lambda e, Vc=Vc, Pt=Pt, h=h, lo=lo, hi=hi, cols=cols, pn=pn: e.matmul(
                            pn[lo:hi, cols], lhsT=Vc, rhs=Pt[:, h, 128:256], start=False, stop=True),
                             reads=["P%d" % alt] + vtok, writes=["ps%d" % (2 + gi % 2)])
                        S.op("pe", lambda e, Pt=Pt, h=h, lo=lo, hi=hi, cols=cols, pd=pd: e.matmul(
                            pd[lo:hi, cols], lhsT=ones_bf[:, 0:64], rhs=Pt[:, h, 0:128], start=True, stop=False),
                             reads=["P%d" % alt, "ones"], writes=["ps%d" % (4 + gi % 2)])
                        S.op("pe", lambda e, Pt=Pt, h=h, lo=lo, hi=hi, cols=cols, pd=pd: e.matmul(
                            pd[lo:hi, cols], lhsT=ones_bf[:, 0:64], rhs=Pt[:, h, 128:256], start=False, stop=True),
                             reads=["P%d" % alt, "ones"], writes=["ps%d" % (4 + gi % 2)])
                    if j == 3 and not os.environ.get("ATT_NOACC"):
                        g = groups_[gi]
                        s0, r0 = g[0]
                        if d == 1:
                            an = accn[:, s0 * 128:(s0 + 4) * 128]
                            ad = accd[:, s0 * 128:(s0 + 4) * 128]
                            pnv, pdv = pn[:, :], pd[:, :]
                        else:
                            an = accn_r[:, s0, r0:r0 + 4, :]
                            ad = accd_r[:, s0, r0:r0 + 4, :]
                            pnv = pn[:, :].rearrange("p (r i) -> p r i", i=128)
                            pdv = pd[:, :].rearrange("p (r i) -> p r i", i=128)
                        if first_branch:
                            S.op("dve", lambda e, an=an, pnv=pnv: e.tensor_copy(out=an, in_=pnv),
                                 reads=["ps%d" % (2 + gi % 2)], writes=["accn"])
                            S.op("dve", lambda e, ad=ad, pdv=pdv: e.tensor_copy(out=ad, in_=pdv),
                                 reads=["ps%d" % (4 + gi % 2)], writes=["accd"])
                        else:
                            S.op("dve", lambda e, an=an, pnv=pnv: e.tensor_tensor(out=an, in0=an, in1=pnv, op=ALU.add),
                                 reads=["ps%d" % (2 + gi % 2), "accn"], writes=["accn"])
                            S.op("dve", lambda e, ad=ad, pdv=pdv: e.tensor_tensor(out=ad, in0=ad, in1=pdv, op=ALU.add),
                                 reads=["ps%d" % (4 + gi % 2), "accd"], writes=["accd"])

                units = units[:DBG_NU]
                NOPIPE = bool(os.environ.get("ATT_NOPIPE"))
                if units and not NOPIPE:
                    scores(0, units[0])
                for n, u in enumerate(units):
                    if NOPIPE:
                        scores(n, u)
                    elif n + 1 < len(units):
                        scores(n + 1, units[n + 1])
                    rest(n, u)
                first_branch = False
                if c + 1 < 8 and d == branches[0]:
                    load_chunk(c + 1)
                if c + 1 < 4:
                    load_v(c + 1, d)
            if c == 5:
                load_v(6, 1)
            if not grpA:
                j4 = l * 4 + (c - 4)
                S.op("dve", lambda e, j4=j4: e.tensor_scalar(out=accd, in0=accd, scalar1=sinkcol[:, j4:j4 + 1], scalar2=None, op0=ALU.add),
                     reads=["accd", "sinkcol"], writes=["accd"])
            S.op("dve", lambda e: e.reciprocal(out=rden, in_=accd), reads=["accd"], writes=["rden"])
            S.op("pool", lambda e: e.tensor_tensor(out=oc, in0=accn, in1=rden, op=ALU.mult), reads=["accn", "rden"], writes=["oc"])
            dma("sp", oT[:, c, :], oc, reads=["oc"], writes=["oT"], key="oc")
        S.barrier()
        A.release(mk)

    def phase_ffn(l, li_dense, li_moe, last):
        moe = layer_types[l] == 1
        mk = A.mark()
        g0 = l * NG
        yacc = A.alloc([128, 8, D], F32)
        hTf = A.alloc([128, KC, 1024], BF16)
        gate = A.alloc([128, 8, NE], F32)
        wd = [A.alloc([128, NJ // 2, D], BF16) for _ in range(2)]
        wg = [A.alloc([128, KC, 256], BF16) for _ in range(2)]
        wu = [A.alloc([128, KC, 256], BF16) for _ in range(2)]
        sg = [A.alloc([128, 512], F32) for _ in range(2)]
        rt32 = A.alloc([128, KC, NE], F32)
        mU = A.mark()
        aT = A.alloc([128, NJ, 1024], BF16)
        A.release(mU)
        wout = A.alloc([128, KC, D], BF16)
        ot = A.alloc([128, 8, 512], BF16)
        mixT = A.alloc([128, 8, 512], BF16)
        rsa = A.alloc([128, 2, 512], F32)
        rsb = A.alloc([128, 2, 512], F32)
        xn = A.alloc([128, D], F32)
        junk = A.alloc([128, D], BF16)
        h32 = A.alloc([128, KC, 512], F32)
        sm = A.alloc([128, 16], F32)
        gsc = A.alloc([128, 48], F32)
        gfin = A.alloc([128, D], F32) if last else None
        NEXP = NE if moe else 1
        if moe:
            dma("sp", rt32, router[li_moe].rearrange("(k p) e -> p k e", p=128), writes=["rt32"], key="rt32")
        if last:
            dma("sp", gfin, fin_norm.partition_broadcast(128), writes=["gfin"], key="gfin")

        def wsrc(e):
            if moe:
                return mwg[li_moe, e], mwu[li_moe, e], mwd[li_moe, e]
            return dwg[li_dense], dwu[li_dense], dwd[li_dense]

        def load_gu(e, g):
            b = g % 2
            G_, U_, _ = wsrc(e)
            dma("pool", wg[b], G_[:, g * 256:(g + 1) * 256].rearrange("(k p) c -> p k c", p=128), writes=["wg%d" % b], key="wg%d" % b)
            dma("pool", wu[b], U_[:, g * 256:(g + 1) * 256].rearrange("(k p) c -> p k c", p=128), writes=["wu%d" % b], key="wu%d" % b)

        def load_wd(e, half):
            _, _, D_ = wsrc(e)
            for q in range(2):
                j0 = half * 14 + q * 7
                dma("pool", wd[half][:, q * 7:(q + 1) * 7, :], D_[j0 * 128:(j0 + 7) * 128, :].rearrange("(j p) c -> p j c", p=128),
                    writes=["wd%d" % half], key="wd%d" % half)

        for t4 in range(T // 1024):
            load_gu(0, 0)
            load_gu(0, 1)
            load_wd(0, 0)
            load_wd(0, 1)
            dma("pool", wout[:, 0:4, :], w_out[l][0:512, :].rearrange("(k p) c -> p k c", p=128), writes=["wout0"], key="wout0")
            dma("pool", wout[:, 4:8, :], w_out[l][512:1024, :].rearrange("(k p) c -> p k c", p=128), writes=["wout1"], key="wout1")
            dma("sp", yacc, (x_in if l == 0 else xres)[t4 * 1024:(t4 + 1) * 1024, :].rearrange("(b p) d -> p b d", p=128),
                writes=["yacc%d" % i for i in range(8)], key="yacc")
            for st in range(2):
                tok0 = t4 * 1024 + st * 512
                dma("sp", ot, oT[:, :, tok0:tok0 + 512], writes=["ot"], key="ot")
                S.op("act", lambda e: e.activation(out=mixT, in_=ot, func=AF.Square), reads=["ot"],
                     writes=["mixT"] + ["mixT%d" % c for c in range(8)])
                for grp in range(2):
                    for c in range(4):
                        S.op("pe", lambda e, grp=grp, c=c: e.matmul(ps[grp][:, :], lhsT=ones_bf, rhs=mixT[:, grp * 4 + c, :],
                                                                    start=(c == 0), stop=(c == 3)),
                             reads=["mixT", "ones"], writes=["ps%d" % grp])
                    rsx = rsa if grp == 0 else rsb
                    S.op("act", lambda e, grp=grp, rsx=rsx: e.activation(out=rsx[:, 0, :], in_=ps[grp][:, :], func=AF.Sqrt,
                                                                         bias=epsc[:, 0:1], scale=1.0 / 512),
                         reads=["ps%d" % grp, "epsc"], writes=["rs%d_0" % grp])
                    S.op("dve", lambda e, rsx=rsx: e.reciprocal(out=rsx[:, 1, :], in_=rsx[:, 0, :]),
                         reads=["rs%d_0" % grp], writes=["rs%d_1" % grp])
                for c in range(8):
                    rsx = rsa if c < 4 else rsb
                    gc = g0 + 16 + c
                    S.op("dve", lambda e, c=c, rsx=rsx, gc=gc: e.scalar_tensor_tensor(
                        out=mixT[:, c, :], in0=ot[:, c, :], scalar=gcol[:, gc:gc + 1], in1=rsx[:, 1, :], op0=ALU.mult, op1=ALU.mult),
                         reads=["ot", "rs%d_1" % (0 if c < 4 else 1), "gcol%d" % l], writes=["mixT%d" % c, "mixT"])
                mreads = ["mixT%d" % c for c in range(8)]
                for bb in range(4):
                    blk = st * 4 + bb
                    for half in range(2):
                        pb = ps[2 + half]
                        for c in range(8):
                            S.op("pe", lambda e, bb=bb, half=half, c=c, pb=pb: e.matmul(
                                pb[:, :], lhsT=mixT[:, c, bb * 128:(bb + 1) * 128], rhs=wout[:, c, half * 512:(half + 1) * 512],
                                start=(c == 0), stop=(c == 7)),
                                 reads=[mreads[c], "wout%d" % (c // 4)], writes=["ps%d" % (2 + half)])
                        S.op("dve", lambda e, blk=blk, half=half, pb=pb: e.tensor_tensor(
                            out=yacc[:, blk, half * 512:(half + 1) * 512], in0=yacc[:, blk, half * 512:(half + 1) * 512],
                            in1=pb[:, :], op=ALU.add),
                             reads=["ps%d" % (2 + half), "yacc%d" % blk], writes=["yacc%d" % blk])
                    S.op("act", lambda e, blk=blk: e.activation(out=junk, in_=yacc[:, blk, :], func=AF.Square, accum_out=sm[:, 0:1]),
                         reads=["yacc%d" % blk], writes=["sm0"])
                    S.op("act", lambda e: e.activation(out=sm[:, 1:2], in_=sm[:, 0:1], func=AF.Sqrt, bias=epsc[:, 0:1], scale=1.0 / D),
                         reads=["sm0", "epsc"], writes=["sm1"])
                    S.op("dve", lambda e: e.reciprocal(out=sm[:, 2:3], in_=sm[:, 1:2]), reads=["sm1"], writes=["sm2"])
                    S.op("dve", lambda e, blk=blk: e.tensor_scalar(out=xn, in0=yacc[:, blk, :], scalar1=sm[:, 2:3], scalar2=None, op0=ALU.mult),
                         reads=["yacc%d" % blk, "sm2"], writes=["xn"])
                    for kc in range(KC):
                        pt = ps[4 + (kc // 4)]
                        S.op("pe", lambda e, kc=kc, pt=pt: e.transpose(pt[:, (kc % 4) * 128:(kc % 4 + 1) * 128],
                                                                       xn[:, kc * 128:(kc + 1) * 128], ident),
                             reads=["xn", "ident"], writes=["ps%d" % (4 + kc // 4)])
                    for kc in range(KC):
                        pt = ps[4 + (kc // 4)]
                        S.op("act", lambda e, kc=kc, pt=pt, bb=bb: e.activation(
                            out=h32[:, kc, bb * 128:(bb + 1) * 128], in_=pt[:, (kc % 4) * 128:(kc % 4 + 1) * 128], func=AF.Copy,
                            scale=gcol[:, g0 + 8 + kc:g0 + 8 + kc + 1]),
                             reads=["ps%d" % (4 + kc // 4), "gcol%d" % l], writes=["h32_%d" % bb])
                    if moe:
                        for kc in range(KC):
                            S.op("pe", lambda e, kc=kc, bb=bb: e.matmul(ps[6][:, bb * 8:(bb + 1) * 8], lhsT=h32[:, kc, bb * 128:(bb + 1) * 128],
                                                                        rhs=rt32[:, kc, :], start=(kc == 0), stop=(kc == KC - 1)),
                                 reads=["h32_%d" % bb, "rt32"], writes=["ps6"])
                S.op("pool", lambda e, st=st: e.tensor_copy(out=hTf[:, :, st * 512:(st + 1) * 512], in_=h32),
                     reads=["h32_%d" % i for i in range(4)], writes=["hTf%d" % st])
                if moe:
                    lg = sm
                    for bb in range(4):
                        blk = st * 4 + bb
                        Lg = ps[6][:, bb * 8:(bb + 1) * 8]
                        gb = gate[:, blk, :]
                        S.op("dve", lambda e, Lg=Lg: e.tensor_copy(out=gsc[:, 0:8], in_=Lg), reads=["ps6"], writes=["lg"])
                        S.op("dve", lambda e: e.tensor_reduce(out=sm[:, 4:5], in_=gsc[:, 0:8], axis=AX.X, op=ALU.max),
                             reads=["lg"], writes=["m1"])
                        S.op("dve", lambda e: e.tensor_scalar(out=gsc[:, 8:16], in0=gsc[:, 0:8], scalar1=sm[:, 4:5], scalar2=None,
                                                              op0=ALU.is_equal), reads=["lg", "m1"], writes=["eq"])
                        S.op("dve", lambda e: e.scalar_tensor_tensor(out=gsc[:, 16:24], in0=gsc[:, 8:16], scalar=-1e30,
                                                                     in1=gsc[:, 0:8], op0=ALU.mult, op1=ALU.add),
                             reads=["eq", "lg"], writes=["lg2"])
                        S.op("dve", lambda e: e.tensor_reduce(out=sm[:, 5:6], in_=gsc[:, 16:24], axis=AX.X, op=ALU.max),
                             reads=["lg2"], writes=["m2"])
                        S.op("dve", lambda e: e.tensor_scalar(out=gsc[:, 24:32], in0=gsc[:, 0:8], scalar1=sm[:, 5:6], scalar2=None,
                                                              op0=ALU.is_ge), reads=["lg", "m2"], writes=["sel"])
                        S.op("dve", lambda e: e.tensor_scalar(out=sm[:, 6:7], in0=sm[:, 4:5], scalar1=-1.0, scalar2=None, op0=ALU.mult),
                             reads=["m1"], writes=["nm1"])
                        S.op("act", lambda e: e.activation(out=gsc[:, 32:40], in_=gsc[:, 0:8], func=AF.Exp, bias=sm[:, 6:7]),
                             reads=["lg", "nm1"], writes=["ex"])
                        S.op("dve", lambda e: e.tensor_tensor(out=gsc[:, 40:48], in0=gsc[:, 32:40], in1=gsc[:, 24:32], op=ALU.mult),
                             reads=["ex", "sel"], writes=["exs"])
                        S.op("dve", lambda e: e.tensor_reduce(out=sm[:, 7:8], in_=gsc[:, 40:48], axis=AX.X, op=ALU.add),
                             reads=["exs"], writes=["se"])
                        S.op("dve", lambda e: e.reciprocal(out=sm[:, 8:9], in_=sm[:, 7:8]), reads=["se"], writes=["rse"])
                        S.op("dve", lambda e, gb=gb: e.tensor_scalar(out=gb, in0=gsc[:, 40:48], scalar1=sm[:, 8:9], scalar2=None, op0=ALU.mult),
                             reads=["exs", "rse"], writes=["gate%d" % blk])
            S.barrier()
            hreads = ["hTf0", "hTf1"]
            for e_ in range(NEXP):
                for g in range(14):
                    b = g % 2
                    for jj in range(2):
                        j = g * 2 + jj
                        for th in range(2):
                            a = (jj * 2 + th) % 2
                            pG, pU = ps[a], ps[2 + a]
                            for kc in range(KC):
                                S.op("pe", lambda e, kc=kc, b=b, jj=jj, th=th, pG=pG: e.matmul(
                                    pG[:, :], lhsT=wg[b][:, kc, jj * 128:(jj + 1) * 128], rhs=hTf[:, kc, th * 512:(th + 1) * 512],
                                    start=(kc == 0), stop=(kc == KC - 1)),
                                     reads=["wg%d" % b, hreads[th]], writes=["ps%d" % a])
                            for kc in range(KC):
                                S.op("pe", lambda e, kc=kc, b=b, jj=jj, th=th, pU=pU: e.matmul(
                                    pU[:, :], lhsT=wu[b][:, kc, jj * 128:(jj + 1) * 128], rhs=hTf[:, kc, th * 512:(th + 1) * 512],
                                    start=(kc == 0), stop=(kc == KC - 1)),
                                     reads=["wu%d" % b, hreads[th]], writes=["ps%d" % (2 + a)])
                            S.op("act", lambda e, a=a, pG=pG: e.activation(out=sg[a], in_=pG[:, :], func=AF.Silu),
                                 reads=["ps%d" % a], writes=["sg%d" % a])
                            S.op("dve", lambda e, a=a, pU=pU, j=j, th=th: e.tensor_tensor(
                                out=aT[:, j, th * 512:(th + 1) * 512], in0=sg[a], in1=pU[:, :], op=ALU.mult),
                                 reads=["sg%d" % a, "ps%d" % (2 + a)], writes=["aT%d" % (j // 14)])
                    if g + 2 < 14:
                        load_gu(e_, g + 2)
                    elif e_ + 1 < NEXP:
                        load_gu(e_ + 1, g + 2 - 14)
                for half in range(2):
                    for blk in range(8):
                        for ch in range(2):
                            pb = ps[4 + (blk * 2 + ch) % 4]
                            ptok = "ps%d" % (4 + (blk * 2 + ch) % 4)
                            for jx in range(14):
                                j = half * 14 + jx
                                S.op("pe", lambda e, blk=blk, ch=ch, jx=jx, j=j, pb=pb, half=half: e.matmul(
                                    pb[:, :], lhsT=aT[:, j, blk * 128:(blk + 1) * 128], rhs=wd[half][:, jx, ch * 512:(ch + 1) * 512],
                                    start=(jx == 0), stop=(jx == 13)),
                                     reads=["aT%d" % half, "wd%d" % half], writes=[ptok])
                            ys = yacc[:, blk, ch * 512:(ch + 1) * 512]
                            if moe:
                                S.op("dve", lambda e, pb=pb, ys=ys, blk=blk, e_=e_: e.scalar_tensor_tensor(
                                    out=ys, in0=pb[:, :], scalar=gate[:, blk, e_:e_ + 1], in1=ys, op0=ALU.mult, op1=ALU.add),
                                     reads=[ptok, "yacc%d" % blk, "gate%d" % blk], writes=["yacc%d" % blk])
                            else:
                                S.op("dve", lambda e, pb=pb, ys=ys: e.tensor_tensor(out=ys, in0=ys, in1=pb[:, :], op=ALU.add),
                                     reads=[ptok, "yacc%d" % blk], writes=["yacc%d" % blk])
                    if e_ + 1 < NEXP:
                        load_wd(e_ + 1, half)
            if not last:
                dma("sp", xres[t4 * 1024:(t4 + 1) * 1024, :].rearrange("(b p) d -> p b d", p=128), yacc,
                    reads=["yacc%d" % i for i in range(8)], writes=["xres"], key="xst")
            else:
                S.barrier()
                for blk in range(8):
                    S.op("act", lambda e, blk=blk: e.activation(out=junk, in_=yacc[:, blk, :], func=AF.Square, accum_out=sm[:, 0:1]),
                         reads=["yacc%d" % blk], writes=["sm0"])
                    S.op("act", lambda e: e.activation(out=sm[:, 1:2], in_=sm[:, 0:1], func=AF.Sqrt, bias=epsc[:, 0:1], scale=1.0 / D),
                         reads=["sm0", "epsc"], writes=["sm1"])
                    S.op("dve", lambda e: e.reciprocal(out=sm[:, 2:3], in_=sm[:, 1:2]), reads=["sm1"], writes=["sm2"])
                    S.op("dve", lambda e, blk=blk: e.scalar_tensor_tensor(out=yacc[:, blk, :], in0=yacc[:, blk, :], scalar=sm[:, 2:3],
                                                                         in1=gfin, op0=ALU.mult, op1=ALU.mult),
                         reads=["yacc%d" % blk, "sm2", "gfin"], writes=["yacc%d" % blk])
                dma("sp", out[t4 * 1024:(t4 + 1) * 1024, :].rearrange("(b p) d -> p b d", p=128), yacc,
                    reads=["yacc%d" % i for i in range(8)], writes=["out"], key="xst")
            S.barrier()
        A.release(mk)

    nd = nm = 0
    for l, lt in enumerate(layer_types):
        if stop_after == "setup":
            break
        if not os.environ.get("SKIP_PROJ"):
            phase_proj(l)
            if stop_after == "proj":
                break
            phase_exchange()
            if stop_after == "exch":
                break
        phase_attn(l)
        if stop_after == "attn":
            break
        phase_ffn(l, nd, nm, l == L - 1)
        if lt == 0:
            nd += 1
        else:
            nm += 1
    S.emit()
    return nc


def _consts():
    k = np.arange(128)[:, None]
    q = np.arange(128)[None, :]
    ident = (k == q).astype(np.float32)
    src = (q // 64) * 64 + ((q % 64) + 32) % 64
    perm = (k == src).astype(np.float32)
    mask = np.stack([(q >= k), (k >= q), (k > q)], axis=1).astype(np.float32)
    p = np.arange(128)
    invf = (10000.0 ** (-(p % 32).astype(np.float64) / 32.0)).astype(np.float32)
    sgn = np.where((p % 64) < 32, -1.0, 1.0).astype(np.float32)
    col = np.stack([invf, sgn], axis=1).astype(np.float32)
    return ident, perm, np.ascontiguousarray(mask), np.ascontiguousarray(col)


_CACHE = {}


def run_layers(inputs, layer_types, debug_out=(), stop_after=None):
    L = len(layer_types)
    key = (tuple(layer_types), tuple(debug_out), stop_after)
    if key not in _CACHE:
        _CACHE[key] = build_program(list(layer_types), debug_out=debug_out, stop_after=stop_after)
    nc = _CACHE[key]
    ident, perm, mask, col = _consts()
    f32 = lambda a: np.ascontiguousarray(np.asarray(a, dtype=np.float32))
    x = f32(inputs["x"])
    pos = np.ascontiguousarray(np.asarray(inputs["positions"], dtype=np.int32))
    shared = {
        "c_ident": ident, "c_perm": perm, "c_mask": mask, "c_col": col,
        "attn_norm": f32(inputs["attn_norm"])[:L], "w_in": f32(inputs["w_in"])[:L],
        "mix_norm_a": f32(inputs["mix_norm_a"])[:L], "mix_norm_b": f32(inputs["mix_norm_b"])[:L],
        "sinks": f32(inputs["sinks"])[:L].reshape(1, L * 8), "w_out": f32(inputs["w_out"])[:L],
        "ffn_norm": f32(inputs["ffn_norm"])[:L], "final_norm": f32(inputs["final_norm"]).reshape(1, D),
    }
    nd = sum(1 for t in layer_types if t == 0)
    nm = sum(1 for t in layer_types if t == 1)
    if nd:
        shared["dense_w_gate"] = f32(inputs["dense_w_gate"])[:nd]
        shared["dense_w_up"] = f32(inputs["dense_w_up"])[:nd]
        shared["dense_w_down"] = f32(inputs["dense_w_down"])[:nd]
    if nm:
        shared["router"] = f32(inputs["router"])[:nm]
        shared["moe_w_gate"] = f32(inputs["moe_w_gate"])[:nm]
        shared["moe_w_up"] = f32(inputs["moe_w_up"])[:nm]
        shared["moe_w_down"] = f32(inputs["moe_w_down"])[:nm]
    in_maps = []
    for c in range(NCORES):
        b, h = c // 2, c % 2
        m = dict(shared)
        m["x"] = np.ascontiguousarray(x[b, h * T:(h + 1) * T, :])
        m["pos"] = np.ascontiguousarray(pos[h * T:(h + 1) * T][None, :])
        m["flag"] = np.full((128, 1), float(h), dtype=np.float32)
        in_maps.append(m)
    res = run_bass_kernel_spmd(nc, in_maps, core_ids=list(range(NCORES)))
    outp = np.empty((4, 2 * T, D), dtype=np.float32)
    for c in range(NCORES):
        b, h = c // 2, c % 2
        outp[b, h * T:(h + 1) * T, :] = np.asarray(res.results[c]["out"], dtype=np.float32)
    if debug_out:
        return outp, res.results
    return outp


def kernel(**inputs):
    return run_layers(inputs, [0, 1, 0, 1])
```

```python
import contextlib
import os
import numpy as np
import concourse.bass as bass
import concourse.mybir as mybir
from concourse.bass_utils import run_bass_kernel_spmd

F32 = mybir.dt.float32
BF16 = mybir.dt.bfloat16
I32 = mybir.dt.int32
AF = mybir.ActivationFunctionType
ALU = mybir.AluOpType
AX = mybir.AxisListType

ENGS = ("pe", "act", "dve", "pool", "sp")

NCORES = 8
T = 4096
TH = 2048
TE = T + TH
D = 1024
KC = 8
FF = 3584
NJ = FF // 128
NE = 8
INW = 2304
EPS = 1e-5
DIL = (1, 4, 16)


class Op:
    __slots__ = ("eng", "fn", "deps", "signal", "count", "dma_key", "is_dma", "inc")

    def __init__(self, eng, fn, dma_key, inc=16):
        self.eng = eng
        self.fn = fn
        self.deps = []
        self.signal = False
        self.count = None
        self.dma_key = dma_key
        self.is_dma = dma_key is not None
        self.inc = inc


class Sched:
    def __init__(self, nc, same_engine_sync=True):
        self.nc = nc
        self.ops = {e: [] for e in ENGS}
        self.last_w = {}
        self.readers = {}
        self.dma_counts = {}
        self.same_engine_sync = same_engine_sync

    def op(self, eng, fn, reads=(), writes=(), dma_key=None, inc=16):
        o = Op(eng, fn, dma_key, inc)
        deps = []
        for t in reads:
            w = self.last_w.get(t)
            if w is not None:
                deps.append((w, 0))
        for t in writes:
            w = self.last_w.get(t)
            if w is not None:
                deps.append((w, 1))
            for r in self.readers.get(t, ()):
                deps.append((r, 2))
        seen = set()
        for p, kind in deps:
            if id(p) in seen:
                continue
            if (not p.is_dma) and p.eng == eng:
                if eng == "pe" or kind == 2 or not self.same_engine_sync:
                    continue
            seen.add(id(p))
            o.deps.append(p)
            if not p.is_dma:
                p.signal = True
        for t in reads:
            self.readers.setdefault(t, []).append(o)
        for t in writes:
            self.last_w[t] = o
            self.readers[t] = []
        if o.is_dma:
            c = self.dma_counts.get(dma_key, 0) + 1
            self.dma_counts[dma_key] = c
            o.count = c
        self.ops[eng].append(o)
        return o

    def barrier(self):
        lasts = []
        dma_last = {}
        for e in ENGS:
            lst = [o for o in self.ops[e] if not o.is_dma and o.fn is not None]
            if lst:
                lasts.append(lst[-1])
            for o in self.ops[e]:
                if o.is_dma:
                    dma_last[o.dma_key] = o
        for e in ENGS:
            o = Op(e, None, None)
            for p in lasts:
                if p.eng != e:
                    o.deps.append(p)
                    p.signal = True
            o.deps.extend(dma_last.values())
            self.ops[e].append(o)
        self.last_w = {}
        self.readers = {}

    def emit(self):
        nc = self.nc
        with contextlib.ExitStack() as st:
            esem = {e: st.enter_context(nc.semaphore("s_" + e)) for e in ENGS}
            dsem = {k: st.enter_context(nc.semaphore("d_%d" % i)) for i, k in enumerate(self.dma_counts)}
            block = st.enter_context(nc.Block())
            for e in ENGS:
                c = 0
                for o in self.ops[e]:
                    if (not o.is_dma) and o.signal:
                        c += 1
                        o.count = c

            def event(p):
                if p.is_dma:
                    return dsem[p.dma_key], p.inc * p.count
                return esem[p.eng], p.count

            def run(e, h):
                waited = {}
                for o in self.ops[e]:
                    for p in o.deps:
                        s, v = event(p)
                        if waited.get(id(s), 0) >= v:
                            continue
                        waited[id(s)] = v
                        h.wait_ge(s, v)
                    if o.fn is None:
                        continue
                    ins = o.fn(h)
                    if o.is_dma:
                        if o.inc == 1:
                            ins.then_inc(dsem[o.dma_key])
                        else:
                            ins.then_inc(dsem[o.dma_key], o.inc)
                    elif o.signal:
                        ins.then_inc(esem[e], 1)

            block.tensor(lambda h: run("pe", h))
            block.scalar(lambda h: run("act", h))
            block.vector(lambda h: run("dve", h))
            block.gpsimd(lambda h: run("pool", h))
            block.sync(lambda h: run("sp", h))


class Arena:
    def __init__(self, nc, nbytes=206 * 1024):
        self.big = nc.alloc_sbuf_tensor("arena", [128, nbytes], mybir.dt.uint8)
        self.off = 0
        self.limit = nbytes

    def alloc(self, shape, dtype):
        esz = 2 if dtype == BF16 else 4
        n = int(np.prod(shape[1:]))
        off = (self.off + 63) // 64 * 64
        assert off + n * esz <= self.limit, ("SBUF arena overflow", off, n * esz, self.limit)
        self.off = off + n * esz
        ap = self.big[0:shape[0], off:off + n * esz].bitcast(dtype)
        if len(shape) == 3:
            ap = ap.rearrange("p (a b) -> p a b", b=shape[2])
        elif len(shape) == 4:
            ap = ap.rearrange("p (a b c) -> p a b c", b=shape[2], c=shape[3])
        return ap

    def mark(self):
        return self.off

    def release(self, m):
        self.off = m


def build_program(layer_types, groups=None, stop_after=None, debug_out=()):
    L = len(layer_types)
    ND = max(1, sum(1 for t in layer_types if t == 0))
    NM = max(1, sum(1 for t in layer_types if t == 1))
    if groups is None:
        groups = [[0, 1], [2, 3], [4, 5], [6, 7]]
    nc = bass.Bass("TRN2", target_bir_lowering=False)
    S = Sched(nc)
    A = Arena(nc)

    def din(name, shape, dt=F32):
        return nc.dram_tensor(name, list(shape), dt, kind="ExternalInput").ap()

    x_in = din("x", [T, D])
    pos_in = din("pos", [1, T], I32)
    flag_in = din("flag", [128, 1])
    cident_in = din("c_ident", [128, 128])
    cperm_in = din("c_perm", [128, 128])
    cmask_in = din("c_mask", [128, 3, 128])
    ccol_in = din("c_col", [128, 2])
    attn_norm = din("attn_norm", [L, D])
    w_in = din("w_in", [L, D, INW])
    mix_a = din("mix_norm_a", [L, 512])
    mix_b = din("mix_norm_b", [L, 512])
    sinks = din("sinks", [1, L * 8])
    w_out = din("w_out", [L, D, D])
    ffn_norm = din("ffn_norm", [L, D])
    if 0 in layer_types:
        dwg = din("dense_w_gate", [ND, D, FF])
        dwu = din("dense_w_up", [ND, D, FF])
        dwd = din("dense_w_down", [ND, FF, D])
    if 1 in layer_types:
        router = din("router", [NM, D, NE])
        mwg = din("moe_w_gate", [NM, NE, D, FF])
        mwu = din("moe_w_up", [NM, NE, D, FF])
        mwd = din("moe_w_down", [NM, NE, FF, D])
    fin_norm = din("final_norm", [1, D])
    out = nc.dram_tensor("out", [T, D], F32, kind="ExternalOutput").ap()

    def dscr(name, shape, dt):
        if name in debug_out:
            return nc.dram_tensor(name, list(shape), dt, kind="ExternalOutput").ap()
        return nc.dram_tensor(name, list(shape), dt).ap()

    xres = dscr("xres", [T, D], F32)
    qT = dscr("qT", [128, 8, T], BF16)
    kT = dscr("kT", [128, 5, TE], BF16)
    vbuf = dscr("vbuf", [TE, 640], BF16)
    oT = dscr("oT", [128, 8, T], BF16)
    cs = dscr("cs", [128, 2, T], F32)
    sendk = dscr("sendk", [5, 128, TH], BF16)
    recvk = dscr("recvk", [5, 256, TH], BF16)
    sendv = dscr("sendv", [2, 1024, 640], BF16)
    recvv = dscr("recvv", [2, 2048, 640], BF16)

    ps = [nc.alloc_psum_tensor("ps%d" % i, [128, 512], F32)[:, :] for i in range(8)]

    ident = A.alloc([128, 128], F32)
    ones_bf = A.alloc([128, 128], BF16)
    perm_bf = A.alloc([128, 128], BF16)
    maskf = A.alloc([128, 3, 128], F32)
    flagc = A.alloc([128, 1], F32)
    ccol = A.alloc([128, 2], F32)
    NG = 24
    gcol = A.alloc([128, L * NG], F32)
    esink = A.alloc([128, L * 8], F32)
    sinkcol = A.alloc([128, L * 4], F32)
    epsc = A.alloc([128, 1], F32)
    negpi = A.alloc([128, 1], F32)

    def dma(q, out_ap, in_ap, reads=(), writes=(), key=None, **kw):
        return S.op(q, lambda e: e.dma_start(out=out_ap, in_=in_ap, **kw), reads=reads, writes=writes, dma_key=key)

    m0 = A.mark()
    permf = A.alloc([128, 128], F32)
    dma("sp", ident, cident_in, writes=["ident"], key="c0")
    dma("sp", permf, cperm_in, writes=["permf"], key="c1")
    dma("sp", maskf, cmask_in, writes=["maskf"], key="c2")
    dma("sp", flagc, flag_in, writes=["flagc"], key="c3")
    dma("sp", ccol, ccol_in, writes=["ccol"], key="c4")
    dma("sp", esink, sinks.partition_broadcast(128), writes=["esink"], key="c5")
    for l in range(L):
        b0 = l * NG
        dma("sp", gcol[:, b0:b0 + 8], attn_norm[l].rearrange("(k p) -> p k", p=128), writes=["gcol%d" % l],
            key="g0", allow_slow_non_contiguous=True)
        dma("sp", gcol[:, b0 + 8:b0 + 16], ffn_norm[l].rearrange("(k p) -> p k", p=128), writes=["gcol%d" % l],
            key="g0", allow_slow_non_contiguous=True)
        dma("sp", gcol[:, b0 + 16:b0 + 20], mix_a[l].rearrange("(k p) -> p k", p=128), writes=["gcol%d" % l],
            key="g0", allow_slow_non_contiguous=True)
        dma("sp", gcol[:, b0 + 20:b0 + 24], mix_b[l].rearrange("(k p) -> p k", p=128), writes=["gcol%d" % l],
            key="g0", allow_slow_non_contiguous=True)
    S.op("pool", lambda e: e.memset(ones_bf, 1.0), writes=["ones"])
    S.op("pool", lambda e: e.memset(epsc, EPS), writes=["epsc"])
    S.op("pool", lambda e: e.memset(negpi, -3.1415925), writes=["negpi"])
    S.op("dve", lambda e: e.tensor_copy(out=perm_bf, in_=permf), reads=["permf"], writes=["perm"])
    S.op("act", lambda e: e.activation(out=esink, in_=esink, func=AF.Exp), reads=["esink"], writes=["esink"])
    for l in range(L):
        for j in range(4):
            c0 = l * 8 + 2 * j
            S.op("dve", lambda e, c0=c0, l=l, j=j: e.tensor_copy(out=sinkcol[0:64, l * 4 + j:l * 4 + j + 1],
                                                              in_=esink[0:64, c0:c0 + 1]),
                 reads=["esink"], writes=["sinkcol"])
            S.op("dve", lambda e, c0=c0, l=l, j=j: e.tensor_copy(out=sinkcol[64:128, l * 4 + j:l * 4 + j + 1],
                                                              in_=esink[64:128, c0 + 1:c0 + 2]),
                 reads=["esink"], writes=["sinkcol"])
    CH = 1024
    posi = A.alloc([128, CH], I32)
    posf = A.alloc([128, CH], F32)
    tt_ = A.alloc([128, CH], F32)
    kf_ = A.alloc([128, CH], F32)
    ki_ = A.alloc([128, CH], I32)
    msk_ = A.alloc([128, CH], F32)
    tab = A.alloc([128, 2, CH], F32)
    INV2PI = float(1.0 / (2.0 * np.pi))
    for ch in range(T // CH):
        dma("sp", posi, pos_in[:, ch * CH:(ch + 1) * CH].partition_broadcast(128), writes=["posi"], key="posi")
        S.op("dve", lambda e: e.tensor_copy(out=posf, in_=posi), reads=["posi"], writes=["posf"])
        S.op("dve", lambda e: e.tensor_scalar(out=posf, in0=posf, scalar1=ccol[:, 0:1], scalar2=INV2PI,
                                              op0=ALU.mult, op1=ALU.mult), reads=["posf", "ccol"], writes=["posf"])
        for which, off in ((1, 0.5), (0, 0.75)):
            S.op("dve", lambda e, off=off: e.tensor_scalar(out=tt_, in0=posf, scalar1=float(off), scalar2=None,
                                                           op0=ALU.add), reads=["posf"], writes=["tt"])
            S.op("dve", lambda e: e.tensor_copy(out=ki_, in_=tt_), reads=["tt"], writes=["ki"])
            S.op("dve", lambda e: e.tensor_copy(out=kf_, in_=ki_), reads=["ki"], writes=["kf"])
            S.op("dve", lambda e: e.tensor_tensor(out=tt_, in0=tt_, in1=kf_, op=ALU.subtract), reads=["tt", "kf"], writes=["tt"])
            S.op("dve", lambda e: e.tensor_scalar(out=msk_, in0=tt_, scalar1=0.0, scalar2=None, op0=ALU.is_lt),
                 reads=["tt"], writes=["msk"])
            S.op("dve", lambda e: e.tensor_tensor(out=tt_, in0=tt_, in1=msk_, op=ALU.add), reads=["tt", "msk"], writes=["tt"])
            S.op("act", lambda e, which=which: e.activation(out=tab[:, which, :], in_=tt_, func=AF.Sin,
                                                            bias=negpi[:, 0:1], scale=6.283185),
                 reads=["tt", "negpi"], writes=["tab%d" % which])
        S.op("dve", lambda e: e.tensor_scalar(out=tab[:, 1, :], in0=tab[:, 1, :], scalar1=ccol[:, 1:2], scalar2=None,
                                              op0=ALU.mult), reads=["tab1", "ccol"], writes=["tab1"])
        dma("sp", cs[:, :, ch * CH:(ch + 1) * CH], tab, reads=["tab0", "tab1"], writes=["cs"], key="cs")
    S.barrier()
    A.release(m0)

    def rms_rstd(ssq_ap, rt_ap, rstd_ap, n, reads, wtok):
        S.op("act", lambda e: e.activation(out=rt_ap, in_=ssq_ap, func=AF.Sqrt, bias=epsc[:, 0:1], scale=1.0 / n),
             reads=list(reads) + ["epsc"], writes=[wtok + "_rt"])
        S.op("dve", lambda e: e.reciprocal(out=rstd_ap, in_=rt_ap), reads=[wtok + "_rt"], writes=[wtok])

    def phase_proj(l):
        mk = A.mark()
        win = A.alloc([128, KC, INW], BF16)
        for kc in range(KC):
            dma("pool", win[:, kc, :], w_in[l][kc * 128:(kc + 1) * 128, :],
                writes=["win%d" % (kc // 2)], key="win%d" % (kc // 2), max_dma_last_dim=4096)
        xt = [A.alloc([128, 4, D], F32) for _ in range(2)]
        xn = A.alloc([128, 4, D], F32)
        junk = A.alloc([128, D], BF16)
        ssq = A.alloc([128, 4], F32)
        rt = A.alloc([128, 4], F32)
        rstd = A.alloc([128, 4], F32)
        hT = [A.alloc([128, KC, 512], BF16) for _ in range(2)]
        cst = [A.alloc([128, 2, 512], F32) for _ in range(2)]
        tb = [A.alloc([128, 512], BF16) for _ in range(2)]
        ra = [A.alloc([128, 512], F32) for _ in range(2)]
        rb = [A.alloc([128, 512], F32) for _ in range(2)]
        qkst = A.alloc([128, 13, 512], BF16)
        vst = A.alloc([128, 4, 640], BF16)
        xsrc = x_in if l == 0 else xres
        g0 = l * NG
        colstarts = [0, 128, 256, 384, 1536, 1664, 1792, 1920, 512, 640, 768, 896, 2048]
        NT = T // 512

        def load_i(it, tt):
            b = it % 2
            dma("sp", xt[b], xsrc[tt * 512:(tt + 1) * 512, :].rearrange("(b p) d -> p b d", p=128),
                writes=["xt%d" % b], key="xt%d" % b)
            dma("sp", cst[b], cs[:, :, tt * 512:(tt + 1) * 512], writes=["cst%d" % b], key="cst%d" % b)

        order = [4, 5, 6, 7, 0, 1, 2, 3]
        load_i(0, order[0])
        for it, tt in enumerate(order):
            b = it % 2
            if it + 1 < NT:
                load_i(it + 1, order[it + 1])
            X = xt[b]
            H = hT[b]
            for bb in range(4):
                S.op("act", lambda e, bb=bb, X=X: e.activation(out=junk, in_=X[:, bb, :], func=AF.Square,
                                                               accum_out=ssq[:, bb:bb + 1]),
                     reads=["xt%d" % b], writes=["ssq%d" % bb])
            rms_rstd(ssq, rt, rstd, D, ["ssq%d" % i for i in range(4)], "rstd")
            for bb in range(4):
                S.op("dve", lambda e, bb=bb, X=X: e.tensor_scalar(out=xn[:, bb, :], in0=X[:, bb, :],
                                                                  scalar1=rstd[:, bb:bb + 1], scalar2=None, op0=ALU.mult),
                     reads=["xt%d" % b, "rstd"], writes=["xn%d" % bb])
            for kc in range(KC):
                pt = ps[kc % 2]
                for bb in range(4):
                    S.op("pe", lambda e, bb=bb, kc=kc, pt=pt: e.transpose(pt[:, bb * 128:(bb + 1) * 128],
                                                                          xn[:, bb, kc * 128:(kc + 1) * 128], ident),
                         reads=["xn%d" % bb, "ident"], writes=["ps%d" % (kc % 2)])
                eng = "act" if kc % 2 == 0 else "dve"
                if eng == "act":
                    S.op("act", lambda e, kc=kc, pt=pt, H=H: e.activation(out=H[:, kc, :], in_=pt, func=AF.Copy,
                                                                          scale=gcol[:, g0 + kc:g0 + kc + 1]),
                         reads=["ps%d" % (kc % 2), "gcol%d" % l], writes=["hT%d_%d" % (b, kc)])
                else:
                    S.op("dve", lambda e, kc=kc, pt=pt, H=H: e.tensor_scalar(out=H[:, kc, :], in0=pt,
                                                                             scalar1=gcol[:, g0 + kc:g0 + kc + 1],
                                                                             scalar2=None, op0=ALU.mult),
                         reads=["ps%d" % (kc % 2), "gcol%d" % l], writes=["hT%d_%d" % (b, kc)])
            hreads = ["hT%d_%d" % (b, kc) for kc in range(KC)]

            def rope(si, b=b):
                a = si % 2
                cb = cst[b]
                S.op("pe", lambda e, a=a: e.matmul(ps[4 + a][:, :], lhsT=perm_bf, rhs=tb[a], start=True, stop=True),
                     reads=["tb%d" % a, "perm"], writes=["ps%d" % (4 + a)])
                S.op("dve", lambda e, a=a, cb=cb: e.tensor_tensor(out=ra[a], in0=tb[a], in1=cb[:, 0, :], op=ALU.mult),
                     reads=["tb%d" % a, "cst%d" % b], writes=["ra%d" % a])
                S.op("dve", lambda e, a=a, cb=cb: e.tensor_tensor(out=rb[a], in0=ps[4 + a][:, :], in1=cb[:, 1, :], op=ALU.mult),
                     reads=["ps%d" % (4 + a), "cst%d" % b], writes=["rb%d" % a])
                S.op("pool", lambda e, a=a, si=si: e.tensor_tensor(out=qkst[:, si, :], in0=ra[a], in1=rb[a], op=ALU.add),
                     reads=["ra%d" % a, "rb%d" % a], writes=["qkst%d" % si])

            pending = None
            for si, c0 in enumerate(colstarts):
                a = si % 2
                for kc in range(KC):
                    S.op("pe", lambda e, a=a, kc=kc, c0=c0, H=H: e.matmul(ps[2 + a][:, :], lhsT=win[:, kc, c0:c0 + 128],
                                                                          rhs=H[:, kc, :], start=(kc == 0), stop=(kc == KC - 1)),
                         reads=[hreads[kc], "win%d" % (kc // 2)], writes=["ps%d" % (2 + a)])
                S.op("act", lambda e, a=a: e.copy(out=tb[a], in_=ps[2 + a][:, :]), reads=["ps%d" % (2 + a)], writes=["tb%d" % a])
                if pending is not None:
                    rope(pending)
                pending = si
            rope(pending)
            for bb in range(4):
                pv = ps[6 + bb % 2]
                for kc in range(KC):
                    S.op("pe", lambda e, bb=bb, kc=kc, pv=pv, H=H: e.matmul(pv[:, :], lhsT=H[:, kc, bb * 128:(bb + 1) * 128],
                                                                            rhs=win[:, kc, 1024:1536], start=(kc == 0), stop=(kc == KC - 1)),
                         reads=[hreads[kc], "win%d" % (kc // 2)], writes=["ps%d" % (6 + bb % 2)])
                S.op("act", lambda e, bb=bb, pv=pv: e.copy(out=vst[:, bb, 0:512], in_=pv[:, :]),
                     reads=["ps%d" % (6 + bb % 2)], writes=["vst%d" % bb])
                pw = ps[4 + bb % 2]
                for kc in range(KC):
                    S.op("pe", lambda e, bb=bb, kc=kc, pw=pw, H=H: e.matmul(pw[:, 0:128], lhsT=H[:, kc, bb * 128:(bb + 1) * 128],
                                                                            rhs=win[:, kc, 2176:2304], start=(kc == 0), stop=(kc == KC - 1)),
                         reads=[hreads[kc], "win%d" % (kc // 2)], writes=["ps%d" % (4 + bb % 2)])
                S.op("dve", lambda e, bb=bb, pw=pw: e.tensor_copy(out=vst[:, bb, 512:640], in_=pw[:, 0:128]),
                     reads=["ps%d" % (4 + bb % 2)], writes=["vst%d" % bb])
            dma("sp", qT[:, :, tt * 512:(tt + 1) * 512], qkst[:, 0:8, :], reads=["qkst%d" % i for i in range(8)],
                writes=["qT"], key="qTst")
            dma("sp", kT[:, :, TH + tt * 512:TH + (tt + 1) * 512], qkst[:, 8:13, :],
                reads=["qkst%d" % i for i in range(8, 13)], writes=["kT"], key="kTst")
            dma("sp", vbuf[TH + tt * 512:TH + (tt + 1) * 512, :].rearrange("(b p) c -> p b c", p=128), vst,
                reads=["vst%d" % i for i in range(4)], writes=["vbuf"], key="vst")
            if it == 3:
                phase_exchange()
        phase_exchange_recv()
        S.barrier()
        A.release(mk)

    def phase_exchange():
        for c in range(5):
            dma("sp", sendk[c], kT[:, c, T:TE], reads=["kT"], writes=["sendk%d" % c], key="ex0")
        for u in range(2):
            dma("sp", sendv[u], vbuf[T + u * 1024:T + (u + 1) * 1024, :], reads=["vbuf"], writes=["sendv%d" % u], key="ex1")
        for c in range(5):
            S.op("pool", lambda e, c=c: e.collective_compute("AllGather", ALU.bypass, replica_groups=groups,
                                                             ins=[sendk[c].opt()], outs=[recvk[c].opt()]),
                 reads=["sendk%d" % c], writes=["recvk%d" % c], dma_key="cc", inc=1)
        for u in range(2):
            S.op("pool", lambda e, u=u: e.collective_compute("AllGather", ALU.bypass, replica_groups=groups,
                                                             ins=[sendv[u].opt()], outs=[recvv[u].opt()]),
                 reads=["sendv%d" % u], writes=["recvv%d" % u], dma_key="cc", inc=1)

    def phase_exchange_recv():
        for c in range(5):
            dma("sp", kT[:, c, 0:TH], recvk[c, 0:128, :], reads=["recvk%d" % c], writes=["kT"], key="ex2")
        for u in range(2):
            dma("sp", vbuf[u * 1024:(u + 1) * 1024, :], recvv[u, 0:1024, :], reads=["recvv%d" % u], writes=["vbuf"], key="ex3")

    def phase_attn(l):
        mk = A.mark()
        kct = [A.alloc([128, TE], BF16) for _ in range(2)]
        qct = [A.alloc([128, T], BF16) for _ in range(2)]
        vA = {d: A.alloc([128, TE // 128, 128], BF16) for d in DIL}
        vB = A.alloc([128, TE // 128, 64], BF16)
        accn = A.alloc([128, T], F32)
        accd = A.alloc([128, T], F32)
        rden = A.alloc([128, T], F32)
        oc = A.alloc([128, T], BF16)
        P = [A.alloc([128, 2, 256], BF16) for _ in range(2)]
        M4 = {}
        for nm, prev_idx in (("A", 1), ("B", 2)):
            for halo in (0, 1):
                mt = A.alloc([128, 2, 256], BF16)
                M4[(nm, halo)] = mt
                for h in range(2):
                    if halo:
                        S.op("dve", lambda e, mt=mt, h=h, prev_idx=prev_idx: e.tensor_scalar(
                            out=mt[:, h, 0:128], in0=maskf[:, prev_idx, :], scalar1=flagc[:, 0:1], scalar2=None, op0=ALU.mult),
                             reads=["maskf", "flagc"], writes=["M4"])
                    else:
                        S.op("dve", lambda e, mt=mt, h=h, prev_idx=prev_idx: e.tensor_copy(out=mt[:, h, 0:128], in_=maskf[:, prev_idx, :]),
                             reads=["maskf"], writes=["M4"])
                    S.op("dve", lambda e, mt=mt, h=h: e.tensor_copy(out=mt[:, h, 128:256], in_=maskf[:, 0, :]),
                         reads=["maskf"], writes=["M4"])
        ps_sa = [[ps[0], ps[1]], [ps[6], ps[7]]]

        def load_chunk(c):
            b = c % 2
            if c < 4:
                dma("sp", kct[b], kT[:, c, :], writes=["kct%d" % b], key="kct%d" % b)
            else:
                g = (c - 4) // 2
                dma("sp", kct[b][0:64, :], kT[g * 64:(g + 1) * 64, 4, :], writes=["kct%d" % b], key="kct%d" % b)
                dma("sp", kct[b][64:128, :], kT[g * 64:(g + 1) * 64, 4, :], writes=["kct%d" % b], key="kct%d" % b)
            dma("sp", qct[b], qT[:, c, :], writes=["qct%d" % b], key="qct%d" % b)

        def load_v(c, d):
            if c < 4:
                cols = slice(c * 128, (c + 1) * 128)
                if d == 1:
                    for u in range(TE // 2048):
                        src = vbuf[u * 2048:(u + 1) * 2048, cols].rearrange("(s i) c -> i s c", i=128)
                        dma("sp", vA[d][:, u * 16:(u + 1) * 16, :], src, writes=["vA%d" % d], key="vA%d" % d)
                else:
                    seg = 128 * d
                    for sg_ in range(TE // seg):
                        src = vbuf[sg_ * seg:(sg_ + 1) * seg, cols].rearrange("(i r) c -> i r c", r=d)
                        dma("sp", vA[d][:, sg_ * d:(sg_ + 1) * d, :], src, writes=["vA%d_%d" % (d, sg_ % 2)], key="vA%d_%d" % (d, sg_ % 2))
            else:
                g = (c - 4) // 2
                for u in range(TE // 2048):
                    src = vbuf[u * 2048:(u + 1) * 2048, 512 + g * 64:512 + (g + 1) * 64].rearrange("(s i) c -> i s c", i=128)
                    dma("sp", vB[:, u * 16:(u + 1) * 16, :], src, writes=["vB"], key="vB")

        DBG_NCH = int(os.environ.get("ATT_NCH", "8"))
        DBG_BR = tuple(int(v) for v in os.environ.get("ATT_BR", "1,4,16").split(","))
        DBG_NU = int(os.environ.get("ATT_NU", "9999"))
        load_chunk(0)
        for d in DIL:
            load_v(0, d)
        load_v(4, 1)
        for c in range(DBG_NCH):
            b = c % 2
            grpA = c < 4
            branches = tuple(d for d in DIL if d in DBG_BR) if grpA else (1,)
            KCt, QCt = kct[b], qct[b]
            first_branch = True
            for d in branches:
                nseg = T // (128 * d)
                Sh = TH // (128 * d)
                Qr = QCt.rearrange("p (s i r) -> p s r i", i=128, r=d)
                Kr = KCt.rearrange("p (s i r) -> p s r i", i=128, r=d)
                accn_r = accn.rearrange("p (s i r) -> p s r i", i=128, r=d)
                accd_r = accd.rearrange("p (s i r) -> p s r i", i=128, r=d)
                if grpA:
                    Vt = vA[d]
                    vtok = ["vA%d" % d] if d == 1 else ["vA%d_0" % d, "vA%d_1" % d]
                else:
                    Vt = vB
                    vtok = ["vB"]
                if d == 1:
                    groups_ = [[(s, 0) for s in range(g4 * 4, g4 * 4 + 4)] for g4 in range(nseg // 4)]
                else:
                    groups_ = [[(s, r) for r in range(r0, r0 + 4)] for s in range(nseg) for r0 in range(0, d, 4)]
                units = []
                for gi, g in enumerate(groups_):
                    for j, (s, r) in enumerate(g):
                        units.append((gi, j, s, r))

                def scores(n, u):
                    gi, j, s, r = u
                    alt = n % 2
                    Qv = Qr[:, s, r, :]
                    Kp = Kr[:, s + Sh - 1, r, :]
                    Kc_ = Kr[:, s + Sh, r, :]
                    for h in range(2):
                        lo, hi = h * 64, (h + 1) * 64
                        S.op("pe", lambda e, h=h, lo=lo, hi=hi, Kp=Kp, Qv=Qv, alt=alt: e.matmul(
                            ps_sa[alt][h][:, 0:128], lhsT=Kp[lo:hi, :], rhs=Qv[lo:hi, :], start=True, stop=True),
                             reads=["kct%d" % b, "qct%d" % b], writes=["pss%d" % alt])
                        S.op("pe", lambda e, h=h, lo=lo, hi=hi, Kc_=Kc_, Qv=Qv, alt=alt: e.matmul(
                            ps_sa[alt][h][:, 128:256], lhsT=Kc_[lo:hi, :], rhs=Qv[lo:hi, :], start=True, stop=True),
                             reads=["kct%d" % b, "qct%d" % b], writes=["pss%d" % alt])

                def rest(n, u):
                    gi, j, s, r = u
                    alt = n % 2
                    Pt = P[alt]
                    pn = ps[2 + gi % 2]
                    pd = ps[4 + gi % 2]
                    for h in range(2):
                        S.op("act", lambda e, h=h, Pt=Pt, alt=alt: e.activation(out=Pt[:, h, :], in_=ps_sa[alt][h][:, 0:256],
                                                                               func=AF.Exp, scale=0.125),
                             reads=["pss%d" % alt], writes=["P%d" % alt])
                    mt = M4[("A" if grpA else "B", 1 if s == 0 else 0)]
                    S.op("pool", lambda e, Pt=Pt, mt=mt: e.tensor_tensor(out=Pt, in0=Pt, in1=mt, op=ALU.mult),
                         reads=["P%d" % alt, "M4"], writes=["P%d" % alt])
                    nprev = (s + Sh - 1) * d + r
                    ncur = (s + Sh) * d + r
                    for h in range(2):
                        lo, hi = h * 64, (h + 1) * 64
                        if grpA:
                            Vp, Vc = Vt[:, nprev, lo:hi], Vt[:, ncur, lo:hi]
                        else:
                            Vp, Vc = Vt[:, nprev, :], Vt[:, ncur, :]
                        cols = slice(j * 128, (j + 1) * 128)
                        S.op("pe", lambda e, Vp=Vp, Pt=Pt, h=h, lo=lo, hi=hi, cols=cols, pn=pn: e.matmul(
                            pn[lo:hi, cols], lhsT=Vp, rhs=Pt[:, h, 0:128], start=True, stop=False),
                             reads=["P%d" % alt] + vtok, writes=["ps%d" % (2 + gi % 2)])
                        S.op("pe", lambda e, Vc=Vc, Pt=Pt, h=h, lo=lo, hi=hi, cols=cols, pn=pn: e.matmul(
                            pn[lo:hi, cols], lhsT=Vc, rhs=Pt[:, h, 128:256], start=False, stop=True),
                             reads=["P%d" % alt] + vtok, writes=["ps%d" % (2 + gi % 2)])
                        S.op("pe", lambda e, Pt=Pt, h=h, lo=lo, hi=hi, cols=cols, pd=pd: e.matmul(
                            pd[lo:hi, cols], lhsT=ones_bf[:, 0:64], rhs=Pt[:, h, 0:128], start=True, stop=False),
                             reads=["P%d" % alt, "ones"], writes=["ps%d" % (4 + gi % 2)])
                        S.op("pe", lambda e, Pt=Pt, h=h, lo=lo, hi=hi, cols=cols, pd=pd: e.matmul(
                            pd[lo:hi, cols], lhsT=ones_bf[:, 0:64], rhs=Pt[:, h, 128:256], start=False, stop=True),
                             reads=["P%d" % alt, "ones"], writes=["ps%d" % (4 + gi % 2)])
                    if j == 3 and not os.environ.get("ATT_NOACC"):
                        g = groups_[gi]
                        s0, r0 = g[0]
                        if d == 1:
                            an = accn[:, s0 * 128:(s0 + 4) * 128]
                            ad = accd[:, s0 * 128:(s0 + 4) * 128]
                            pnv, pdv = pn[:, :], pd[:, :]
                        else:
                            an = accn_r[:, s0, r0:r0 + 4, :]
                            ad = accd_r[:, s0, r0:r0 + 4, :]
                            pnv = pn[:, :].rearrange("p (r i) -> p r i", i=128)
                            pdv = pd[:, :].rearrange("p (r i) -> p r i", i=128)
                        if first_branch:
                            S.op("dve", lambda e, an=an, pnv=pnv: e.tensor_copy(out=an, in_=pnv),
                                 reads=["ps%d" % (2 + gi % 2)], writes=["accn"])
                            S.op("dve", lambda e, ad=ad, pdv=pdv: e.tensor_copy(out=ad, in_=pdv),
                                 reads=["ps%d" % (4 + gi % 2)], writes=["accd"])
                        else:
                            S.op("dve", lambda e, an=an, pnv=pnv: e.tensor_tensor(out=an, in0=an, in1=pnv, op=ALU.add),
                                 reads=["ps%d" % (2 + gi % 2), "accn"], writes=["accn"])
                            S.op("dve", lambda e, ad=ad, pdv=pdv: e.tensor_tensor(out=ad, in0=ad, in1=pdv, op=ALU.add),
                                 reads=["ps%d" % (4 + gi % 2), "accd"], writes=["accd"])

                units = units[:DBG_NU]
                NOPIPE = bool(os.environ.get("ATT_NOPIPE"))
                if units and not NOPIPE:
                    scores(0, units[0])
                for n, u in enumerate(units):
                    if NOPIPE:
                        scores(n, u)
                    elif n + 1 < len(units):
                        scores(n + 1, units[n + 1])
                    rest(n, u)
                first_branch = False
                if c + 1 < 8 and d == branches[0]:
                    load_chunk(c + 1)
                if c + 1 < 4:
                    load_v(c + 1, d)
            if c == 5:
                load_v(6, 1)
            if not grpA:
                j4 = l * 4 + (c - 4)
                S.op("dve", lambda e, j4=j4: e.tensor_scalar(out=accd, in0=accd, scalar1=sinkcol[:, j4:j4 + 1], scalar2=None, op0=ALU.add),
                     reads=["accd", "sinkcol"], writes=["accd"])
            S.op("dve", lambda e: e.reciprocal(out=rden, in_=accd), reads=["accd"], writes=["rden"])
            S.op("pool", lambda e: e.tensor_tensor(out=oc, in0=accn, in1=rden, op=ALU.mult), reads=["accn", "rden"], writes=["oc"])
            dma("sp", oT[:, c, :], oc, reads=["oc"], writes=["oT"], key="oc")
        S.barrier()
        A.release(mk)

    def phase_ffn(l, li_dense, li_moe, last):
        moe = layer_types[l] == 1
        mk = A.mark()
        g0 = l * NG
        yacc = A.alloc([128, 8, D], F32)
        hTf = A.alloc([128, KC, 1024], BF16)
        gate = A.alloc([128, 8, NE], F32)
        wd = [A.alloc([128, NJ // 2, D], BF16) for _ in range(2)]
        wg = [A.alloc([128, KC, 256], BF16) for _ in range(2)]
        wu = [A.alloc([128, KC, 256], BF16) for _ in range(2)]
        sg = [A.alloc([128, 512], F32) for _ in range(2)]
        rt32 = A.alloc([128, KC, NE], F32)
        mU = A.mark()
        aT = A.alloc([128, NJ, 1024], BF16)
        A.release(mU)
        wout = A.alloc([128, KC, D], BF16)
        ot = A.alloc([128, 8, 512], BF16)
        mixT = A.alloc([128, 8, 512], BF16)
        rsa = A.alloc([128, 2, 512], F32)
        rsb = A.alloc([128, 2, 512], F32)
        xn = A.alloc([128, D], F32)
        xn2 = [xn, A.alloc([128, D], F32)]
        junk = A.alloc([128, D], BF16)
        h32 = A.alloc([128, KC, 512], F32)
        sm = A.alloc([128, 16], F32)
        gsc = A.alloc([128, 48], F32)
        gfin = A.alloc([128, D], F32) if last else None
        NEXP = NE if moe else 1
        if moe:
            dma("sp", rt32, router[li_moe].rearrange("(k p) e -> p k e", p=128), writes=["rt32"], key="rt32")
        if last:
            dma("sp", gfin, fin_norm.partition_broadcast(128), writes=["gfin"], key="gfin")

        def wsrc(e):
            if moe:
                return mwg[li_moe, e], mwu[li_moe, e], mwd[li_moe, e]
            return dwg[li_dense], dwu[li_dense], dwd[li_dense]

        def load_gu(e, g):
            b = g % 2
            G_, U_, _ = wsrc(e)
            dma("pool", wg[b], G_[:, g * 256:(g + 1) * 256].rearrange("(k p) c -> p k c", p=128), writes=["wg%d" % b], key="wg%d" % b)
            dma("pool", wu[b], U_[:, g * 256:(g + 1) * 256].rearrange("(k p) c -> p k c", p=128), writes=["wu%d" % b], key="wu%d" % b)

        def load_wd(e, half):
            _, _, D_ = wsrc(e)
            for q in range(2):
                j0 = half * 14 + q * 7
                dma("pool", wd[half][:, q * 7:(q + 1) * 7, :], D_[j0 * 128:(j0 + 7) * 128, :].rearrange("(j p) c -> p j c", p=128),
                    writes=["wd%d" % half], key="wd%d" % half)

        for t4 in range(T // 1024):
            dma("pool", wout[:, 0:4, :], w_out[l][0:512, :].rearrange("(k p) c -> p k c", p=128), writes=["wout0"], key="wout0")
            dma("pool", wout[:, 4:8, :], w_out[l][512:1024, :].rearrange("(k p) c -> p k c", p=128), writes=["wout1"], key="wout1")
            load_gu(0, 0)
            load_gu(0, 1)
            load_wd(0, 0)
            load_wd(0, 1)
            dma("sp", yacc, (x_in if l == 0 else xres)[t4 * 1024:(t4 + 1) * 1024, :].rearrange("(b p) d -> p b d", p=128),
                writes=["yacc%d" % i for i in range(8)], key="yacc")
            for st in range(2):
                tok0 = t4 * 1024 + st * 512
                dma("sp", ot, oT[:, :, tok0:tok0 + 512], writes=["ot"], key="ot")
                S.op("act", lambda e: e.activation(out=mixT, in_=ot, func=AF.Square), reads=["ot"],
                     writes=["mixT"] + ["mixT%d" % c for c in range(8)])
                for grp in range(2):
                    for c in range(4):
                        S.op("pe", lambda e, grp=grp, c=c: e.matmul(ps[grp][:, :], lhsT=ones_bf, rhs=mixT[:, grp * 4 + c, :],
                                                                    start=(c == 0), stop=(c == 3)),
                             reads=["mixT", "ones"], writes=["ps%d" % grp])
                    rsx = rsa if grp == 0 else rsb
                    S.op("act", lambda e, grp=grp, rsx=rsx: e.activation(out=rsx[:, 0, :], in_=ps[grp][:, :], func=AF.Sqrt,
                                                                         bias=epsc[:, 0:1], scale=1.0 / 512),
                         reads=["ps%d" % grp, "epsc"], writes=["rs%d_0" % grp])
                    S.op("dve", lambda e, rsx=rsx: e.reciprocal(out=rsx[:, 1, :], in_=rsx[:, 0, :]),
                         reads=["rs%d_0" % grp], writes=["rs%d_1" % grp])
                for c in range(8):
                    rsx = rsa if c < 4 else rsb
                    gc = g0 + 16 + c
                    S.op("dve", lambda e, c=c, rsx=rsx, gc=gc: e.scalar_tensor_tensor(
                        out=mixT[:, c, :], in0=ot[:, c, :], scalar=gcol[:, gc:gc + 1], in1=rsx[:, 1, :], op0=ALU.mult, op1=ALU.mult),
                         reads=["ot", "rs%d_1" % (0 if c < 4 else 1), "gcol%d" % l], writes=["mixT%d" % c, "mixT"])
                mreads = ["mixT%d" % c for c in range(8)]

                def p3_mm(bb):
                    blk = st * 4 + bb
                    q = bb % 2
                    xq = xn2[q]
                    for half in range(2):
                        pb = ps[2 + half]
                        for c in range(8):
                            S.op("pe", lambda e, bb=bb, half=half, c=c, pb=pb: e.matmul(
                                pb[:, :], lhsT=mixT[:, c, bb * 128:(bb + 1) * 128], rhs=wout[:, c, half * 512:(half + 1) * 512],
                                start=(c == 0), stop=(c == 7)),
                                 reads=[mreads[c], "wout%d" % (c // 4)], writes=["ps%d" % (2 + half)])
                        S.op("dve", lambda e, blk=blk, half=half, pb=pb: e.tensor_tensor(
                            out=yacc[:, blk, half * 512:(half + 1) * 512], in0=yacc[:, blk, half * 512:(half + 1) * 512],
                            in1=pb[:, :], op=ALU.add),
                             reads=["ps%d" % (2 + half), "yacc%d" % blk], writes=["yacc%d" % blk])
                    c0 = 9 + 3 * q if q else 0
                    S.op("act", lambda e, blk=blk, c0=c0: e.activation(out=junk, in_=yacc[:, blk, :], func=AF.Square, accum_out=sm[:, c0:c0 + 1]),
                         reads=["yacc%d" % blk], writes=["sm0_%d" % q])
                    S.op("act", lambda e, c0=c0: e.activation(out=sm[:, c0 + 1:c0 + 2], in_=sm[:, c0:c0 + 1], func=AF.Sqrt, bias=epsc[:, 0:1], scale=1.0 / D),
                         reads=["sm0_%d" % q, "epsc"], writes=["sm1_%d" % q])
                    S.op("dve", lambda e, c0=c0: e.reciprocal(out=sm[:, c0 + 2:c0 + 3], in_=sm[:, c0 + 1:c0 + 2]), reads=["sm1_%d" % q], writes=["sm2_%d" % q])
                    S.op("dve", lambda e, blk=blk, c0=c0, xq=xq: e.tensor_scalar(out=xq, in0=yacc[:, blk, :], scalar1=sm[:, c0 + 2:c0 + 3], scalar2=None, op0=ALU.mult),
                         reads=["yacc%d" % blk, "sm2_%d" % q], writes=["xn_%d" % q])

                def p3_tr(bb):
                    q = bb % 2
                    xq = xn2[q]
                    pbank = (4, 5) if q == 0 else (0, 1)
                    for kc in range(KC):
                        bi = pbank[kc // 4]
                        S.op("pe", lambda e, kc=kc, bi=bi, xq=xq: e.transpose(ps[bi][:, (kc % 4) * 128:(kc % 4 + 1) * 128],
                                                                             xq[:, kc * 128:(kc + 1) * 128], ident),
                             reads=["xn_%d" % q, "ident"], writes=["ps%d" % bi])
                    for kc in range(KC):
                        bi = pbank[kc // 4]
                        S.op("act", lambda e, kc=kc, bi=bi, bb=bb: e.activation(
                            out=h32[:, kc, bb * 128:(bb + 1) * 128], in_=ps[bi][:, (kc % 4) * 128:(kc % 4 + 1) * 128], func=AF.Copy,
                            scale=gcol[:, g0 + 8 + kc:g0 + 8 + kc + 1]),
                             reads=["ps%d" % bi, "gcol%d" % l], writes=["h32_%d" % bb])
                    if moe:
                        for kc in range(KC):
                            S.op("pe", lambda e, kc=kc, bb=bb: e.matmul(ps[6][:, bb * 8:(bb + 1) * 8], lhsT=h32[:, kc, bb * 128:(bb + 1) * 128],
                                                                        rhs=rt32[:, kc, :], start=(kc == 0), stop=(kc == KC - 1)),
                                 reads=["h32_%d" % bb, "rt32"], writes=["ps6"])

                p3_mm(0)
                for bb in range(4):
                    if bb + 1 < 4:
                        p3_mm(bb + 1)
                    p3_tr(bb)
                S.op("pool", lambda e, st=st: e.tensor_copy(out=hTf[:, :, st * 512:(st + 1) * 512], in_=h32),
                     reads=["h32_%d" % i for i in range(4)], writes=["hTf%d" % st])
                if moe:
                    lg = sm
                    for bb in range(4):
                        blk = st * 4 + bb
                        Lg = ps[6][:, bb * 8:(bb + 1) * 8]
                        gb = gate[:, blk, :]
                        S.op("dve", lambda e, Lg=Lg: e.tensor_copy(out=gsc[:, 0:8], in_=Lg), reads=["ps6"], writes=["lg"])
                        S.op("dve", lambda e: e.tensor_reduce(out=sm[:, 4:5], in_=gsc[:, 0:8], axis=AX.X, op=ALU.max),
                             reads=["lg"], writes=["m1"])
                        S.op("dve", lambda e: e.tensor_scalar(out=gsc[:, 8:16], in0=gsc[:, 0:8], scalar1=sm[:, 4:5], scalar2=None,
                                                              op0=ALU.is_equal), reads=["lg", "m1"], writes=["eq"])
                        S.op("dve", lambda e: e.scalar_tensor_tensor(out=gsc[:, 16:24], in0=gsc[:, 8:16], scalar=-1e30,
                                                                     in1=gsc[:, 0:8], op0=ALU.mult, op1=ALU.add),
                             reads=["eq", "lg"], writes=["lg2"])
                        S.op("dve", lambda e: e.tensor_reduce(out=sm[:, 5:6], in_=gsc[:, 16:24], axis=AX.X, op=ALU.max),
                             reads=["lg2"], writes=["m2"])
                        S.op("dve", lambda e: e.tensor_scalar(out=gsc[:, 24:32], in0=gsc[:, 0:8], scalar1=sm[:, 5:6], scalar2=None,
                                                              op0=ALU.is_ge), reads=["lg", "m2"], writes=["sel"])
                        S.op("dve", lambda e: e.tensor_scalar(out=sm[:, 6:7], in0=sm[:, 4:5], scalar1=-1.0, scalar2=None, op0=ALU.mult),
                             reads=["m1"], writes=["nm1"])
                        S.op("act", lambda e: e.activation(out=gsc[:, 32:40], in_=gsc[:, 0:8], func=AF.Exp, bias=sm[:, 6:7]),
                             reads=["lg", "nm1"], writes=["ex"])
                        S.op("dve", lambda e: e.tensor_tensor(out=gsc[:, 40:48], in0=gsc[:, 32:40], in1=gsc[:, 24:32], op=ALU.mult),
                             reads=["ex", "sel"], writes=["exs"])
                        S.op("dve", lambda e: e.tensor_reduce(out=sm[:, 7:8], in_=gsc[:, 40:48], axis=AX.X, op=ALU.add),
                             reads=["exs"], writes=["se"])
                        S.op("dve", lambda e: e.reciprocal(out=sm[:, 8:9], in_=sm[:, 7:8]), reads=["se"], writes=["rse"])
                        S.op("dve", lambda e, gb=gb: e.tensor_scalar(out=gb, in0=gsc[:, 40:48], scalar1=sm[:, 8:9], scalar2=None, op0=ALU.mult),
                             reads=["exs", "rse"], writes=["gate%d" % blk])
            S.barrier()
            hreads = ["hTf0", "hTf1"]
            for e_ in range(NEXP):
                for g in range(14):
                    b = g % 2
                    for jj in range(2):
                        j = g * 2 + jj
                        for th in range(2):
                            a = (jj * 2 + th) % 2
                            pG, pU = ps[a], ps[2 + a]
                            for kc in range(KC):
                                S.op("pe", lambda e, kc=kc, b=b, jj=jj, th=th, pG=pG: e.matmul(
                                    pG[:, :], lhsT=wg[b][:, kc, jj * 128:(jj + 1) * 128], rhs=hTf[:, kc, th * 512:(th + 1) * 512],
                                    start=(kc == 0), stop=(kc == KC - 1)),
                                     reads=["wg%d" % b, hreads[th]], writes=["ps%d" % a])
                            for kc in range(KC):
                                S.op("pe", lambda e, kc=kc, b=b, jj=jj, th=th, pU=pU: e.matmul(
                                    pU[:, :], lhsT=wu[b][:, kc, jj * 128:(jj + 1) * 128], rhs=hTf[:, kc, th * 512:(th + 1) * 512],
                                    start=(kc == 0), stop=(kc == KC - 1)),
                                     reads=["wu%d" % b, hreads[th]], writes=["ps%d" % (2 + a)])
                            S.op("act", lambda e, a=a, pG=pG: e.activation(out=sg[a], in_=pG[:, :], func=AF.Silu),
                                 reads=["ps%d" % a], writes=["sg%d" % a])
                            S.op("dve", lambda e, a=a, pU=pU, j=j, th=th: e.tensor_tensor(
                                out=aT[:, j, th * 512:(th + 1) * 512], in0=sg[a], in1=pU[:, :], op=ALU.mult),
                                 reads=["sg%d" % a, "ps%d" % (2 + a)], writes=["aT%d" % (j // 14)])
                    if g + 2 < 14:
                        load_gu(e_, g + 2)
                    elif e_ + 1 < NEXP:
                        load_gu(e_ + 1, g + 2 - 14)
                for half in range(2):
                    for blk in range(8):
                        for ch in range(2):
                            pb = ps[4 + (blk * 2 + ch) % 4]
                            ptok = "ps%d" % (4 + (blk * 2 + ch) % 4)
                            for jx in range(14):
                                j = half * 14 + jx
                                S.op("pe", lambda e, blk=blk, ch=ch, jx=jx, j=j, pb=pb, half=half: e.matmul(
                                    pb[:, :], lhsT=aT[:, j, blk * 128:(blk + 1) * 128], rhs=wd[half][:, jx, ch * 512:(ch + 1) * 512],
                                    start=(jx == 0), stop=(jx == 13)),
                                     reads=["aT%d" % half, "wd%d" % half], writes=[ptok])
                            ys = yacc[:, blk, ch * 512:(ch + 1) * 512]
                            if moe:
                                S.op("dve", lambda e, pb=pb, ys=ys, blk=blk, e_=e_: e.scalar_tensor_tensor(
                                    out=ys, in0=pb[:, :], scalar=gate[:, blk, e_:e_ + 1], in1=ys, op0=ALU.mult, op1=ALU.add),
                                     reads=[ptok, "yacc%d" % blk, "gate%d" % blk], writes=["yacc%d" % blk])
                            else:
                                S.op("dve", lambda e, pb=pb, ys=ys: e.tensor_tensor(out=ys, in0=ys, in1=pb[:, :], op=ALU.add),
                                     reads=[ptok, "yacc%d" % blk], writes=["yacc%d" % blk])
                    if e_ + 1 < NEXP:
                        load_wd(e_ + 1, half)
            if not last:
                dma("sp", xres[t4 * 1024:(t4 + 1) * 1024, :].rearrange("(b p) d -> p b d", p=128), yacc,
                    reads=["yacc%d" % i for i in range(8)], writes=["xres"], key="xst")
            else:
                S.barrier()
                for blk in range(8):
                    S.op("act", lambda e, blk=blk: e.activation(out=junk, in_=yacc[:, blk, :], func=AF.Square, accum_out=sm[:, 0:1]),
                         reads=["yacc%d" % blk], writes=["sm0"])
                    S.op("act", lambda e: e.activation(out=sm[:, 1:2], in_=sm[:, 0:1], func=AF.Sqrt, bias=epsc[:, 0:1], scale=1.0 / D),
                         reads=["sm0", "epsc"], writes=["sm1"])
                    S.op("dve", lambda e: e.reciprocal(out=sm[:, 2:3], in_=sm[:, 1:2]), reads=["sm1"], writes=["sm2"])
                    S.op("dve", lambda e, blk=blk: e.scalar_tensor_tensor(out=yacc[:, blk, :], in0=yacc[:, blk, :], scalar=sm[:, 2:3],
                                                                         in1=gfin, op0=ALU.mult, op1=ALU.mult),
                         reads=["yacc%d" % blk, "sm2", "gfin"], writes=["yacc%d" % blk])
                dma("sp", out[t4 * 1024:(t4 + 1) * 1024, :].rearrange("(b p) d -> p b d", p=128), yacc,
                    reads=["yacc%d" % i for i in range(8)], writes=["out"], key="xst")
            S.barrier()
        A.release(mk)

    nd = nm = 0
    for l, lt in enumerate(layer_types):
        if stop_after == "setup":
            break
        if not os.environ.get("SKIP_PROJ"):
            phase_proj(l)
            if stop_after in ("proj", "exch"):
                break
        phase_attn(l)
        if stop_after == "attn":
            break
        phase_ffn(l, nd, nm, l == L - 1)
        if lt == 0:
            nd += 1
        else:
            nm += 1
    S.emit()
    return nc


def _consts():
    k = np.arange(128)[:, None]
    q = np.arange(128)[None, :]
    ident = (k == q).astype(np.float32)
    src = (q // 64) * 64 + ((q % 64) + 32) % 64
    perm = (k == src).astype(np.float32)
    mask = np.stack([(q >= k), (k >= q), (k > q)], axis=1).astype(np.float32)
    p = np.arange(128)
    invf = (10000.0 ** (-(p % 32).astype(np.float64) / 32.0)).astype(np.float32)
    sgn = np.where((p % 64) < 32, -1.0, 1.0).astype(np.float32)
    col = np.stack([invf, sgn], axis=1).astype(np.float32)
    return ident, perm, np.ascontiguousarray(mask), np.ascontiguousarray(col)


_CACHE = {}


def run_layers(inputs, layer_types, debug_out=(), stop_after=None):
    L = len(layer_types)
    key = (tuple(layer_types), tuple(debug_out), stop_after)
    if key not in _CACHE:
        _CACHE[key] = build_program(list(layer_types), debug_out=debug_out, stop_after=stop_after)
    nc = _CACHE[key]
    ident, perm, mask, col = _consts()
    f32 = lambda a: np.ascontiguousarray(np.asarray(a, dtype=np.float32))
    x = f32(inputs["x"])
    pos = np.ascontiguousarray(np.asarray(inputs["positions"], dtype=np.int32))
    shared = {
        "c_ident": ident, "c_perm": perm, "c_mask": mask, "c_col": col,
        "attn_norm": f32(inputs["attn_norm"])[:L], "w_in": f32(inputs["w_in"])[:L],
        "mix_norm_a": f32(inputs["mix_norm_a"])[:L], "mix_norm_b": f32(inputs["mix_norm_b"])[:L],
        "sinks": f32(inputs["sinks"])[:L].reshape(1, L * 8), "w_out": f32(inputs["w_out"])[:L],
        "ffn_norm": f32(inputs["ffn_norm"])[:L], "final_norm": f32(inputs["final_norm"]).reshape(1, D),
    }
    nd = sum(1 for t in layer_types if t == 0)
    nm = sum(1 for t in layer_types if t == 1)
    if nd:
        shared["dense_w_gate"] = f32(inputs["dense_w_gate"])[:nd]
        shared["dense_w_up"] = f32(inputs["dense_w_up"])[:nd]
        shared["dense_w_down"] = f32(inputs["dense_w_down"])[:nd]
    if nm:
        shared["router"] = f32(inputs["router"])[:nm]
        shared["moe_w_gate"] = f32(inputs["moe_w_gate"])[:nm]
        shared["moe_w_up"] = f32(inputs["moe_w_up"])[:nm]
        shared["moe_w_down"] = f32(inputs["moe_w_down"])[:nm]
    in_maps = []
    for c in range(NCORES):
        b, h = c // 2, c % 2
        m = dict(shared)
        m["x"] = np.ascontiguousarray(x[b, h * T:(h + 1) * T, :])
        m["pos"] = np.ascontiguousarray(pos[h * T:(h + 1) * T][None, :])
        m["flag"] = np.full((128, 1), float(h), dtype=np.float32)
        in_maps.append(m)
    res = run_bass_kernel_spmd(nc, in_maps, core_ids=list(range(NCORES)))
    outp = np.empty((4, 2 * T, D), dtype=np.float32)
    for c in range(NCORES):
        b, h = c // 2, c % 2
        outp[b, h * T:(h + 1) * T, :] = np.asarray(res.results[c]["out"], dtype=np.float32)
    if debug_out:
        return outp, res.results
    return outp


def kernel(**inputs):
    return run_layers(inputs, [0, 1, 0, 1])
```

```python
import contextlib
import os
import numpy as np
import concourse.bass as bass
import concourse.mybir as mybir
from concourse.bass_utils import run_bass_kernel_spmd

F32 = mybir.dt.float32
BF16 = mybir.dt.bfloat16
I32 = mybir.dt.int32
AF = mybir.ActivationFunctionType
ALU = mybir.AluOpType
AX = mybir.AxisListType

ENGS = ("pe", "act", "dve", "pool", "sp")

NCORES = 8
T = 4096
TH = 2048
TE = T + TH
D = 1024
KC = 8
FF = 3584
NJ = FF // 128
NE = 8
INW = 2304
EPS = 1e-5
DIL = (1, 4, 16)


class Op:
    __slots__ = ("eng", "fn", "deps", "signal", "count", "dma_key", "is_dma", "inc")

    def __init__(self, eng, fn, dma_key, inc=16):
        self.eng = eng
        self.fn = fn
        self.deps = []
        self.signal = False
        self.count = None
        self.dma_key = dma_key
        self.is_dma = dma_key is not None
        self.inc = inc


class Sched:
    def __init__(self, nc, same_engine_sync=True):
        self.nc = nc
        self.ops = {e: [] for e in ENGS}
        self.last_w = {}
        self.readers = {}
        self.dma_counts = {}
        self.same_engine_sync = same_engine_sync

    def op(self, eng, fn, reads=(), writes=(), dma_key=None, inc=16):
        o = Op(eng, fn, dma_key, inc)
        deps = []
        for t in reads:
            w = self.last_w.get(t)
            if w is not None:
                deps.append((w, 0))
        for t in writes:
            w = self.last_w.get(t)
            if w is not None:
                deps.append((w, 1))
            for r in self.readers.get(t, ()):
                deps.append((r, 2))
        seen = set()
        for p, kind in deps:
            if id(p) in seen:
                continue
            if (not p.is_dma) and p.eng == eng:
                if eng == "pe" or kind == 2 or not self.same_engine_sync:
                    continue
            seen.add(id(p))
            o.deps.append(p)
            if not p.is_dma:
                p.signal = True
        for t in reads:
            self.readers.setdefault(t, []).append(o)
        for t in writes:
            self.last_w[t] = o
            self.readers[t] = []
        if o.is_dma:
            c = self.dma_counts.get(dma_key, 0) + 1
            self.dma_counts[dma_key] = c
            o.count = c
        self.ops[eng].append(o)
        return o

    def barrier(self):
        lasts = []
        dma_last = {}
        for e in ENGS:
            lst = [o for o in self.ops[e] if not o.is_dma and o.fn is not None]
            if lst:
                lasts.append(lst[-1])
            for o in self.ops[e]:
                if o.is_dma:
                    dma_last[o.dma_key] = o
        for e in ENGS:
            o = Op(e, None, None)
            for p in lasts:
                if p.eng != e:
                    o.deps.append(p)
                    p.signal = True
            o.deps.extend(dma_last.values())
            self.ops[e].append(o)
        self.last_w = {}
        self.readers = {}

    def emit(self):
        nc = self.nc
        with contextlib.ExitStack() as st:
            esem = {e: st.enter_context(nc.semaphore("s_" + e)) for e in ENGS}
            dsem = {k: st.enter_context(nc.semaphore("d_%d" % i)) for i, k in enumerate(self.dma_counts)}
            block = st.enter_context(nc.Block())
            for e in ENGS:
                c = 0
                for o in self.ops[e]:
                    if (not o.is_dma) and o.signal:
                        c += 1
                        o.count = c

            def event(p):
                if p.is_dma:
                    return dsem[p.dma_key], p.inc * p.count
                return esem[p.eng], p.count

            def run(e, h):
                waited = {}
                for o in self.ops[e]:
                    for p in o.deps:
                        s, v = event(p)
                        if waited.get(id(s), 0) >= v:
                            continue
                        waited[id(s)] = v
                        h.wait_ge(s, v)
                    if o.fn is None:
                        continue
                    ins = o.fn(h)
                    if o.is_dma:
                        if o.inc == 1:
                            ins.then_inc(dsem[o.dma_key])
                        else:
                            ins.then_inc(dsem[o.dma_key], o.inc)
                    elif o.signal:
                        ins.then_inc(esem[e], 1)

            block.tensor(lambda h: run("pe", h))
            block.scalar(lambda h: run("act", h))
            block.vector(lambda h: run("dve", h))
            block.gpsimd(lambda h: run("pool", h))
            block.sync(lambda h: run("sp", h))


class Arena:
    def __init__(self, nc, nbytes=206 * 1024):
        self.big = nc.alloc_sbuf_tensor("arena", [128, nbytes], mybir.dt.uint8)
        self.off = 0
        self.limit = nbytes

    def alloc(self, shape, dtype):
        esz = 2 if dtype == BF16 else 4
        n = int(np.prod(shape[1:]))
        off = (self.off + 63) // 64 * 64
        assert off + n * esz <= self.limit, ("SBUF arena overflow", off, n * esz, self.limit)
        self.off = off + n * esz
        ap = self.big[0:shape[0], off:off + n * esz].bitcast(dtype)
        if len(shape) == 3:
            ap = ap.rearrange("p (a b) -> p a b", b=shape[2])
        elif len(shape) == 4:
            ap = ap.rearrange("p (a b c) -> p a b c", b=shape[2], c=shape[3])
        return ap

    def mark(self):
        return self.off

    def release(self, m):
        self.off = m


def build_program(layer_types, groups=None, stop_after=None, debug_out=()):
    L = len(layer_types)
    ND = max(1, sum(1 for t in layer_types if t == 0))
    NM = max(1, sum(1 for t in layer_types if t == 1))
    if groups is None:
        groups = [[0, 1], [2, 3], [4, 5], [6, 7]]
    nc = bass.Bass("TRN2", target_bir_lowering=False)
    S = Sched(nc)
    A = Arena(nc)

    def din(name, shape, dt=F32):
        return nc.dram_tensor(name, list(shape), dt, kind="ExternalInput").ap()

    x_in = din("x", [T, D])
    pos_in = din("pos", [1, T], I32)
    flag_in = din("flag", [128, 1])
    cident_in = din("c_ident", [128, 128])
    cperm_in = din("c_perm", [128, 128])
    cmask_in = din("c_mask", [128, 3, 128])
    ccol_in = din("c_col", [128, 2])
    attn_norm = din("attn_norm", [L, D])
    w_in = din("w_in", [L, D, INW])
    mix_a = din("mix_norm_a", [L, 512])
    mix_b = din("mix_norm_b", [L, 512])
    sinks = din("sinks", [1, L * 8])
    w_out = din("w_out", [L, D, D])
    ffn_norm = din("ffn_norm", [L, D])
    if 0 in layer_types:
        dwg = din("dense_w_gate", [ND, D, FF])
        dwu = din("dense_w_up", [ND, D, FF])
        dwd = din("dense_w_down", [ND, FF, D])
    if 1 in layer_types:
        router = din("router", [NM, D, NE])
        mwg = din("moe_w_gate", [NM, NE, D, FF])
        mwu = din("moe_w_up", [NM, NE, D, FF])
        mwd = din("moe_w_down", [NM, NE, FF, D])
    fin_norm = din("final_norm", [1, D])
    out = nc.dram_tensor("out", [T, D], F32, kind="ExternalOutput").ap()

    def dscr(name, shape, dt):
        if name in debug_out:
            return nc.dram_tensor(name, list(shape), dt, kind="ExternalOutput").ap()
        return nc.dram_tensor(name, list(shape), dt).ap()

    xres = dscr("xres", [T, D], F32)
    qT = dscr("qT", [128, 8, T], BF16)
    kT = dscr("kT", [128, 5, TE], BF16)
    vbuf = dscr("vbuf", [TE, 640], BF16)
    oT = dscr("oT", [128, 8, T], BF16)
    cs = dscr("cs", [128, 2, T], F32)
    sendk = dscr("sendk", [5, 128, TH], BF16)
    recvk = dscr("recvk", [5, 256, TH], BF16)
    sendv = dscr("sendv", [2, 1024, 640], BF16)
    recvv = dscr("recvv", [2, 2048, 640], BF16)

    ps = [nc.alloc_psum_tensor("ps%d" % i, [128, 512], F32)[:, :] for i in range(8)]

    ident = A.alloc([128, 128], F32)
    ones_bf = A.alloc([128, 128], BF16)
    perm_bf = A.alloc([128, 128], BF16)
    maskf = A.alloc([128, 3, 128], F32)
    flagc = A.alloc([128, 1], F32)
    ccol = A.alloc([128, 2], F32)
    NG = 24
    gcol = A.alloc([128, L * NG], F32)
    esink = A.alloc([128, L * 8], F32)
    sinkcol = A.alloc([128, L * 4], F32)
    epsc = A.alloc([128, 1], F32)
    negpi = A.alloc([128, 1], F32)

    def dma(q, out_ap, in_ap, reads=(), writes=(), key=None, **kw):
        return S.op(q, lambda e: e.dma_start(out=out_ap, in_=in_ap, **kw), reads=reads, writes=writes, dma_key=key)

    m0 = A.mark()
    permf = A.alloc([128, 128], F32)
    dma("sp", ident, cident_in, writes=["ident"], key="c0")
    dma("sp", permf, cperm_in, writes=["permf"], key="c1")
    dma("sp", maskf, cmask_in, writes=["maskf"], key="c2")
    dma("sp", flagc, flag_in, writes=["flagc"], key="c3")
    dma("sp", ccol, ccol_in, writes=["ccol"], key="c4")
    dma("sp", esink, sinks.partition_broadcast(128), writes=["esink"], key="c5")
    for l in range(L):
        b0 = l * NG
        dma("sp", gcol[:, b0:b0 + 8], attn_norm[l].rearrange("(k p) -> p k", p=128), writes=["gcol%d" % l],
            key="g0", allow_slow_non_contiguous=True)
        dma("sp", gcol[:, b0 + 8:b0 + 16], ffn_norm[l].rearrange("(k p) -> p k", p=128), writes=["gcol%d" % l],
            key="g0", allow_slow_non_contiguous=True)
        dma("sp", gcol[:, b0 + 16:b0 + 20], mix_a[l].rearrange("(k p) -> p k", p=128), writes=["gcol%d" % l],
            key="g0", allow_slow_non_contiguous=True)
        dma("sp", gcol[:, b0 + 20:b0 + 24], mix_b[l].rearrange("(k p) -> p k", p=128), writes=["gcol%d" % l],
            key="g0", allow_slow_non_contiguous=True)
    S.op("pool", lambda e: e.memset(ones_bf, 1.0), writes=["ones"])
    S.op("pool", lambda e: e.memset(epsc, EPS), writes=["epsc"])
    S.op("pool", lambda e: e.memset(negpi, -3.1415925), writes=["negpi"])
    S.op("dve", lambda e: e.tensor_copy(out=perm_bf, in_=permf), reads=["permf"], writes=["perm"])
    S.op("act", lambda e: e.activation(out=esink, in_=esink, func=AF.Exp), reads=["esink"], writes=["esink"])
    for l in range(L):
        for j in range(4):
            c0 = l * 8 + 2 * j
            S.op("dve", lambda e, c0=c0, l=l, j=j: e.tensor_copy(out=sinkcol[0:64, l * 4 + j:l * 4 + j + 1],
                                                              in_=esink[0:64, c0:c0 + 1]),
                 reads=["esink"], writes=["sinkcol"])
            S.op("dve", lambda e, c0=c0, l=l, j=j: e.tensor_copy(out=sinkcol[64:128, l * 4 + j:l * 4 + j + 1],
                                                              in_=esink[64:128, c0 + 1:c0 + 2]),
                 reads=["esink"], writes=["sinkcol"])
    CH = 1024
    posi = A.alloc([128, CH], I32)
    posf = A.alloc([128, CH], F32)
    tt_ = A.alloc([128, CH], F32)
    kf_ = A.alloc([128, CH], F32)
    ki_ = A.alloc([128, CH], I32)
    msk_ = A.alloc([128, CH], F32)
    tab = A.alloc([128, 2, CH], F32)
    INV2PI = float(1.0 / (2.0 * np.pi))
    for ch in range(T // CH):
        dma("sp", posi, pos_in[:, ch * CH:(ch + 1) * CH].partition_broadcast(128), writes=["posi"], key="posi")
        S.op("dve", lambda e: e.tensor_copy(out=posf, in_=posi), reads=["posi"], writes=["posf"])
        S.op("dve", lambda e: e.tensor_scalar(out=posf, in0=posf, scalar1=ccol[:, 0:1], scalar2=INV2PI,
                                              op0=ALU.mult, op1=ALU.mult), reads=["posf", "ccol"], writes=["posf"])
        for which, off in ((1, 0.5), (0, 0.75)):
            S.op("dve", lambda e, off=off: e.tensor_scalar(out=tt_, in0=posf, scalar1=float(off), scalar2=None,
                                                           op0=ALU.add), reads=["posf"], writes=["tt"])
            S.op("dve", lambda e: e.tensor_copy(out=ki_, in_=tt_), reads=["tt"], writes=["ki"])
            S.op("dve", lambda e: e.tensor_copy(out=kf_, in_=ki_), reads=["ki"], writes=["kf"])
            S.op("dve", lambda e: e.tensor_tensor(out=tt_, in0=tt_, in1=kf_, op=ALU.subtract), reads=["tt", "kf"], writes=["tt"])
            S.op("dve", lambda e: e.tensor_scalar(out=msk_, in0=tt_, scalar1=0.0, scalar2=None, op0=ALU.is_lt),
                 reads=["tt"], writes=["msk"])
            S.op("dve", lambda e: e.tensor_tensor(out=tt_, in0=tt_, in1=msk_, op=ALU.add), reads=["tt", "msk"], writes=["tt"])
            S.op("act", lambda e, which=which: e.activation(out=tab[:, which, :], in_=tt_, func=AF.Sin,
                                                            bias=negpi[:, 0:1], scale=6.283185),
                 reads=["tt", "negpi"], writes=["tab%d" % which])
        S.op("dve", lambda e: e.tensor_scalar(out=tab[:, 1, :], in0=tab[:, 1, :], scalar1=ccol[:, 1:2], scalar2=None,
                                              op0=ALU.mult), reads=["tab1", "ccol"], writes=["tab1"])
        dma("sp", cs[:, :, ch * CH:(ch + 1) * CH], tab, reads=["tab0", "tab1"], writes=["cs"], key="cs")
    S.barrier()
    A.release(m0)

    def rms_rstd(ssq_ap, rt_ap, rstd_ap, n, reads, wtok):
        S.op("act", lambda e: e.activation(out=rt_ap, in_=ssq_ap, func=AF.Sqrt, bias=epsc[:, 0:1], scale=1.0 / n),
             reads=list(reads) + ["epsc"], writes=[wtok + "_rt"])
        S.op("dve", lambda e: e.reciprocal(out=rstd_ap, in_=rt_ap), reads=[wtok + "_rt"], writes=[wtok])

    def phase_proj(l):
        mk = A.mark()
        win = A.alloc([128, KC, INW], BF16)
        for kc in range(KC):
            dma("pool", win[:, kc, :], w_in[l][kc * 128:(kc + 1) * 128, :],
                writes=["win%d" % (kc // 2)], key="win%d" % (kc // 2), max_dma_last_dim=4096)
        xt = [A.alloc([128, 4, D], F32) for _ in range(2)]
        xn = A.alloc([128, 4, D], F32)
        junk = A.alloc([128, D], BF16)
        ssq = A.alloc([128, 4], F32)
        rt = A.alloc([128, 4], F32)
        rstd = A.alloc([128, 4], F32)
        hT = [A.alloc([128, KC, 512], BF16) for _ in range(2)]
        cst = [A.alloc([128, 2, 512], F32) for _ in range(2)]
        tb = [A.alloc([128, 512], BF16) for _ in range(2)]
        ra = [A.alloc([128, 512], F32) for _ in range(2)]
        rb = [A.alloc([128, 512], F32) for _ in range(2)]
        qkst = A.alloc([128, 13, 512], BF16)
        vst = A.alloc([128, 4, 640], BF16)
        xsrc = x_in if l == 0 else xres
        g0 = l * NG
        colstarts = [0, 128, 256, 384, 1536, 1664, 1792, 1920, 512, 640, 768, 896, 2048]
        NT = T // 512

        def load_i(it, tt):
            b = it % 2
            dma("sp", xt[b], xsrc[tt * 512:(tt + 1) * 512, :].rearrange("(b p) d -> p b d", p=128),
                writes=["xt%d" % b], key="xt%d" % b)
            dma("sp", cst[b], cs[:, :, tt * 512:(tt + 1) * 512], writes=["cst%d" % b], key="cst%d" % b)

        order = [4, 5, 6, 7, 0, 1, 2, 3]
        load_i(0, order[0])
        for it, tt in enumerate(order):
            b = it % 2
            if it + 1 < NT:
                load_i(it + 1, order[it + 1])
            X = xt[b]
            H = hT[b]
            for bb in range(4):
                S.op("act", lambda e, bb=bb, X=X: e.activation(out=junk, in_=X[:, bb, :], func=AF.Square,
                                                               accum_out=ssq[:, bb:bb + 1]),
                     reads=["xt%d" % b], writes=["ssq%d" % bb])
            rms_rstd(ssq, rt, rstd, D, ["ssq%d" % i for i in range(4)], "rstd")
            for bb in range(4):
                S.op("dve", lambda e, bb=bb, X=X: e.tensor_scalar(out=xn[:, bb, :], in0=X[:, bb, :],
                                                                  scalar1=rstd[:, bb:bb + 1], scalar2=None, op0=ALU.mult),
                     reads=["xt%d" % b, "rstd"], writes=["xn%d" % bb])
            for kc in range(KC):
                pt = ps[kc % 2]
                for bb in range(4):
                    S.op("pe", lambda e, bb=bb, kc=kc, pt=pt: e.transpose(pt[:, bb * 128:(bb + 1) * 128],
                                                                          xn[:, bb, kc * 128:(kc + 1) * 128], ident),
                         reads=["xn%d" % bb, "ident"], writes=["ps%d" % (kc % 2)])
                eng = "act" if kc % 2 == 0 else "dve"
                if eng == "act":
                    S.op("act", lambda e, kc=kc, pt=pt, H=H: e.activation(out=H[:, kc, :], in_=pt, func=AF.Copy,
                                                                          scale=gcol[:, g0 + kc:g0 + kc + 1]),
                         reads=["ps%d" % (kc % 2), "gcol%d" % l], writes=["hT%d_%d" % (b, kc)])
                else:
                    S.op("dve", lambda e, kc=kc, pt=pt, H=H: e.tensor_scalar(out=H[:, kc, :], in0=pt,
                                                                             scalar1=gcol[:, g0 + kc:g0 + kc + 1],
                                                                             scalar2=None, op0=ALU.mult),
                         reads=["ps%d" % (kc % 2), "gcol%d" % l], writes=["hT%d_%d" % (b, kc)])
            hreads = ["hT%d_%d" % (b, kc) for kc in range(KC)]

            def rope(si, b=b):
                a = si % 2
                cb = cst[b]
                S.op("pe", lambda e, a=a: e.matmul(ps[4 + a][:, :], lhsT=perm_bf, rhs=tb[a], start=True, stop=True),
                     reads=["tb%d" % a, "perm"], writes=["ps%d" % (4 + a)])
                S.op("dve", lambda e, a=a, cb=cb: e.tensor_tensor(out=ra[a], in0=tb[a], in1=cb[:, 0, :], op=ALU.mult),
                     reads=["tb%d" % a, "cst%d" % b], writes=["ra%d" % a])
                S.op("dve", lambda e, a=a, cb=cb: e.tensor_tensor(out=rb[a], in0=ps[4 + a][:, :], in1=cb[:, 1, :], op=ALU.mult),
                     reads=["ps%d" % (4 + a), "cst%d" % b], writes=["rb%d" % a])
                S.op("pool", lambda e, a=a, si=si: e.tensor_tensor(out=qkst[:, si, :], in0=ra[a], in1=rb[a], op=ALU.add),
                     reads=["ra%d" % a, "rb%d" % a], writes=["qkst%d" % si])

            pending = None
            for si, c0 in enumerate(colstarts):
                a = si % 2
                for kc in range(KC):
                    S.op("pe", lambda e, a=a, kc=kc, c0=c0, H=H: e.matmul(ps[2 + a][:, :], lhsT=win[:, kc, c0:c0 + 128],
                                                                          rhs=H[:, kc, :], start=(kc == 0), stop=(kc == KC - 1)),
                         reads=[hreads[kc], "win%d" % (kc // 2)], writes=["ps%d" % (2 + a)])
                S.op("act", lambda e, a=a: e.copy(out=tb[a], in_=ps[2 + a][:, :]), reads=["ps%d" % (2 + a)], writes=["tb%d" % a])
                if pending is not None:
                    rope(pending)
                pending = si
            rope(pending)
            for bb in range(4):
                pv = ps[6 + bb % 2]
                for kc in range(KC):
                    S.op("pe", lambda e, bb=bb, kc=kc, pv=pv, H=H: e.matmul(pv[:, :], lhsT=H[:, kc, bb * 128:(bb + 1) * 128],
                                                                            rhs=win[:, kc, 1024:1536], start=(kc == 0), stop=(kc == KC - 1)),
                         reads=[hreads[kc], "win%d" % (kc // 2)], writes=["ps%d" % (6 + bb % 2)])
                S.op("act", lambda e, bb=bb, pv=pv: e.copy(out=vst[:, bb, 0:512], in_=pv[:, :]),
                     reads=["ps%d" % (6 + bb % 2)], writes=["vst%d" % bb])
                pw = ps[4 + bb % 2]
                for kc in range(KC):
                    S.op("pe", lambda e, bb=bb, kc=kc, pw=pw, H=H: e.matmul(pw[:, 0:128], lhsT=H[:, kc, bb * 128:(bb + 1) * 128],
                                                                            rhs=win[:, kc, 2176:2304], start=(kc == 0), stop=(kc == KC - 1)),
                         reads=[hreads[kc], "win%d" % (kc // 2)], writes=["ps%d" % (4 + bb % 2)])
                S.op("dve", lambda e, bb=bb, pw=pw: e.tensor_copy(out=vst[:, bb, 512:640], in_=pw[:, 0:128]),
                     reads=["ps%d" % (4 + bb % 2)], writes=["vst%d" % bb])
            dma("sp", qT[:, :, tt * 512:(tt + 1) * 512], qkst[:, 0:8, :], reads=["qkst%d" % i for i in range(8)],
                writes=["qT"], key="qTst")
            dma("sp", kT[:, :, TH + tt * 512:TH + (tt + 1) * 512], qkst[:, 8:13, :],
                reads=["qkst%d" % i for i in range(8, 13)], writes=["kT"], key="kTst")
            dma("sp", vbuf[TH + tt * 512:TH + (tt + 1) * 512, :].rearrange("(b p) c -> p b c", p=128), vst,
                reads=["vst%d" % i for i in range(4)], writes=["vbuf"], key="vst")
            if it == 3:
                phase_exchange()
        phase_exchange_recv()
        S.barrier()
        A.release(mk)

    def phase_exchange():
        for c in range(5):
            dma("sp", sendk[c], kT[:, c, T:TE], reads=["kT"], writes=["sendk%d" % c], key="ex0")
        for u in range(2):
            dma("sp", sendv[u], vbuf[T + u * 1024:T + (u + 1) * 1024, :], reads=["vbuf"], writes=["sendv%d" % u], key="ex1")
        for c in range(5):
            S.op("pool", lambda e, c=c: e.collective_compute("AllGather", ALU.bypass, replica_groups=groups,
                                                             ins=[sendk[c].opt()], outs=[recvk[c].opt()]),
                 reads=["sendk%d" % c], writes=["recvk%d" % c], dma_key="cc", inc=1)
        for u in range(2):
            S.op("pool", lambda e, u=u: e.collective_compute("AllGather", ALU.bypass, replica_groups=groups,
                                                             ins=[sendv[u].opt()], outs=[recvv[u].opt()]),
                 reads=["sendv%d" % u], writes=["recvv%d" % u], dma_key="cc", inc=1)

    def phase_exchange_recv():
        for c in range(5):
            dma("sp", kT[:, c, 0:TH], recvk[c, 0:128, :], reads=["recvk%d" % c], writes=["kT"], key="ex2")
        for u in range(2):
            dma("sp", vbuf[u * 1024:(u + 1) * 1024, :], recvv[u, 0:1024, :], reads=["recvv%d" % u], writes=["vbuf"], key="ex3")

    def phase_attn(l):
        mk = A.mark()
        kct = [A.alloc([128, TE], BF16) for _ in range(2)]
        qct = [A.alloc([128, T], BF16) for _ in range(2)]
        vA = {d: A.alloc([128, TE // 128, 128], BF16) for d in DIL}
        vB = A.alloc([128, TE // 128, 64], BF16)
        accn = A.alloc([128, T], F32)
        accd = A.alloc([128, T], F32)
        rden = A.alloc([128, T], F32)
        oc = A.alloc([128, T], BF16)
        P = [A.alloc([128, 2, 256], BF16) for _ in range(2)]
        M4 = {}
        for nm, prev_idx in (("A", 1), ("B", 2)):
            for halo in (0, 1):
                mt = A.alloc([128, 2, 256], BF16)
                M4[(nm, halo)] = mt
                for h in range(2):
                    if halo:
                        S.op("dve", lambda e, mt=mt, h=h, prev_idx=prev_idx: e.tensor_scalar(
                            out=mt[:, h, 0:128], in0=maskf[:, prev_idx, :], scalar1=flagc[:, 0:1], scalar2=None, op0=ALU.mult),
                             reads=["maskf", "flagc"], writes=["M4"])
                    else:
                        S.op("dve", lambda e, mt=mt, h=h, prev_idx=prev_idx: e.tensor_copy(out=mt[:, h, 0:128], in_=maskf[:, prev_idx, :]),
                             reads=["maskf"], writes=["M4"])
                    S.op("dve", lambda e, mt=mt, h=h: e.tensor_copy(out=mt[:, h, 128:256], in_=maskf[:, 0, :]),
                         reads=["maskf"], writes=["M4"])
        ps_sa = [[ps[0], ps[1]], [ps[6], ps[7]]]

        def load_chunk(c):
            b = c % 2
            if c < 4:
                dma("sp", kct[b], kT[:, c, :], writes=["kct%d" % b], key="kct%d" % b)
            else:
                g = (c - 4) // 2
                dma("sp", kct[b][0:64, :], kT[g * 64:(g + 1) * 64, 4, :], writes=["kct%d" % b], key="kct%d" % b)
                dma("sp", kct[b][64:128, :], kT[g * 64:(g + 1) * 64, 4, :], writes=["kct%d" % b], key="kct%d" % b)
            dma("sp", qct[b], qT[:, c, :], writes=["qct%d" % b], key="qct%d" % b)

        def load_v(c, d):
            if c < 4:
                cols = slice(c * 128, (c + 1) * 128)
                if d == 1:
                    for u in range(TE // 2048):
                        src = vbuf[u * 2048:(u + 1) * 2048, cols].rearrange("(s i) c -> i s c", i=128)
                        dma("sp", vA[d][:, u * 16:(u + 1) * 16, :], src, writes=["vA%d" % d], key="vA%d" % d)
                else:
                    seg = 128 * d
                    for sg_ in range(TE // seg):
                        src = vbuf[sg_ * seg:(sg_ + 1) * seg, cols].rearrange("(i r) c -> i r c", r=d)
                        dma("sp", vA[d][:, sg_ * d:(sg_ + 1) * d, :], src, writes=["vA%d_%d" % (d, sg_ % 2)], key="vA%d_%d" % (d, sg_ % 2))
            else:
                g = (c - 4) // 2
                for u in range(TE // 2048):
                    src = vbuf[u * 2048:(u + 1) * 2048, 512 + g * 64:512 + (g + 1) * 64].rearrange("(s i) c -> i s c", i=128)
                    dma("sp", vB[:, u * 16:(u + 1) * 16, :], src, writes=["vB"], key="vB")

        DBG_NCH = int(os.environ.get("ATT_NCH", "8"))
        DBG_BR = tuple(int(v) for v in os.environ.get("ATT_BR", "1,4,16").split(","))
        DBG_NU = int(os.environ.get("ATT_NU", "9999"))
        load_chunk(0)
        for d in DIL:
            load_v(0, d)
        load_v(4, 1)
        for c in range(DBG_NCH):
            b = c % 2
            grpA = c < 4
            branches = tuple(d for d in DIL if d in DBG_BR) if grpA else (1,)
            KCt, QCt = kct[b], qct[b]
            first_branch = True
            for d in branches:
                nseg = T // (128 * d)
                Sh = TH // (128 * d)
                Qr = QCt.rearrange("p (s i r) -> p s r i", i=128, r=d)
                Kr = KCt.rearrange("p (s i r) -> p s r i", i=128, r=d)
                accn_r = accn.rearrange("p (s i r) -> p s r i", i=128, r=d)
                accd_r = accd.rearrange("p (s i r) -> p s r i", i=128, r=d)
                if grpA:
                    Vt = vA[d]
                    vtok = ["vA%d" % d] if d == 1 else ["vA%d_0" % d, "vA%d_1" % d]
                else:
                    Vt = vB
                    vtok = ["vB"]
                if d == 1:
                    groups_ = [[(s, 0) for s in range(g4 * 4, g4 * 4 + 4)] for g4 in range(nseg // 4)]
                else:
                    groups_ = [[(s, r) for r in range(r0, r0 + 4)] for s in range(nseg) for r0 in range(0, d, 4)]
                units = []
                for gi, g in enumerate(groups_):
                    for j, (s, r) in enumerate(g):
                        units.append((gi, j, s, r))

                def scores(n, u):
                    gi, j, s, r = u
                    alt = n % 2
                    Qv = Qr[:, s, r, :]
                    Kp = Kr[:, s + Sh - 1, r, :]
                    Kc_ = Kr[:, s + Sh, r, :]
                    for h in range(2):
                        lo, hi = h * 64, (h + 1) * 64
                        S.op("pe", lambda e, h=h, lo=lo, hi=hi, Kp=Kp, Qv=Qv, alt=alt: e.matmul(
                            ps_sa[alt][h][:, 0:128], lhsT=Kp[lo:hi, :], rhs=Qv[lo:hi, :], start=True, stop=True),
                             reads=["kct%d" % b, "qct%d" % b], writes=["pss%d" % alt])
                        S.op("pe", lambda e, h=h, lo=lo, hi=hi, Kc_=Kc_, Qv=Qv, alt=alt: e.matmul(
                            ps_sa[alt][h][:, 128:256], lhsT=Kc_[lo:hi, :], rhs=Qv[lo:hi, :], start=True, stop=True),
                             reads=["kct%d" % b, "qct%d" % b], writes=["pss%d" % alt])

                def rest(n, u):
                    gi, j, s, r = u
                    alt = n % 2
                    Pt = P[alt]
                    pn = ps[2 + gi % 2]
                    pd = ps[4 + gi % 2]
                    for h in range(2):
                        S.op("act", lambda e, h=h, Pt=Pt, alt=alt: e.activation(out=Pt[:, h, :], in_=ps_sa[alt][h][:, 0:256],
                                                                               func=AF.Exp, scale=0.125),
                             reads=["pss%d" % alt], writes=["P%d" % alt])
                    mt = M4[("A" if grpA else "B", 1 if s == 0 else 0)]
                    S.op("pool", lambda e, Pt=Pt, mt=mt: e.tensor_tensor(out=Pt, in0=Pt, in1=mt, op=ALU.mult),
                         reads=["P%d" % alt, "M4"], writes=["P%d" % alt])
                    nprev = (s + Sh - 1) * d + r
                    ncur = (s + Sh) * d + r
                    for h in range(2):
                        lo, hi = h * 64, (h + 1) * 64
                        if grpA:
                            Vp, Vc = Vt[:, nprev, lo:hi], Vt[:, ncur, lo:hi]
                        else:
                            Vp, Vc = Vt[:, nprev, :], Vt[:, ncur, :]
                        cols = slice(j * 128, (j + 1) * 128)
                        S.op("pe", lambda e, Vp=Vp, Pt=Pt, h=h, lo=lo, hi=hi, cols=cols, pn=pn: e.matmul(
                            pn[lo:hi, cols], lhsT=Vp, rhs=Pt[:, h, 0:128], start=True, stop=False),
                             reads=["P%d" % alt] + vtok, writes=["ps%d" % (2 + gi % 2)])
                        S.op("pe", lambda e, Vc=Vc, Pt=Pt, h=h, lo=lo, hi=hi, cols=cols, pn=pn: e.matmul(
                            pn[lo:hi, cols], lhsT=Vc, rhs=Pt[:, h, 128:256], start=False, stop=True),
                             reads=["P%d" % alt] + vtok, writes=["ps%d" % (2 + gi % 2)])
                        S.op("pe", lambda e, Pt=Pt, h=h, lo=lo, hi=hi, cols=cols, pd=pd: e.matmul(
                            pd[lo:hi, cols], lhsT=ones_bf[:, 0:64], rhs=Pt[:, h, 0:128], start=True, stop=False),
                             reads=["P%d" % alt, "ones"], writes=["ps%d" % (4 + gi % 2)])
                        S.op("pe", lambda e, Pt=Pt, h=h, lo=lo, hi=hi, cols=cols, pd=pd: e.matmul(
                            pd[lo:hi, cols], lhsT=ones_bf[:, 0:64], rhs=Pt[:, h, 128:256], start=False, stop=True),
                             reads=["P%d" % alt, "ones"], writes=["ps%d" % (4 + gi % 2)])
                    if j == 3 and not os.environ.get("ATT_NOACC"):
                        g = groups_[gi]
                        s0, r0 = g[0]
                        if d == 1:
                            an = accn[:, s0 * 128:(s0 + 4) * 128]
                            ad = accd[:, s0 * 128:(s0 + 4) * 128]
                            pnv, pdv = pn[:, :], pd[:, :]
                            pieces = [s0 // 4]
                        else:
                            an = accn_r[:, s0, r0:r0 + 4, :]
                            ad = accd_r[:, s0, r0:r0 + 4, :]
                            pnv = pn[:, :].rearrange("p (r i) -> p r i", i=128)
                            pdv = pd[:, :].rearrange("p (r i) -> p r i", i=128)
                            seglen = 128 * d
                            pieces = list(range(s0 * seglen // 512, (s0 + 1) * seglen // 512))
                        ntok = ["accnP%d" % p_ for p_ in pieces]
                        dtok = ["accdP%d" % p_ for p_ in pieces]
                        if first_branch:
                            S.op("dve", lambda e, an=an, pnv=pnv: e.tensor_copy(out=an, in_=pnv),
                                 reads=["ps%d" % (2 + gi % 2)], writes=ntok)
                            S.op("dve", lambda e, ad=ad, pdv=pdv: e.tensor_copy(out=ad, in_=pdv),
                                 reads=["ps%d" % (4 + gi % 2)], writes=dtok)
                        else:
                            S.op("dve", lambda e, an=an, pnv=pnv: e.tensor_tensor(out=an, in0=an, in1=pnv, op=ALU.add),
                                 reads=["ps%d" % (2 + gi % 2)] + ntok, writes=ntok)
                            S.op("dve", lambda e, ad=ad, pdv=pdv: e.tensor_tensor(out=ad, in0=ad, in1=pdv, op=ALU.add),
                                 reads=["ps%d" % (4 + gi % 2)] + dtok, writes=dtok)

                units = units[:DBG_NU]
                NOPIPE = bool(os.environ.get("ATT_NOPIPE"))
                if units and not NOPIPE:
                    scores(0, units[0])
                for n, u in enumerate(units):
                    if NOPIPE:
                        scores(n, u)
                    elif n + 1 < len(units):
                        scores(n + 1, units[n + 1])
                    rest(n, u)
                first_branch = False
                if c + 1 < 8 and d == branches[0]:
                    load_chunk(c + 1)
                if c + 1 < 4:
                    load_v(c + 1, d)
            if c == 5:
                load_v(6, 1)
            for pc in range(T // 512):
                cs_ = slice(pc * 512, (pc + 1) * 512)
                if not grpA:
                    j4 = l * 4 + (c - 4)
                    S.op("dve", lambda e, j4=j4, cs_=cs_: e.tensor_scalar(out=accd[:, cs_], in0=accd[:, cs_], scalar1=sinkcol[:, j4:j4 + 1],
                                                                        scalar2=None, op0=ALU.add),
                         reads=["accdP%d" % pc, "sinkcol"], writes=["accdP%d" % pc])
                S.op("dve", lambda e, cs_=cs_: e.reciprocal(out=rden[:, cs_], in_=accd[:, cs_]), reads=["accdP%d" % pc], writes=["rdenP%d" % pc])
                S.op("pool", lambda e, cs_=cs_: e.tensor_tensor(out=oc[:, cs_], in0=accn[:, cs_], in1=rden[:, cs_], op=ALU.mult),
                     reads=["accnP%d" % pc, "rdenP%d" % pc], writes=["ocP%d" % pc, "oc"])
            dma("sp", oT[:, c, :], oc, reads=["oc"], writes=["oT"], key="oc")
        S.barrier()
        A.release(mk)

    def phase_ffn(l, li_dense, li_moe, last):
        moe = layer_types[l] == 1
        mk = A.mark()
        g0 = l * NG
        yacc = A.alloc([128, 8, D], F32)
        hTf = A.alloc([128, KC, 1024], BF16)
        gate = A.alloc([128, 8, NE], F32)
        wd = [A.alloc([128, NJ // 2, D], BF16) for _ in range(2)]
        wg = [A.alloc([128, KC, 256], BF16) for _ in range(2)]
        wu = [A.alloc([128, KC, 256], BF16) for _ in range(2)]
        sg = [A.alloc([128, 512], F32) for _ in range(2)]
        rt32 = A.alloc([128, KC, NE], F32)
        mU = A.mark()
        aT = A.alloc([128, NJ, 1024], BF16)
        A.release(mU)
        wout = A.alloc([128, KC, D], BF16)
        ot = A.alloc([128, 8, 512], BF16)
        mixT = A.alloc([128, 8, 512], BF16)
        rsa = A.alloc([128, 2, 512], F32)
        rsb = A.alloc([128, 2, 512], F32)
        xn = A.alloc([128, D], F32)
        xn2 = [xn, A.alloc([128, D], F32)]
        junk = A.alloc([128, D], BF16)
        h32 = A.alloc([128, KC, 512], F32)
        sm = A.alloc([128, 16], F32)
        gsc = A.alloc([128, 48], F32)
        gfin = A.alloc([128, D], F32) if last else None
        NEXP = NE if moe else 1
        if moe:
            dma("sp", rt32, router[li_moe].rearrange("(k p) e -> p k e", p=128), writes=["rt32"], key="rt32")
        if last:
            dma("sp", gfin, fin_norm.partition_broadcast(128), writes=["gfin"], key="gfin")

        def wsrc(e):
            if moe:
                return mwg[li_moe, e], mwu[li_moe, e], mwd[li_moe, e]
            return dwg[li_dense], dwu[li_dense], dwd[li_dense]

        def load_gu(e, g):
            b = g % 2
            G_, U_, _ = wsrc(e)
            dma("pool", wg[b], G_[:, g * 256:(g + 1) * 256].rearrange("(k p) c -> p k c", p=128), writes=["wg%d" % b], key="wg%d" % b)
            dma("pool", wu[b], U_[:, g * 256:(g + 1) * 256].rearrange("(k p) c -> p k c", p=128), writes=["wu%d" % b], key="wu%d" % b)

        def load_wd(e, half):
            _, _, D_ = wsrc(e)
            for q in range(2):
                j0 = half * 14 + q * 7
                dma("pool", wd[half][:, q * 7:(q + 1) * 7, :], D_[j0 * 128:(j0 + 7) * 128, :].rearrange("(j p) c -> p j c", p=128),
                    writes=["wd%d" % half], key="wd%d" % half)

        for t4 in range(T // 1024):
            dma("pool", wout[:, 0:4, :], w_out[l][0:512, :].rearrange("(k p) c -> p k c", p=128), writes=["wout0"], key="wout0")
            dma("pool", wout[:, 4:8, :], w_out[l][512:1024, :].rearrange("(k p) c -> p k c", p=128), writes=["wout1"], key="wout1")
            load_gu(0, 0)
            load_gu(0, 1)
            load_wd(0, 0)
            load_wd(0, 1)
            dma("sp", yacc, (x_in if l == 0 else xres)[t4 * 1024:(t4 + 1) * 1024, :].rearrange("(b p) d -> p b d", p=128),
                writes=["yacc%d" % i for i in range(8)], key="yacc")
            for st in range(2):
                tok0 = t4 * 1024 + st * 512
                dma("sp", ot, oT[:, :, tok0:tok0 + 512], writes=["ot"], key="ot")
                S.op("act", lambda e: e.activation(out=mixT, in_=ot, func=AF.Square), reads=["ot"],
                     writes=["mixT"] + ["mixT%d" % c for c in range(8)])
                for grp in range(2):
                    for c in range(4):
                        S.op("pe", lambda e, grp=grp, c=c: e.matmul(ps[grp][:, :], lhsT=ones_bf, rhs=mixT[:, grp * 4 + c, :],
                                                                    start=(c == 0), stop=(c == 3)),
                             reads=["mixT", "ones"], writes=["ps%d" % grp])
                    rsx = rsa if grp == 0 else rsb
                    S.op("act", lambda e, grp=grp, rsx=rsx: e.activation(out=rsx[:, 0, :], in_=ps[grp][:, :], func=AF.Sqrt,
                                                                         bias=epsc[:, 0:1], scale=1.0 / 512),
                         reads=["ps%d" % grp, "epsc"], writes=["rs%d_0" % grp])
                    S.op("dve", lambda e, rsx=rsx: e.reciprocal(out=rsx[:, 1, :], in_=rsx[:, 0, :]),
                         reads=["rs%d_0" % grp], writes=["rs%d_1" % grp])
                for c in range(8):
                    rsx = rsa if c < 4 else rsb
                    gc = g0 + 16 + c
                    S.op("dve", lambda e, c=c, rsx=rsx, gc=gc: e.scalar_tensor_tensor(
                        out=mixT[:, c, :], in0=ot[:, c, :], scalar=gcol[:, gc:gc + 1], in1=rsx[:, 1, :], op0=ALU.mult, op1=ALU.mult),
                         reads=["ot", "rs%d_1" % (0 if c < 4 else 1), "gcol%d" % l], writes=["mixT%d" % c, "mixT"])
                mreads = ["mixT%d" % c for c in range(8)]

                def p3_mm(bb):
                    blk = st * 4 + bb
                    q = bb % 2
                    xq = xn2[q]
                    for half in range(2):
                        pb = ps[2 + half]
                        for c in range(8):
                            S.op("pe", lambda e, bb=bb, half=half, c=c, pb=pb: e.matmul(
                                pb[:, :], lhsT=mixT[:, c, bb * 128:(bb + 1) * 128], rhs=wout[:, c, half * 512:(half + 1) * 512],
                                start=(c == 0), stop=(c == 7)),
                                 reads=[mreads[c], "wout%d" % (c // 4)], writes=["ps%d" % (2 + half)])
                        S.op("dve", lambda e, blk=blk, half=half, pb=pb: e.tensor_tensor(
                            out=yacc[:, blk, half * 512:(half + 1) * 512], in0=yacc[:, blk, half * 512:(half + 1) * 512],
                            in1=pb[:, :], op=ALU.add),
                             reads=["ps%d" % (2 + half), "yacc%d" % blk], writes=["yacc%d" % blk])
                    c0 = 9 + 3 * q if q else 0
                    S.op("act", lambda e, blk=blk, c0=c0: e.activation(out=junk, in_=yacc[:, blk, :], func=AF.Square, accum_out=sm[:, c0:c0 + 1]),
                         reads=["yacc%d" % blk], writes=["sm0_%d" % q])
                    S.op("act", lambda e, c0=c0: e.activation(out=sm[:, c0 + 1:c0 + 2], in_=sm[:, c0:c0 + 1], func=AF.Sqrt, bias=epsc[:, 0:1], scale=1.0 / D),
                         reads=["sm0_%d" % q, "epsc"], writes=["sm1_%d" % q])
                    S.op("dve", lambda e, c0=c0: e.reciprocal(out=sm[:, c0 + 2:c0 + 3], in_=sm[:, c0 + 1:c0 + 2]), reads=["sm1_%d" % q], writes=["sm2_%d" % q])
                    S.op("dve", lambda e, blk=blk, c0=c0, xq=xq: e.tensor_scalar(out=xq, in0=yacc[:, blk, :], scalar1=sm[:, c0 + 2:c0 + 3], scalar2=None, op0=ALU.mult),
                         reads=["yacc%d" % blk, "sm2_%d" % q], writes=["xn_%d" % q])

                def p3_tr(bb):
                    q = bb % 2
                    xq = xn2[q]
                    pbank = (4, 5) if q == 0 else (0, 1)
                    for kc in range(KC):
                        bi = pbank[kc // 4]
                        S.op("pe", lambda e, kc=kc, bi=bi, xq=xq: e.transpose(ps[bi][:, (kc % 4) * 128:(kc % 4 + 1) * 128],
                                                                             xq[:, kc * 128:(kc + 1) * 128], ident),
                             reads=["xn_%d" % q, "ident"], writes=["ps%d" % bi])
                    for kc in range(KC):
                        bi = pbank[kc // 4]
                        S.op("act", lambda e, kc=kc, bi=bi, bb=bb: e.activation(
                            out=h32[:, kc, bb * 128:(bb + 1) * 128], in_=ps[bi][:, (kc % 4) * 128:(kc % 4 + 1) * 128], func=AF.Copy,
                            scale=gcol[:, g0 + 8 + kc:g0 + 8 + kc + 1]),
                             reads=["ps%d" % bi, "gcol%d" % l], writes=["h32_%d" % bb])
                    if moe:
                        for kc in range(KC):
                            S.op("pe", lambda e, kc=kc, bb=bb: e.matmul(ps[6][:, bb * 8:(bb + 1) * 8], lhsT=h32[:, kc, bb * 128:(bb + 1) * 128],
                                                                        rhs=rt32[:, kc, :], start=(kc == 0), stop=(kc == KC - 1)),
                                 reads=["h32_%d" % bb, "rt32"], writes=["ps6"])

                p3_mm(0)
                for bb in range(4):
                    if bb + 1 < 4:
                        p3_mm(bb + 1)
                    p3_tr(bb)
                S.op("pool", lambda e, st=st: e.tensor_copy(out=hTf[:, :, st * 512:(st + 1) * 512], in_=h32),
                     reads=["h32_%d" % i for i in range(4)], writes=["hTf%d" % st])
                if moe:
                    lg = sm
                    for bb in range(4):
                        blk = st * 4 + bb
                        Lg = ps[6][:, bb * 8:(bb + 1) * 8]
                        gb = gate[:, blk, :]
                        S.op("dve", lambda e, Lg=Lg: e.tensor_copy(out=gsc[:, 0:8], in_=Lg), reads=["ps6"], writes=["lg"])
                        S.op("dve", lambda e: e.tensor_reduce(out=sm[:, 4:5], in_=gsc[:, 0:8], axis=AX.X, op=ALU.max),
                             reads=["lg"], writes=["m1"])
                        S.op("dve", lambda e: e.tensor_scalar(out=gsc[:, 8:16], in0=gsc[:, 0:8], scalar1=sm[:, 4:5], scalar2=None,
                                                              op0=ALU.is_equal), reads=["lg", "m1"], writes=["eq"])
                        S.op("dve", lambda e: e.scalar_tensor_tensor(out=gsc[:, 16:24], in0=gsc[:, 8:16], scalar=-1e30,
                                                                     in1=gsc[:, 0:8], op0=ALU.mult, op1=ALU.add),
                             reads=["eq", "lg"], writes=["lg2"])
                        S.op("dve", lambda e: e.tensor_reduce(out=sm[:, 5:6], in_=gsc[:, 16:24], axis=AX.X, op=ALU.max),
                             reads=["lg2"], writes=["m2"])
                        S.op("dve", lambda e: e.tensor_scalar(out=gsc[:, 24:32], in0=gsc[:, 0:8], scalar1=sm[:, 5:6], scalar2=None,
                                                              op0=ALU.is_ge), reads=["lg", "m2"], writes=["sel"])
                        S.op("dve", lambda e: e.tensor_scalar(out=sm[:, 6:7], in0=sm[:, 4:5], scalar1=-1.0, scalar2=None, op0=ALU.mult),
                             reads=["m1"], writes=["nm1"])
                        S.op("act", lambda e: e.activation(out=gsc[:, 32:40], in_=gsc[:, 0:8], func=AF.Exp, bias=sm[:, 6:7]),
                             reads=["lg", "nm1"], writes=["ex"])
                        S.op("dve", lambda e: e.tensor_tensor(out=gsc[:, 40:48], in0=gsc[:, 32:40], in1=gsc[:, 24:32], op=ALU.mult),
                             reads=["ex", "sel"], writes=["exs"])
                        S.op("dve", lambda e: e.tensor_reduce(out=sm[:, 7:8], in_=gsc[:, 40:48], axis=AX.X, op=ALU.add),
                             reads=["exs"], writes=["se"])
                        S.op("dve", lambda e: e.reciprocal(out=sm[:, 8:9], in_=sm[:, 7:8]), reads=["se"], writes=["rse"])
                        S.op("dve", lambda e, gb=gb: e.tensor_scalar(out=gb, in0=gsc[:, 40:48], scalar1=sm[:, 8:9], scalar2=None, op0=ALU.mult),
                             reads=["exs", "rse"], writes=["gate%d" % blk])
            S.barrier()
            hreads = ["hTf0", "hTf1"]
            for e_ in range(NEXP):
                for g in range(14):
                    b = g % 2
                    for jj in range(2):
                        j = g * 2 + jj
                        for th in range(2):
                            a = (jj * 2 + th) % 2
                            pG, pU = ps[a], ps[2 + a]
                            for kc in range(KC):
                                S.op("pe", lambda e, kc=kc, b=b, jj=jj, th=th, pG=pG: e.matmul(
                                    pG[:, :], lhsT=wg[b][:, kc, jj * 128:(jj + 1) * 128], rhs=hTf[:, kc, th * 512:(th + 1) * 512],
                                    start=(kc == 0), stop=(kc == KC - 1)),
                                     reads=["wg%d" % b, hreads[th]], writes=["ps%d" % a])
                            for kc in range(KC):
                                S.op("pe", lambda e, kc=kc, b=b, jj=jj, th=th, pU=pU: e.matmul(
                                    pU[:, :], lhsT=wu[b][:, kc, jj * 128:(jj + 1) * 128], rhs=hTf[:, kc, th * 512:(th + 1) * 512],
                                    start=(kc == 0), stop=(kc == KC - 1)),
                                     reads=["wu%d" % b, hreads[th]], writes=["ps%d" % (2 + a)])
                            S.op("act", lambda e, a=a, pG=pG: e.activation(out=sg[a], in_=pG[:, :], func=AF.Silu),
                                 reads=["ps%d" % a], writes=["sg%d" % a])
                            S.op("dve", lambda e, a=a, pU=pU, j=j, th=th: e.tensor_tensor(
                                out=aT[:, j, th * 512:(th + 1) * 512], in0=sg[a], in1=pU[:, :], op=ALU.mult),
                                 reads=["sg%d" % a, "ps%d" % (2 + a)], writes=["aT%d" % (j // 14)])
                    if g + 2 < 14:
                        load_gu(e_, g + 2)
                    elif e_ + 1 < NEXP:
                        load_gu(e_ + 1, g + 2 - 14)
                for half in range(2):
                    for blk in range(8):
                        for ch in range(2):
                            pb = ps[4 + (blk * 2 + ch) % 4]
                            ptok = "ps%d" % (4 + (blk * 2 + ch) % 4)
                            for jx in range(14):
                                j = half * 14 + jx
                                S.op("pe", lambda e, blk=blk, ch=ch, jx=jx, j=j, pb=pb, half=half: e.matmul(
                                    pb[:, :], lhsT=aT[:, j, blk * 128:(blk + 1) * 128], rhs=wd[half][:, jx, ch * 512:(ch + 1) * 512],
                                    start=(jx == 0), stop=(jx == 13)),
                                     reads=["aT%d" % half, "wd%d" % half], writes=[ptok])
                            ys = yacc[:, blk, ch * 512:(ch + 1) * 512]
                            if moe:
                                S.op("dve", lambda e, pb=pb, ys=ys, blk=blk, e_=e_: e.scalar_tensor_tensor(
                                    out=ys, in0=pb[:, :], scalar=gate[:, blk, e_:e_ + 1], in1=ys, op0=ALU.mult, op1=ALU.add),
                                     reads=[ptok, "yacc%d" % blk, "gate%d" % blk], writes=["yacc%d" % blk])
                            else:
                                S.op("dve", lambda e, pb=pb, ys=ys: e.tensor_tensor(out=ys, in0=ys, in1=pb[:, :], op=ALU.add),
                                     reads=[ptok, "yacc%d" % blk], writes=["yacc%d" % blk])
                    if e_ + 1 < NEXP:
                        load_wd(e_ + 1, half)
            if not last:
                dma("sp", xres[t4 * 1024:(t4 + 1) * 1024, :].rearrange("(b p) d -> p b d", p=128), yacc,
                    reads=["yacc%d" % i for i in range(8)], writes=["xres"], key="xst")
            else:
                S.barrier()
                for blk in range(8):
                    S.op("act", lambda e, blk=blk: e.activation(out=junk, in_=yacc[:, blk, :], func=AF.Square, accum_out=sm[:, 0:1]),
                         reads=["yacc%d" % blk], writes=["sm0"])
                    S.op("act", lambda e: e.activation(out=sm[:, 1:2], in_=sm[:, 0:1], func=AF.Sqrt, bias=epsc[:, 0:1], scale=1.0 / D),
                         reads=["sm0", "epsc"], writes=["sm1"])
                    S.op("dve", lambda e: e.reciprocal(out=sm[:, 2:3], in_=sm[:, 1:2]), reads=["sm1"], writes=["sm2"])
                    S.op("dve", lambda e, blk=blk: e.scalar_tensor_tensor(out=yacc[:, blk, :], in0=yacc[:, blk, :], scalar=sm[:, 2:3],
                                                                         in1=gfin, op0=ALU.mult, op1=ALU.mult),
                         reads=["yacc%d" % blk, "sm2", "gfin"], writes=["yacc%d" % blk])
                dma("sp", out[t4 * 1024:(t4 + 1) * 1024, :].rearrange("(b p) d -> p b d", p=128), yacc,
                    reads=["yacc%d" % i for i in range(8)], writes=["out"], key="xst")
            S.barrier()
        A.release(mk)

    nd = nm = 0
    for l, lt in enumerate(layer_types):
        if stop_after == "setup":
            break
        if not os.environ.get("SKIP_PROJ"):
            phase_proj(l)
            if stop_after in ("proj", "exch"):
                break
        phase_attn(l)
        if stop_after == "attn":
            break
        phase_ffn(l, nd, nm, l == L - 1)
        if lt == 0:
            nd += 1
        else:
            nm += 1
    S.emit()
    return nc


def _consts():
    k = np.arange(128)[:, None]
    q = np.arange(128)[None, :]
    ident = (k == q).astype(np.float32)
    src = (q // 64) * 64 + ((q % 64) + 32) % 64
    perm = (k == src).astype(np.float32)
    mask = np.stack([(q >= k), (k >= q), (k > q)], axis=1).astype(np.float32)
    p = np.arange(128)
    invf = (10000.0 ** (-(p % 32).astype(np.float64) / 32.0)).astype(np.float32)
    sgn = np.where((p % 64) < 32, -1.0, 1.0).astype(np.float32)
    col = np.stack([invf, sgn], axis=1).astype(np.float32)
    return ident, perm, np.ascontiguousarray(mask), np.ascontiguousarray(col)


_CACHE = {}


def run_layers(inputs, layer_types, debug_out=(), stop_after=None):
    L = len(layer_types)
    key = (tuple(layer_types), tuple(debug_out), stop_after)
    if key not in _CACHE:
        _CACHE[key] = build_program(list(layer_types), debug_out=debug_out, stop_after=stop_after)
    nc = _CACHE[key]
    ident, perm, mask, col = _consts()
    f32 = lambda a: np.ascontiguousarray(np.asarray(a, dtype=np.float32))
    x = f32(inputs["x"])
    pos = np.ascontiguousarray(np.asarray(inputs["positions"], dtype=np.int32))
    shared = {
        "c_ident": ident, "c_perm": perm, "c_mask": mask, "c_col": col,
        "attn_norm": f32(inputs["attn_norm"])[:L], "w_in": f32(inputs["w_in"])[:L],
        "mix_norm_a": f32(inputs["mix_norm_a"])[:L], "mix_norm_b": f32(inputs["mix_norm_b"])[:L],
        "sinks": f32(inputs["sinks"])[:L].reshape(1, L * 8), "w_out": f32(inputs["w_out"])[:L],
        "ffn_norm": f32(inputs["ffn_norm"])[:L], "final_norm": f32(inputs["final_norm"]).reshape(1, D),
    }
    nd = sum(1 for t in layer_types if t == 0)
    nm = sum(1 for t in layer_types if t == 1)
    if nd:
        shared["dense_w_gate"] = f32(inputs["dense_w_gate"])[:nd]
        shared["dense_w_up"] = f32(inputs["dense_w_up"])[:nd]
        shared["dense_w_down"] = f32(inputs["dense_w_down"])[:nd]
    if nm:
        shared["router"] = f32(inputs["router"])[:nm]
        shared["moe_w_gate"] = f32(inputs["moe_w_gate"])[:nm]
        shared["moe_w_up"] = f32(inputs["moe_w_up"])[:nm]
        shared["moe_w_down"] = f32(inputs["moe_w_down"])[:nm]
    in_maps = []
    for c in range(NCORES):
        b, h = c // 2, c % 2
        m = dict(shared)
        m["x"] = np.ascontiguousarray(x[b, h * T:(h + 1) * T, :])
        m["pos"] = np.ascontiguousarray(pos[h * T:(h + 1) * T][None, :])
        m["flag"] = np.full((128, 1), float(h), dtype=np.float32)
        in_maps.append(m)
    res = run_bass_kernel_spmd(nc, in_maps, core_ids=list(range(NCORES)))
    outp = np.empty((4, 2 * T, D), dtype=np.float32)
    for c in range(NCORES):
        b, h = c // 2, c % 2
        outp[b, h * T:(h + 1) * T, :] = np.asarray(res.results[c]["out"], dtype=np.float32)
    if debug_out:
        return outp, res.results
    return outp


def kernel(**inputs):
    return run_layers(inputs, [0, 1, 0, 1])
```
